# Optimizing a Trainium2 kernel written in Bass

```python
import jax, jax.numpy as jnp
from jax import lax
import numpy as np

D_MODEL = 2048
BATCH = 8
SEQ = 2048
DEPTH = 1

NSA_HEADS = 8
NSA_KV_GROUPS = 2
NSA_HEAD_DIM = 128
CMP_BLOCK = 32
CMP_STRIDE = 16
CMP_HIDDEN = 256
SEL_BLOCK = 64
SEL_TOPK = 8
N_LOCAL_BLOCKS = 2
WINDOW = 512
Q_BLOCK = 64
GLA_HEADS = 4
GLA_KEY_DIM = 128
GLA_VAL_DIM = 256
GATE_RANK = 16
GATE_TAU = 16.0
GLA_CHUNK = 64
GLA_SUB = 16
N_EXPERT_GROUPS = 4
EXPERTS_PER_GROUP = 8
TOPK_IN_GROUP = 2
D_EXPERT = 512

EPS = 1e-6
MASK_VALUE = -1e30

NSA_Q_WIDTH = NSA_HEADS * NSA_HEAD_DIM
NSA_KV_WIDTH = NSA_KV_GROUPS * NSA_HEAD_DIM
GLA_QK_WIDTH = GLA_HEADS * GLA_KEY_DIM
GLA_V_WIDTH = GLA_HEADS * GLA_VAL_DIM
MIX_WIDTH = NSA_Q_WIDTH + GLA_V_WIDTH
IN_PROJ_SIZES = (NSA_Q_WIDTH,) + (NSA_KV_WIDTH,) * 6 + (3 * NSA_HEADS, GLA_QK_WIDTH, GLA_QK_WIDTH, GLA_V_WIDTH, GATE_RANK, GLA_V_WIDTH)
IN_PROJ_WIDTH = sum(IN_PROJ_SIZES)

kernel_name = "hymba_nsa_gla_hmoe_alibi"


def rms_norm(x, gain):
    xf = x.astype(jnp.float32)
    y = xf * lax.rsqrt(jnp.mean(jnp.square(xf), axis=-1, keepdims=True) + EPS)
    return (y * gain.astype(jnp.float32)).astype(x.dtype)


def alibi_slopes(n):
    return jnp.asarray(2.0 ** (-8.0 * np.arange(1, n + 1) / n), dtype=jnp.float32)


def compress_blocks(kv, pos_emb, w1, w2):
    s = kv.shape[2]
    n_cmp = (s - CMP_BLOCK) // CMP_STRIDE + 1
    idx = np.arange(n_cmp)[:, None] * CMP_STRIDE + np.arange(CMP_BLOCK)[None, :]
    blocks = kv[:, :, idx] + pos_emb
    flat = blocks.reshape(blocks.shape[:3] + (CMP_BLOCK * NSA_HEAD_DIM,))
    return jax.nn.gelu(flat @ w1) @ w2


def nsa_attention(q, k_cmp, v_cmp, k_sel, v_sel, k_win, v_win, gate_logits,
                  cmp_pos_k, w_cmp_k1, w_cmp_k2, cmp_pos_v, w_cmp_v1, w_cmp_v2):
    b, s = q.shape[:2]
    g, hpg, dk = NSA_KV_GROUPS, NSA_HEADS // NSA_KV_GROUPS, NSA_HEAD_DIM
    q = q.reshape(b, s, g, hpg, dk).transpose(0, 2, 3, 1, 4)
    to_groups = lambda a: a.reshape(b, s, g, dk).transpose(0, 2, 1, 3)
    gates = jax.nn.sigmoid(gate_logits.astype(jnp.float32)).reshape(b, s, g, hpg, 3).transpose(0, 2, 3, 1, 4)

    kc = compress_blocks(to_groups(k_cmp), cmp_pos_k, w_cmp_k1, w_cmp_k2)
    vc = compress_blocks(to_groups(v_cmp), cmp_pos_v, w_cmp_v1, w_cmp_v2)
    n_cmp = kc.shape[2]
    cmp_end_np = np.arange(n_cmp) * CMP_STRIDE + CMP_BLOCK - 1
    cmp_end = jnp.asarray(cmp_end_np, dtype=jnp.int32)
    cmp_end_f = cmp_end.astype(jnp.float32)

    n_sel = s // SEL_BLOCK
    ks_blocks = to_groups(k_sel).reshape(b, g, n_sel, SEL_BLOCK, dk)
    vs_blocks = to_groups(v_sel).reshape(b, g, n_sel, SEL_BLOCK, dk)
    c_start = np.arange(n_cmp)[:, None] * CMP_STRIDE
    s_start = np.arange(n_sel)[None, :] * SEL_BLOCK
    overlap = jnp.asarray(((c_start < s_start + SEL_BLOCK) & (c_start + CMP_BLOCK > s_start)).astype(np.float32))
    topk = min(SEL_TOPK, n_sel)
    sel_start = jnp.arange(n_sel) * SEL_BLOCK
    blk = jnp.arange(n_sel)
    b_idx = jnp.arange(b)[:, None, None, None]
    g_idx = jnp.arange(g)[None, :, None, None]

    pad = ((0, 0), (0, 0), (WINDOW, 0), (0, 0))
    kwp = jnp.pad(to_groups(k_win), pad)
    vwp = jnp.pad(to_groups(v_win), pad)

    slopes = alibi_slopes(NSA_HEADS).reshape(g, hpg)[None, :, :, None, None]
    scale = dk ** -0.5
    nqb = s // Q_BLOCK
    q_blocks = q.reshape(b, g, hpg, nqb, Q_BLOCK, dk).transpose(3, 0, 1, 2, 4, 5)
    g_blocks = gates.reshape(b, g, hpg, nqb, Q_BLOCK, 3).transpose(3, 0, 1, 2, 4, 5)

    def block_fn(args):
        qb, qc, gc = args
        t = qb * Q_BLOCK + jnp.arange(Q_BLOCK)
        tf = t.astype(jnp.float32)
        s_c = jnp.einsum('bghqd,bgnd->bghqn', qc, kc).astype(jnp.float32) * scale
        s_c = s_c - slopes * (tf[:, None] - cmp_end_f[None, :])
        valid_c = cmp_end[None, :] <= t[:, None]
        s_c = jnp.where(valid_c, s_c, MASK_VALUE)
        p_c = jax.nn.softmax(s_c, axis=-1) * jnp.any(valid_c, axis=-1)[:, None].astype(jnp.float32)
        o_c = jnp.einsum('bghqn,bgnd->bghqd', p_c.astype(vc.dtype), vc).astype(jnp.float32)
        imp = jnp.einsum('bghqn,nj->bgqj', p_c, overlap)
        cur = t // SEL_BLOCK
        causal_blk = sel_start[None, :] <= t[:, None]
        forced = (blk[None, :] == 0) | ((blk[None, :] <= cur[:, None]) & (blk[None, :] > cur[:, None] - N_LOCAL_BLOCKS))
        imp = jnp.where(forced, -MASK_VALUE, jnp.where(causal_blk, imp, MASK_VALUE))
        _, sel = lax.top_k(imp, topk)
        k_g = ks_blocks[b_idx, g_idx, sel]
        v_g = vs_blocks[b_idx, g_idx, sel]
        pos = sel[..., None] * SEL_BLOCK + jnp.arange(SEL_BLOCK)
        dist = t[:, None, None] - pos
        s_s = jnp.einsum('bghqd,bgqksd->bghqks', qc, k_g).astype(jnp.float32) * scale
        s_s = s_s - slopes[..., None] * dist[:, :, None].astype(jnp.float32)
        s_s = jnp.where((dist >= 0)[:, :, None], s_s, MASK_VALUE)
        p_s = jax.nn.softmax(s_s.reshape(s_s.shape[:4] + (topk * SEL_BLOCK,)), axis=-1).reshape(s_s.shape)
        o_s = jnp.einsum('bghqks,bgqksd->bghqd', p_s.astype(v_g.dtype), v_g).astype(jnp.float32)
        start = qb * Q_BLOCK
        kw = lax.dynamic_slice_in_dim(kwp, start, WINDOW + Q_BLOCK, axis=2)
        vw = lax.dynamic_slice_in_dim(vwp, start, WINDOW + Q_BLOCK, axis=2)
        pos_w = start - WINDOW + jnp.arange(WINDOW + Q_BLOCK)
        dw = t[:, None] - pos_w[None, :]
        valid_w = (pos_w[None, :] >= 0) & (dw >= 0) & (dw < WINDOW)
        s_w = jnp.einsum('bghqd,bgkd->bghqk', qc, kw).astype(jnp.float32) * scale - slopes * dw.astype(jnp.float32)
        s_w = jnp.where(valid_w, s_w, MASK_VALUE)
        p_w = jax.nn.softmax(s_w, axis=-1)
        o_w = jnp.einsum('bghqk,bgkd->bghqd', p_w.astype(vw.dtype), vw).astype(jnp.float32)
        out = gc[..., 0:1] * o_c + gc[..., 1:2] * o_s + gc[..., 2:3] * o_w
        return out.astype(qc.dtype)

    outs = lax.map(block_fn, (jnp.arange(nqb), q_blocks, g_blocks))
    return outs.transpose(1, 0, 4, 2, 3, 5).reshape(b, s, NSA_Q_WIDTH)


def gla_attention(q, k, v, alpha_lr, out_gate, w_alpha2, b_alpha, g_norm):
    b, s = q.shape[:2]
    h, dk, dv = GLA_HEADS, GLA_KEY_DIM, GLA_VAL_DIM
    out_dtype = q.dtype
    q = q.reshape(b, s, h, dk).astype(jnp.float32) * dk ** -0.5
    k = k.reshape(b, s, h, dk).astype(jnp.float32)
    v = v.reshape(b, s, h, dv).astype(jnp.float32)
    glog = jax.nn.log_sigmoid((alpha_lr @ w_alpha2 + b_alpha).astype(jnp.float32)) / GATE_TAU
    glog = glog.reshape(b, s, h, dk)
    n_chunks = s // GLA_CHUNK
    ns = GLA_CHUNK // GLA_SUB
    to_chunks = lambda a: a.reshape(b, n_chunks, GLA_CHUNK, h, a.shape[-1]).transpose(1, 0, 3, 2, 4)
    tri = np.tril(np.ones((GLA_SUB, GLA_SUB), dtype=bool))
    strict_lower = np.tril(np.ones((ns, ns), dtype=bool), -1)
    eye = jnp.asarray(np.eye(ns, dtype=np.float32))

    def step(state, inp):
        qc, kc, vc, gc = inp
        bcum = jnp.cumsum(gc, axis=2)
        b_last = bcum[:, :, -1]
        o_inter = jnp.einsum('bhcd,bhde->bhce', qc * jnp.exp(bcum), state)
        qs = qc.reshape(b, h, ns, GLA_SUB, dk)
        ks = kc.reshape(b, h, ns, GLA_SUB, dk)
        vs = vc.reshape(b, h, ns, GLA_SUB, dv)
        bs = bcum.reshape(b, h, ns, GLA_SUB, dk)
        diff = bs[:, :, :, :, None, :] - bs[:, :, :, None, :, :]
        diag = jnp.einsum('bhntk,bhnsk,bhntsk->bhnts', qs, ks, jnp.exp(jnp.where(tri[:, :, None], diff, -jnp.inf)))
        r = bs[:, :, :, -1]
        ex = bs[:, :, :, None] - r[:, :, None, :, None]
        qf = qs[:, :, :, None] * jnp.exp(jnp.where(strict_lower[:, :, None, None], ex, -jnp.inf))
        kf = ks * jnp.exp(r[:, :, :, None] - bs)
        a_off = jnp.einsum('bhijtk,bhjsk->bhijts', qf, kf)
        a = a_off + eye[:, :, None, None] * diag[:, :, :, None]
        o_intra = jnp.einsum('bhijts,bhjse->bhite', a, vs).reshape(b, h, GLA_CHUNK, dv)
        new_state = state * jnp.exp(b_last)[..., None] + jnp.einsum('bhcd,bhce->bhde', kc * jnp.exp(b_last[:, :, None] - bcum), vc)
        return new_state, o_inter + o_intra

    init = jnp.zeros((b, h, dk, dv), jnp.float32)
    _, o = lax.scan(step, init, (to_chunks(q), to_chunks(k), to_chunks(v), to_chunks(glog)))
    o = o.transpose(1, 0, 3, 2, 4).reshape(b, s, h, dv)
    o = o * lax.rsqrt(jnp.mean(jnp.square(o), axis=-1, keepdims=True) + EPS) * g_norm.astype(jnp.float32)
    o = o.reshape(b, s, GLA_V_WIDTH) * jax.nn.silu(out_gate.astype(jnp.float32))
    return o.astype(out_dtype)


def hierarchical_moe(h, w_rg, b_rg, w_re, b_re, w_gate, w_up, w_down):
    b, s, d = h.shape
    tok = h.reshape(b * s, d)
    gprob = jax.nn.softmax((tok @ w_rg).astype(jnp.float32) + b_rg.astype(jnp.float32), axis=-1)
    gsel = jnp.argmax(gprob, axis=-1)
    gw = jnp.take_along_axis(gprob, gsel[:, None], axis=-1)
    elogits = jnp.einsum('td,dge->tge', tok, w_re).astype(jnp.float32) + b_re.astype(jnp.float32)
    elog_sel = jnp.take_along_axis(elogits, gsel[:, None, None], axis=1)[:, 0]
    top_v, top_i = lax.top_k(jax.nn.softmax(elog_sel, axis=-1), TOPK_IN_GROUP)
    top_v = top_v / jnp.sum(top_v, axis=-1, keepdims=True)
    within = jnp.sum(jax.nn.one_hot(top_i, EXPERTS_PER_GROUP, dtype=jnp.float32) * top_v[..., None], axis=1)
    combine = jax.nn.one_hot(gsel, N_EXPERT_GROUPS, dtype=jnp.float32)[:, :, None] * within[:, None, :] * gw[:, :, None]
    out = jnp.zeros((b * s, d), jnp.float32)
    for gi in range(N_EXPERT_GROUPS):
        hid = jax.nn.silu(jnp.einsum('td,edf->tef', tok, w_gate[gi])) * jnp.einsum('td,edf->tef', tok, w_up[gi])
        hid = hid * combine[:, gi, :, None].astype(hid.dtype)
        out = out + jnp.einsum('tef,efd->td', hid, w_down[gi]).astype(jnp.float32)
    return out.reshape(b, s, d).astype(h.dtype)


def setup_inputs(seed: int = 0) -> dict:
    key = jax.random.key(seed)
    ks = jax.random.split(key, 24)
    L = DEPTH
    dk = NSA_HEAD_DIM
    nrm = lambda k, shape, scale: jax.random.normal(k, shape, jnp.float32) * scale
    return {
        "x": nrm(ks[0], (BATCH, SEQ, D_MODEL), 1.0),
        "g_mix_norm": 1.0 + nrm(ks[1], (L, D_MODEL), 0.02),
        "w_in": nrm(ks[2], (L, D_MODEL, IN_PROJ_WIDTH), D_MODEL ** -0.5),
        "b_nsa_gate": nrm(ks[3], (L, 3 * NSA_HEADS), 0.1),
        "cmp_pos_k": nrm(ks[4], (L, CMP_BLOCK, dk), 0.1),
        "w_cmp_k1": nrm(ks[5], (L, CMP_BLOCK * dk, CMP_HIDDEN), (CMP_BLOCK * dk) ** -0.5),
        "w_cmp_k2": nrm(ks[6], (L, CMP_HIDDEN, dk), CMP_HIDDEN ** -0.5),
        "cmp_pos_v": nrm(ks[7], (L, CMP_BLOCK, dk), 0.1),
        "w_cmp_v1": nrm(ks[8], (L, CMP_BLOCK * dk, CMP_HIDDEN), (CMP_BLOCK * dk) ** -0.5),
        "w_cmp_v2": nrm(ks[9], (L, CMP_HIDDEN, dk), CMP_HIDDEN ** -0.5),
        "w_alpha2": nrm(ks[10], (L, GATE_RANK, GLA_QK_WIDTH), GATE_RANK ** -0.5),
        "b_alpha": nrm(ks[11], (L, GLA_QK_WIDTH), 0.1),
        "g_gla_norm": 1.0 + nrm(ks[12], (L, GLA_VAL_DIM), 0.02),
        "w_out": nrm(ks[13], (L, MIX_WIDTH, D_MODEL), MIX_WIDTH ** -0.5),
        "g_ffn_norm": 1.0 + nrm(ks[14], (L, D_MODEL), 0.02),
        "w_router_group": nrm(ks[15], (L, D_MODEL, N_EXPERT_GROUPS), D_MODEL ** -0.5),
        "b_router_group": nrm(ks[16], (L, N_EXPERT_GROUPS), 0.01),
        "w_router_expert": nrm(ks[17], (L, D_MODEL, N_EXPERT_GROUPS, EXPERTS_PER_GROUP), D_MODEL ** -0.5),
        "b_router_expert": nrm(ks[18], (L, N_EXPERT_GROUPS, EXPERTS_PER_GROUP), 0.01),
        "w_expert_gate": nrm(ks[19], (L, N_EXPERT_GROUPS, EXPERTS_PER_GROUP, D_MODEL, D_EXPERT), D_MODEL ** -0.5),
        "w_expert_up": nrm(ks[20], (L, N_EXPERT_GROUPS, EXPERTS_PER_GROUP, D_MODEL, D_EXPERT), D_MODEL ** -0.5),
        "w_expert_down": nrm(ks[21], (L, N_EXPERT_GROUPS, EXPERTS_PER_GROUP, D_EXPERT, D_MODEL), D_EXPERT ** -0.5),
        "g_final_norm": 1.0 + nrm(ks[22], (D_MODEL,), 0.02),
    }


def reference(x, g_mix_norm, w_in, b_nsa_gate, cmp_pos_k, w_cmp_k1, w_cmp_k2, cmp_pos_v, w_cmp_v1, w_cmp_v2,
              w_alpha2, b_alpha, g_gla_norm, w_out, g_ffn_norm, w_router_group, b_router_group,
              w_router_expert, b_router_expert, w_expert_gate, w_expert_up, w_expert_down, g_final_norm):
    split_points = np.cumsum(IN_PROJ_SIZES)[:-1].tolist()
    for l in range(DEPTH):
        h = rms_norm(x, g_mix_norm[l])
        parts = jnp.split(h @ w_in[l], split_points, axis=-1)
        nsa_q, k_cmp, v_cmp, k_sel, v_sel, k_win, v_win, nsa_gate, gla_q, gla_k, gla_v, gla_alpha_lr, gla_out_gate = parts
        nsa_out = nsa_attention(nsa_q, k_cmp, v_cmp, k_sel, v_sel, k_win, v_win, nsa_gate + b_nsa_gate[l],
                                cmp_pos_k[l], w_cmp_k1[l], w_cmp_k2[l], cmp_pos_v[l], w_cmp_v1[l], w_cmp_v2[l])
        gla_out = gla_attention(gla_q, gla_k, gla_v, gla_alpha_lr, gla_out_gate, w_alpha2[l], b_alpha[l], g_gla_norm[l])
        x = x + jnp.concatenate([nsa_out, gla_out], axis=-1) @ w_out[l]
        x = x + hierarchical_moe(rms_norm(x, g_ffn_norm[l]), w_router_group[l], b_router_group[l],
                                 w_router_expert[l], b_router_expert[l], w_expert_gate[l], w_expert_up[l], w_expert_down[l])
    return rms_norm(x, g_final_norm)
```

```python
import numpy as np
import ml_dtypes
from contextlib import ExitStack
import concourse.bass as bass
import concourse.mybir as mybir
from concourse.bass_utils import run_bass_kernel_spmd

F32 = mybir.dt.float32
BF16 = mybir.dt.bfloat16
AF = mybir.ActivationFunctionType
ALU = mybir.AluOpType
AX = mybir.AxisListType

D = 2048
T = 2048
NT = 16
KC = 16
EPS = 1e-6
W_IN = 5672
SBUF_WORDS = 49152


class _Stream:
    def __init__(self, name, issuer, sems, is_dma):
        self.name = name
        self.issuer = issuer
        self.sems = sems
        self.is_dma = is_dma
        self.count = 0

    def target(self, c):
        if not self.is_dma:
            return (self.sems[0], c)
        k = len(self.sems)
        return (self.sems[(c - 1) % k], 16 * ((c - 1) // k + 1))


class Sched:
    ENGINES = ("pe", "dve", "act", "pool", "sp")

    def __init__(self, nc, ring=8):
        self.nc = nc
        self.ring = ring
        self.items = {e: [] for e in self.ENGINES}
        self.streams = {}
        self.waited = {e: {} for e in self.ENGINES}
        self.last_write = {}
        self.readers = {}
        self.n_ops = 0

    def setup(self, stack):
        nc = self.nc
        for e in self.ENGINES:
            s = stack.enter_context(nc.semaphore("s_" + e))
            self.streams[e] = _Stream(e, e, [s], False)
        for q, issuer in (("q_sp", "sp"), ("q_pool", "pool"), ("q_act", "act")):
            sems = [stack.enter_context(nc.semaphore("s_%s_%d" % (q, i))) for i in range(self.ring)]
            self.streams[q] = _Stream(q, issuer, sems, True)

    def _need(self, eng, dep, waits):
        if dep is None:
            return
        sname, c = dep
        st = self.streams[sname]
        if not st.is_dma:
            if sname == eng and eng == "pe":
                return
            if self.waited[eng].get(sname, 0) >= c:
                return
            waits[sname] = max(waits.get(sname, 0), c)
        else:
            w = self.waited[eng].setdefault(sname, set())
            if c in w:
                return
            waits.setdefault(sname, set()).add(c)

    def op(self, eng, fn, reads=(), writes=(), dma=None):
        waits = {}
        for k in reads:
            self._need(eng, self.last_write.get(k), waits)
        for k in writes:
            self._need(eng, self.last_write.get(k), waits)
            for rn, rc in list(self.readers.get(k, {}).items()):
                if rn == eng and dma is None and eng == "pe":
                    continue
                if isinstance(rc, set):
                    for cc in rc:
                        self._need(eng, (rn, cc), waits)
                else:
                    self._need(eng, (rn, rc), waits)
        sname = dma if dma is not None else eng
        st = self.streams[sname]
        assert st.issuer == eng
        st.count += 1
        c = st.count
        wl = []
        if st.is_dma and c > len(st.sems):
            sem, val = st.target(c)
            wl.append((sem, val - 16))
        for n, v in waits.items():
            s2 = self.streams[n]
            if s2.is_dma:
                for cc in sorted(v):
                    wl.append(s2.target(cc))
                    self.waited[eng][n].add(cc)
            else:
                wl.append(s2.target(v))
                self.waited[eng][n] = v
        me = (sname, c)
        for k in reads:
            if st.is_dma:
                self.readers.setdefault(k, {}).setdefault(sname, set()).add(c)
            else:
                self.readers.setdefault(k, {})[sname] = c
        for k in writes:
            self.last_write[k] = me
            self.readers[k] = {}
        sem, _ = st.target(c)
        self.items[eng].append((wl, fn, sem, 16 if st.is_dma else 1))
        self.n_ops += 1
        return me

    def barrier(self):
        for eng in self.ENGINES:
            wl = []
            for n, st in self.streams.items():
                if st.count == 0:
                    continue
                if st.is_dma:
                    w = self.waited[eng].setdefault(n, set())
                    for c in range(max(1, st.count - len(st.sems) + 1), st.count + 1):
                        if c not in w:
                            wl.append(st.target(c))
                            w.add(c)
                else:
                    if n == eng:
                        continue
                    if self.waited[eng].get(n, 0) < st.count:
                        wl.append(st.target(st.count))
                        self.waited[eng][n] = st.count
            if wl:
                self.items[eng].append((wl, None, None, 0))
        self.last_write = {}
        self.readers = {}

    def emit(self, block):
        def run(engname):
            def f(e):
                for wl, fn, sem, inc in self.items[engname]:
                    for (ws, wv) in wl:
                        e.wait_ge(ws, wv)
                    if fn is not None:
                        fn(e).then_inc(sem, inc)
            return f

        block.tensor(run("pe"))
        block.vector(run("dve"))
        block.scalar(run("act"))
        block.gpsimd(run("pool"))
        block.sync(run("sp"))


class Ctx:
    def __init__(self, nc, S, big, ps):
        self.nc = nc
        self.S = S
        self.big = big
        self.ps = ps
        self.base = 0
        self.off = 0
        self.rr = 0

    def persist(self, free, dt):
        ap = self.tile(free, dt)
        self.base = self.off
        return ap

    def reset(self):
        self.off = self.base

    def tile(self, free, dt):
        words = free if dt == F32 else (free + 1) // 2
        words = (words + 7) // 8 * 8
        a = self.big[:, self.off:self.off + words]
        self.off += words
        assert self.off <= SBUF_WORDS, "SBUF overflow %d" % self.off
        if dt == F32:
            a = a[:, 0:free]
        if dt != F32:
            a = a.bitcast(dt)
            if a.shape[1] != free:
                a = a[:, 0:free]
        return a

    def bank(self, b, n=512, dt=F32, off=0):
        if dt == F32:
            return self.ps[:, b * 512 + off:b * 512 + off + n]
        return self.ps[:, b * 512 + off:b * 512 + off + (n + 1) // 2].bitcast(dt)

    def op(self, eng, method, reads, writes, **kw):
        return self.S.op(eng, lambda e: getattr(e, method)(**kw), reads, writes)

    def dma(self, q, out, in_, reads, writes):
        eng = {"q_sp": "sp", "q_pool": "pool", "q_act": "act"}[q]
        return self.S.op(eng, lambda e: e.dma_start(out=out, in_=in_), reads, writes, dma=q)

    def evac(self, out, in_, reads, writes, scale=None):
        self.rr += 1
        if self.rr % 2 == 0:
            if scale is None:
                return self.op("act", "activation", reads, writes, out=out, in_=in_, func=AF.Copy)
            return self.op("act", "activation", reads, writes, out=out, in_=in_, func=AF.Copy, scale=scale)
        if scale is None:
            return self.op("dve", "tensor_copy", reads, writes, out=out, in_=in_)
        return self.op("dve", "tensor_scalar", reads, writes, out=out, in0=in_, scalar1=scale, scalar2=None,
                       op0=ALU.mult)


def v3(ap, a, b):
    return ap.rearrange("p (a b) -> p a b", a=a, b=b)


FM_PARTS = [("q", 0, 1024, 0), ("kcmp", 1024, 256, 1024), ("vcmp", 1280, 256, 1280), ("ksel", 1536, 256, 1536),
            ("kwin", 2048, 256, 1792), ("gq", 2584, 512, 2048), ("gk", 3096, 512, 2560)]
FM_ROWS = 3072
ALPHA_COL = 4632
TM_PARTS = [("vsel", 1792, 256, 0), ("vwin", 2304, 256, 256), ("gv0", 3608, 512, 512), ("gv1", 4120, 512, 1024),
            ("og0", 4648, 512, 1536), ("og1", 5160, 512, 2048)]
TM_COLS = 2560
GATE_COL = 2560


def phase_a(K, io):
    S = K.S
    K.reset()
    hT = K.tile(KC * T, BF16)
    hT3 = v3(hT, KC, T)
    mark = K.off
    xt = [K.tile(D, F32) for _ in range(2)]
    junk = K.tile(D, BF16)
    st = [K.tile(8, F32) for _ in range(2)]
    for ti in range(NT):
        b = ti % 2
        xk, sk = ("xt", b), ("st", b)
        K.dma("q_sp", xt[b], io["x"][ti * 128:(ti + 1) * 128, :], [], [xk])
        K.op("act", "activation", [xk], ["junk", sk], out=junk, in_=xt[b], func=AF.Square, accum_out=st[b][:, 0:1])
        K.op("dve", "tensor_scalar", [sk], [sk], out=st[b][:, 1:2], in0=st[b][:, 0:1], scalar1=1.0 / D, scalar2=EPS,
             op0=ALU.mult, op1=ALU.add)
        K.op("act", "activation", [sk], [sk], out=st[b][:, 2:3], in_=st[b][:, 1:2], func=AF.Sqrt)
        K.op("dve", "reciprocal", [sk], [sk], out=st[b][:, 3:4], in_=st[b][:, 2:3])
        K.op("dve", "tensor_scalar", [xk, sk], [xk], out=xt[b], in0=xt[b], scalar1=st[b][:, 3:4], scalar2=None,
             op0=ALU.mult)
        for q4 in range(4):
            bk = 4 * b + q4
            pk = ("ps", bk)
            for j in range(4):
                kc = q4 * 4 + j
                K.op("pe", "transpose", [xk, "identf"], [pk], out=K.bank(bk, 128, F32, j * 128),
                     in_=xt[b][:, kc * 128:(kc + 1) * 128], identity=io["identf"])
            for j in range(4):
                kc = q4 * 4 + j
                K.evac(hT3[:, kc, ti * 128:(ti + 1) * 128], K.bank(bk, 128, F32, j * 128), [pk, "gmix"], [("hT", ti)],
                       scale=io["gmix"][:, kc:kc + 1])
    S.barrier()
    K.off = mark
    w_in3 = io["w_in"].rearrange("(kc p) c -> p kc c", p=128)
    wfm = [K.tile(KC * 128, BF16) for _ in range(3)]
    ofm = [K.tile(T, BF16) for _ in range(2)]
    ofa = K.tile(T, F32)
    chunks = []
    for (nm, c0, n, r0) in FM_PARTS:
        for j in range(n // 128):
            chunks.append((c0 + j * 128, 128, r0 + j * 128, False))
    chunks.append((ALPHA_COL, 16, 0, True))
    hkeys = [("hT", ti) for ti in range(NT)]
    pb = 0
    for ci, (c0, n, r0, is_alpha) in enumerate(chunks):
        io["bg"].emit(2)
        wb = ci % 3
        wk = ("wfm", wb)
        w3 = v3(wfm[wb], KC, 128)
        K.dma("q_pool", w3[:, :, 0:n], w_in3[:, :, c0:c0 + n], [], [wk])
        ob = ci % 2
        ok = ("ofa",) if is_alpha else ("ofm", ob)
        for tb in range(4):
            bk = pb % 8
            pb += 1
            pk = ("ps", bk)
            for kc in range(KC):
                K.op("pe", "matmul", [wk] + hkeys[tb * 4:tb * 4 + 4], [pk], out=K.bank(bk)[0:n, :],
                     lhsT=w3[:, kc, 0:n], rhs=hT3[:, kc, tb * 512:(tb + 1) * 512], start=(kc == 0), stop=(kc == KC - 1))
            dst = ofa[0:n, tb * 512:(tb + 1) * 512] if is_alpha else ofm[ob][0:n, tb * 512:(tb + 1) * 512]
            K.evac(dst, K.bank(bk)[0:n, :], [pk], [ok])
        if is_alpha:
            K.dma("q_sp", io["s_alphaT"][:, :], ofa[0:16, :], [ok], [("s_alphaT",)])
        else:
            K.dma("q_sp", io["s_fm"][r0:r0 + n, :], ofm[ob][0:n, :], [ok], [("s_fm", r0)])
    wtm = [K.tile(KC * 512, BF16) for _ in range(2)]
    otm = [K.tile(NT * 512, BF16) for _ in range(2)]
    ogt = K.tile(NT * 24, F32)
    bg = K.tile(24, F32)
    K.dma("q_sp", bg, io["b_gate"].partition_broadcast(128), [], ["bg"])
    s_tm3 = io["s_tm"].rearrange("(ti p) c -> p ti c", p=128)
    groups = [(c0, n, t0, False) for (nm, c0, n, t0) in TM_PARTS] + [(GATE_COL, 24, 0, True)]
    for gi, (c0, n, t0, is_gate) in enumerate(groups):
        wb = gi % 2
        wk = ("wtm", wb)
        w3 = v3(wtm[wb], KC, 512)
        K.dma("q_pool", w3[:, :, 0:n], w_in3[:, :, c0:c0 + n], [], [wk])
        ok = ("ogt",) if is_gate else ("otm", wb)
        o3 = v3(ogt, NT, 24) if is_gate else v3(otm[wb], NT, 512)
        for ti in range(NT):
            if ti % 4 == 0:
                io["bg"].emit(1)
            bk = pb % 8
            pb += 1
            pk = ("ps", bk)
            for kc in range(KC):
                K.op("pe", "matmul", [wk, ("hT", ti)], [pk], out=K.bank(bk)[:, 0:n],
                     lhsT=hT3[:, kc, ti * 128:(ti + 1) * 128], rhs=w3[:, kc, 0:n], start=(kc == 0), stop=(kc == KC - 1))
            if is_gate:
                K.op("dve", "tensor_tensor", [pk, "bg"], [ok], out=o3[:, ti, :], in0=K.bank(bk)[:, 0:n], in1=bg,
                     op=ALU.add)
            else:
                K.evac(o3[:, ti, 0:n], K.bank(bk)[:, 0:n], [pk], [ok])
        if is_gate:
            K.dma("q_sp", io["s_gate"].rearrange("(ti p) c -> p ti c", p=128), o3, [ok], [("s_gate",)])
        else:
            K.dma("q_sp", s_tm3[:, :, t0:t0 + n], o3[:, :, 0:n], [ok], [("s_tm", t0)])
    S.barrier()


SCALE = 128.0 ** -0.5
SLOPES = [2.0 ** (-(h + 1)) for h in range(8)]
BIG = 1.0e30
BIGD = 30000.0
MIX_ROWS = 2048


def phase_b(K, io):
    S = K.S
    K.reset()
    kT = K.tile(4 * T, BF16)
    kT3 = v3(kT, 4, T)
    w1 = [K.tile(32 * 256, BF16) for _ in range(2)]
    w2 = [K.tile(2 * 128, BF16) for _ in range(2)]
    pos = [K.tile(32, F32) for _ in range(2)]
    kp = [K.tile(32 * 127, BF16) for _ in range(2)]
    gel = [K.tile(2 * 127, BF16) for _ in range(2)]
    tx2 = K.tile(127, F32)
    tu = K.tile(127, F32)
    K.dma("q_sp", kT3, io["s_fm"][1024:1536, :].rearrange("(a p) t -> p a t", p=128), [], ["kT"])
    for kv, (n1, n2, npz) in enumerate((("w_cmp_k1", "w_cmp_k2", "c_posk"), ("w_cmp_v1", "w_cmp_v2", "c_posv"))):
        K.dma("q_pool", v3(w1[kv], 32, 256), io[n1].rearrange("(l d) j -> d l j", d=128), [], [("w1", kv)])
        K.dma("q_pool", v3(w2[kv], 2, 128), io[n2].rearrange("(jc j) d -> j jc d", j=128), [], [("w2", kv)])
        K.dma("q_sp", pos[kv], io[npz][:, :], [], [("pos", kv)])
    cnt = 0
    for kv in range(2):
        io["bg"].emit(10)
        w13 = v3(w1[kv], 32, 256)
        w23 = v3(w2[kv], 2, 128)
        for g in range(2):
            pb_ = cnt % 2
            cnt += 1
            kp3 = v3(kp[pb_], 32, 127)
            gl3 = v3(gel[pb_], 2, 127)
            for l in range(32):
                eng = "dve" if l % 2 == 0 else "pool"
                K.op(eng, "tensor_scalar", ["kT", ("pos", kv)], [("kp", pb_)], out=kp3[:, l, :],
                     in0=kT3[:, kv * 2 + g, l:l + 2017:16], scalar1=pos[kv][:, l:l + 1], scalar2=None, op0=ALU.add)
            for jc in range(2):
                bk = jc
                pk = ("ps", bk)
                x = K.bank(bk)[:, 0:127]
                for l in range(32):
                    K.op("pe", "matmul", [("w1", kv), ("kp", pb_)], [pk], out=x, lhsT=w13[:, l, jc * 128:(jc + 1) * 128],
                         rhs=kp3[:, l, :], start=(l == 0), stop=(l == 31))
                K.op("act", "activation", [pk], ["tx2"], out=tx2, in_=x, func=AF.Square)
                K.op("dve", "tensor_scalar", ["tx2"], ["tu"], out=tu, in0=tx2, scalar1=0.044715, scalar2=1.0,
                     op0=ALU.mult, op1=ALU.add)
                K.op("dve", "tensor_tensor", ["tu", pk], ["tu"], out=tu, in0=tu, in1=x, op=ALU.mult)
                K.op("act", "activation", ["tu"], ["tx2"], out=tx2, in_=tu, func=AF.Tanh, scale=0.7978845608028654)
                K.op("dve", "tensor_scalar", ["tx2"], ["tu"], out=tu, in0=tx2, scalar1=1.0, scalar2=0.5,
                     op0=ALU.add, op1=ALU.mult)
                K.op("dve", "tensor_tensor", ["tu", pk], [("gel", pb_)], out=gl3[:, jc, :], in0=tu, in1=x, op=ALU.mult)
            bk = 2 + (cnt % 2)
            pk = ("ps", bk)
            if kv == 0:
                o = K.bank(bk)[:, 0:127]
                for jc in range(2):
                    K.op("pe", "matmul", [("w2", kv), ("gel", pb_)], [pk], out=o, lhsT=w23[:, jc, :], rhs=gl3[:, jc, :],
                         start=(jc == 0), stop=(jc == 1))
                K.evac(io["kcT"][:, g * 128:g * 128 + 127], o, [pk], ["kcT"])
            else:
                o = K.bank(bk)[0:127, 0:128]
                for jc in range(2):
                    K.op("pe", "matmul", [("w2", kv), ("gel", pb_)], [pk], out=o, lhsT=gl3[:, jc, :], rhs=w23[:, jc, :],
                         start=(jc == 0), stop=(jc == 1))
                K.evac(io["vc"][0:127, g * 128:(g + 1) * 128], o, [pk], ["vc"])
    S.barrier()


def phase_c(K, io):
    S = K.S
    K.reset()
    qTs = [v3(K.tile(8 * 128, BF16), 8, 128) for _ in range(2)]
    kselT = v3(K.tile(2 * T, BF16), 2, T)
    kwinT = v3(K.tile(2 * T, BF16), 2, T)
    vsel = v3(K.tile(NT * 256, BF16), NT, 256)
    vwin = v3(K.tile(NT * 256, BF16), NT, 256)
    gsig = v3(K.tile(NT * 24, F32), NT, 24)
    Dsel = K.tile(2048, F32)
    Dwin = K.tile(640, F32)
    Dcmp = v3(K.tile(NT * 127, F32), NT, 127)
    rowvalid = K.tile(NT, F32)
    mulmask = v3(K.tile(NT * 32, F32), NT, 32)
    addmask = v3(K.tile(NT * 32, F32), NT, 32)
    overlap = K.tile(32, F32)
    selbias = K.tile(2 * 32, F32)
    s_sel = [K.tile(2048, F32) for _ in range(4)]
    p_sel = [K.tile(2048, BF16) for _ in range(4)]
    pt_sel = [K.tile(2048, BF16) for _ in range(4)]
    s_win = [K.tile(640, F32) for _ in range(4)]
    p_win = [K.tile(640, BF16) for _ in range(4)]
    pt_win = [K.tile(640, BF16) for _ in range(4)]
    stat = [K.tile(8, F32) for _ in range(8)]
    sc = [K.tile(127, F32) for _ in range(4)]
    pc = [K.tile(127, F32) for _ in range(4)]
    pg = [K.tile(128, BF16) for _ in range(4)]
    pnT = [K.tile(128, F32) for _ in range(4)]
    pcT = [K.tile(128, BF16) for _ in range(4)]
    cst = [K.tile(8, F32) for _ in range(4)]
    osb = [K.tile(512, BF16) for _ in range(2)]
    imp2 = K.tile(32, F32)
    mx8 = K.tile(8, F32)
    identf, identb = io["identf"], io["identb"]
    kcT, vc = io["kcT"], io["vc"]
    R4 = range(4)

    s_q3 = io["s_fm"][0:1024, :].rearrange("(h p) t -> p h t", p=128)
    K.dma("q_sp", kselT, io["s_fm"][1536:1792, :].rearrange("(g p) t -> p g t", p=128), [], ["kselT"])
    K.dma("q_sp", kwinT, io["s_fm"][1792:2048, :].rearrange("(g p) t -> p g t", p=128), [], ["kwinT"])
    s_tm3 = io["s_tm"].rearrange("(kt p) c -> p kt c", p=128)
    K.dma("q_sp", vsel, s_tm3[:, :, 0:256], [], ["vsel"])
    K.dma("q_sp", vwin, s_tm3[:, :, 256:512], [], ["vwin"])
    K.dma("q_sp", gsig, io["s_gate"].rearrange("(ti p) c -> p ti c", p=128), [], ["gsig"])
    K.dma("q_sp", Dsel, io["c_dsel"][:, :], [], ["Dsel"])
    K.dma("q_sp", Dwin, io["c_dwin"][:, :], [], ["Dwin"])
    K.dma("q_sp", Dcmp, io["c_dcmp"].rearrange("p (a b) -> p a b", b=127), [], ["Dcmp"])
    K.dma("q_sp", rowvalid, io["c_rowvalid"][:, :], [], ["cm0"])
    K.dma("q_sp", mulmask, io["c_mulmask"].rearrange("p (a b) -> p a b", b=32), [], ["cm1"])
    K.dma("q_sp", addmask, io["c_addmask"].rearrange("p (a b) -> p a b", b=32), [], ["cm2"])
    K.dma("q_sp", overlap[0:127, :], io["c_overlap"][:, :], [], ["cm3"])
    K.op("act", "activation", ["gsig"], ["gsig"], out=gsig, in_=gsig, func=AF.Sigmoid)
    trr = [0]

    def transposes(p_ap, pt_ap, nkt, pk_, ptk):
        for k0 in range(0, nkt, 8):
            n = min(8, nkt - k0)
            bk = trr[0] % 4
            trr[0] += 1
            for j in range(n):
                K.op("pe", "transpose", [pk_, "identb"], [("ps", bk)], out=K.bank(bk, 128, BF16, j * 64),
                     in_=p_ap[:, (k0 + j) * 128:(k0 + j + 1) * 128], identity=identb)
            K.op("act", "activation", [("ps", bk)], [ptk], out=pt_ap[:, k0 * 128:(k0 + n) * 128],
                 in_=K.bank(bk, n * 128, BF16, 0), func=AF.Copy)

    it = 0
    K.dma("q_sp", qTs[0], s_q3[:, :, 0:128], [], [("qT", 0)])
    for qt in range(NT):
        qs = slice(qt * 128, (qt + 1) * 128)
        qT = qTs[qt % 2]
        qk_ = ("qT", qt % 2)
        ql = slice(0, 128)
        if qt + 1 < NT:
            K.dma("q_sp", qTs[(qt + 1) % 2], s_q3[:, :, (qt + 1) * 128:(qt + 2) * 128], [], [("qT", (qt + 1) % 2)])
        io["bg"].emit(13)
        nk = (qt + 1) * 128
        nc_s = (nk + 511) // 512
        off = 1920 - qt * 128
        k0w = max(0, qt * 128 - 512)
        nkw = qt * 128 + 128 - k0w
        nc_w = (nkw + 511) // 512
        coff = k0w - (qt * 128 - 512)
        nb = 2 * (qt + 1)
        for g in range(2):
            H = [g * 4 + hh for hh in R4]
            coefs = [-SLOPES[h] / SCALE for h in H]
            sb = selbias[:, g * 32:(g + 1) * 32]
            for hh in R4:
                K.op("pe", "matmul", [qk_, "kcT"], [("ps", hh)], out=K.bank(hh)[:, 0:127], lhsT=qT[:, H[hh], ql],
                     rhs=kcT[:, g * 128:g * 128 + 127], start=True, stop=True)
            for hh in R4:
                K.op("dve", "scalar_tensor_tensor", ["Dcmp", ("ps", hh)], [("sc", hh)], out=sc[hh], in0=Dcmp[:, qt, :],
                     scalar=coefs[hh], in1=K.bank(hh)[:, 0:127], op0=ALU.mult, op1=ALU.add)
            for hh in R4:
                K.op("dve", "tensor_reduce", [("sc", hh)], [("cst", hh)], out=cst[hh][:, 0:1], in_=sc[hh], axis=AX.X,
                     op=ALU.max)
            for hh in R4:
                K.op("dve", "tensor_scalar", [("cst", hh)], [("cst", hh)], out=cst[hh][:, 1:2], in0=cst[hh][:, 0:1],
                     scalar1=-SCALE, scalar2=None, op0=ALU.mult)
            for hh in R4:
                K.op("act", "activation", [("sc", hh), ("cst", hh)], [("pc", hh), ("cst", hh)], out=pc[hh], in_=sc[hh],
                     func=AF.Exp, scale=SCALE, bias=cst[hh][:, 1:2], accum_out=cst[hh][:, 2:3])
            for hh in R4:
                K.op("dve", "reciprocal", [("cst", hh)], [("cst", hh)], out=cst[hh][:, 3:4], in_=cst[hh][:, 2:3])
            for hh in R4:
                K.op("dve", "tensor_scalar", [("cst", hh), "cm0"], [("cst", hh)], out=cst[hh][:, 4:5],
                     in0=cst[hh][:, 3:4], scalar1=rowvalid[:, qt:qt + 1], scalar2=None, op0=ALU.mult)
            for hh in R4:
                K.op("dve", "tensor_scalar", [("pc", hh), ("cst", hh)], [("pc", hh)], out=pc[hh], in0=pc[hh],
                     scalar1=cst[hh][:, 4:5], scalar2=None, op0=ALU.mult)
            for hh in R4:
                K.op("dve", "tensor_scalar", [("pc", hh), "gsig"], [("pg", hh)], out=pg[hh][:, 0:127], in0=pc[hh],
                     scalar1=gsig[:, qt, H[hh] * 3:H[hh] * 3 + 1], scalar2=None, op0=ALU.mult)
            for hh in R4:
                bk = 4 + hh % 2
                o0 = (hh // 2) * 192
                K.op("pe", "transpose", [("pc", hh), "identf"], [("ps", bk)], out=K.bank(bk, 128, F32, o0)[0:127, :],
                     in_=pc[hh], identity=identf)
                K.op("pe", "transpose", [("pg", hh), "identb"], [("ps", bk)],
                     out=K.bank(bk, 128, BF16, o0 + 128)[0:127, :], in_=pg[hh][:, 0:127], identity=identb)
            for hh in R4:
                bk = 4 + hh % 2
                o0 = (hh // 2) * 192
                K.op("act", "activation", [("ps", bk)], [("pnT", hh)], out=pnT[hh][0:127, :],
                     in_=K.bank(bk, 128, F32, o0)[0:127, :], func=AF.Copy)
                K.op("dve", "tensor_copy", [("ps", bk)], [("pcT", hh)], out=pcT[hh][0:127, :],
                     in_=K.bank(bk, 128, BF16, o0 + 128)[0:127, :])
            for hh in R4:
                K.op("pe", "matmul", [("pnT", hh), "cm3"], [("ps", 6)], out=K.bank(6)[:, 0:32], lhsT=pnT[hh][0:127, :],
                     rhs=overlap[0:127, :], start=(hh == 0), stop=(hh == 3))
            K.op("dve", "tensor_tensor", [("ps", 6), "cm1"], ["imp2"], out=imp2, in0=K.bank(6)[:, 0:32],
                 in1=mulmask[:, qt, :], op=ALU.mult)
            K.op("dve", "tensor_tensor", ["imp2", "cm2"], ["imp2"], out=imp2, in0=imp2, in1=addmask[:, qt, :], op=ALU.add)
            K.op("dve", "max", ["imp2"], ["mx8"], out=mx8, in_=imp2)
            K.op("dve", "tensor_scalar", ["imp2", "mx8"], ["imp2"], out=imp2, in0=imp2, scalar1=mx8[:, 7:8], scalar2=None,
                 op0=ALU.is_ge)
            K.op("dve", "tensor_scalar", ["imp2"], [("selbias", g)], out=sb, in0=imp2, scalar1=-1.0, scalar2=BIG,
                 op0=ALU.add, op1=ALU.mult)

            def qk_sel(hh):
                for c in range(nc_s):
                    w = min(512, nk - c * 512)
                    bk = (hh % 2) * 4 + c
                    K.op("pe", "matmul", [qk_, "kselT"], [("ps", bk)], out=K.bank(bk)[:, 0:w], lhsT=qT[:, H[hh], ql],
                         rhs=kselT[:, g, c * 512:c * 512 + w], start=True, stop=True)

            def p1_sel(hh):
                for c in range(nc_s):
                    w = min(512, nk - c * 512)
                    bk = (hh % 2) * 4 + c
                    K.op("dve", "scalar_tensor_tensor", ["Dsel", ("ps", bk)], [("s_sel", hh)],
                         out=s_sel[hh][:, c * 512:c * 512 + w], in0=Dsel[:, off + c * 512:off + c * 512 + w],
                         scalar=coefs[hh], in1=K.bank(bk)[:, 0:w], op0=ALU.mult, op1=ALU.add)

            def qk_win(hh):
                for c in range(nc_w):
                    w = min(512, nkw - c * 512)
                    bk = hh * 2 + c
                    K.op("pe", "matmul", [qk_, "kwinT"], [("ps", bk)], out=K.bank(bk)[:, 0:w], lhsT=qT[:, H[hh], ql],
                         rhs=kwinT[:, g, k0w + c * 512:k0w + c * 512 + w], start=True, stop=True)

            def p1_win(hh):
                for c in range(nc_w):
                    w = min(512, nkw - c * 512)
                    bk = hh * 2 + c
                    K.op("dve", "scalar_tensor_tensor", ["Dwin", ("ps", bk)], [("s_win", hh)],
                         out=s_win[hh][:, c * 512:c * 512 + w], in0=Dwin[:, coff + c * 512:coff + c * 512 + w],
                         scalar=coefs[hh], in1=K.bank(bk)[:, 0:w], op0=ALU.mult, op1=ALU.add)

            qk_sel(0)
            qk_sel(1)
            p1_sel(0)
            qk_sel(2)
            p1_sel(1)
            qk_sel(3)
            p1_sel(2)
            p1_sel(3)
            for hh in R4:
                ss = s_sel[hh][:, 0:nk]
                K.op("dve", "tensor_tensor", [("s_sel", hh), ("selbias", g)], [("s_sel", hh)], out=v3(ss, nb, 64),
                     in0=v3(ss, nb, 64), in1=sb[:, 0:nb].unsqueeze(2).broadcast_to([128, nb, 64]), op=ALU.add)
            for hh in R4:
                K.op("dve", "tensor_reduce", [("s_sel", hh)], [("stat", hh)], out=stat[hh][:, 0:1],
                     in_=s_sel[hh][:, 0:nk], axis=AX.X, op=ALU.max)
            for hh in R4:
                K.op("dve", "tensor_scalar", [("stat", hh)], [("stat", hh)], out=stat[hh][:, 1:2], in0=stat[hh][:, 0:1],
                     scalar1=-SCALE, scalar2=None, op0=ALU.mult)
            for hh in R4:
                K.op("act", "activation", [("s_sel", hh), ("stat", hh)], [("p_sel", hh), ("stat", hh)],
                     out=p_sel[hh][:, 0:nk], in_=s_sel[hh][:, 0:nk], func=AF.Exp, scale=SCALE, bias=stat[hh][:, 1:2],
                     accum_out=stat[hh][:, 2:3])
            for hh in R4:
                qk_win(hh)
            for hh in R4:
                p1_win(hh)
            for hh in R4:
                K.op("dve", "tensor_reduce", [("s_win", hh)], [("stat", 4 + hh)], out=stat[4 + hh][:, 0:1],
                     in_=s_win[hh][:, 0:nkw], axis=AX.X, op=ALU.max)
            for hh in R4:
                K.op("dve", "tensor_scalar", [("stat", 4 + hh)], [("stat", 4 + hh)], out=stat[4 + hh][:, 1:2],
                     in0=stat[4 + hh][:, 0:1], scalar1=-SCALE, scalar2=None, op0=ALU.mult)
            for hh in R4:
                K.op("act", "activation", [("s_win", hh), ("stat", 4 + hh)], [("p_win", hh), ("stat", 4 + hh)],
                     out=p_win[hh][:, 0:nkw], in_=s_win[hh][:, 0:nkw], func=AF.Exp, scale=SCALE,
                     bias=stat[4 + hh][:, 1:2], accum_out=stat[4 + hh][:, 2:3])
            for (base, pbuf, pname, n_, gi) in ((0, p_sel, "p_sel", nk, 1), (4, p_win, "p_win", nkw, 2)):
                for hh in R4:
                    K.op("dve", "reciprocal", [("stat", base + hh)], [("stat", base + hh)], out=stat[base + hh][:, 3:4],
                         in_=stat[base + hh][:, 2:3])
                for hh in R4:
                    K.op("dve", "tensor_scalar", [("stat", base + hh), "gsig"], [("stat", base + hh)],
                         out=stat[base + hh][:, 4:5], in0=stat[base + hh][:, 3:4],
                         scalar1=gsig[:, qt, H[hh] * 3 + gi:H[hh] * 3 + gi + 1], scalar2=None, op0=ALU.mult)
                for hh in R4:
                    K.op("dve", "tensor_scalar", [(pname, hh), ("stat", base + hh)], [(pname, hh)],
                         out=pbuf[hh][:, 0:n_], in0=pbuf[hh][:, 0:n_], scalar1=stat[base + hh][:, 4:5], scalar2=None,
                         op0=ALU.mult)
            for hh in R4:
                transposes(p_sel[hh], pt_sel[hh], qt + 1, ("p_sel", hh), ("pt_sel", hh))
            for hh in R4:
                transposes(p_win[hh], pt_win[hh], nkw // 128, ("p_win", hh), ("pt_win", hh))
            ob = it % 2
            it += 1
            for hh in R4:
                bk = 4 + hh
                o = K.bank(bk)[:, 0:128]
                nmm = 1 + (qt + 1) + nkw // 128
                i = 1
                K.op("pe", "matmul", ["vc", ("pcT", hh)], [("ps", bk)], out=o, lhsT=vc[0:127, g * 128:(g + 1) * 128],
                     rhs=pcT[hh][0:127, :], start=True, stop=False)
                for kt in range(qt + 1):
                    i += 1
                    K.op("pe", "matmul", ["vsel", ("pt_sel", hh)], [("ps", bk)], out=o,
                         lhsT=vsel[:, kt, g * 128:(g + 1) * 128], rhs=pt_sel[hh][:, kt * 128:(kt + 1) * 128],
                         start=False, stop=False)
                for j in range(nkw // 128):
                    i += 1
                    K.op("pe", "matmul", ["vwin", ("pt_win", hh)], [("ps", bk)], out=o,
                         lhsT=vwin[:, k0w // 128 + j, g * 128:(g + 1) * 128], rhs=pt_win[hh][:, j * 128:(j + 1) * 128],
                         start=False, stop=(i == nmm))
                K.evac(osb[ob][:, hh * 128:(hh + 1) * 128], o, [("ps", bk)], [("osb", ob)])
            K.dma("q_sp", io["s_mixT"][g * 512:(g + 1) * 512, qs].rearrange("(h p) t -> p h t", p=128),
                  v3(osb[ob], 4, 128), [("osb", ob)], [("s_mixT", qt, g)])
    S.barrier()


def phase_d(K, io):
    S = K.S
    K.reset()
    gqk = [v3(K.tile(8 * 128, BF16), 8, 128) for _ in range(2)]
    gv = v3(K.tile(NT * 1024, BF16), NT, 1024)
    og = v3(K.tile(NT * 1024, BF16), NT, 1024)
    glaT = v3(K.tile(8 * T, BF16), 8, T)
    alphaT = K.tile(T, F32)
    wa2 = K.tile(512, F32)
    ba = K.tile(512, F32)
    ones = K.tile(128, F32)
    tris = K.tile(128, F32)
    mask01 = K.tile(128, F32)
    gnb = K.tile(256, F32)
    st = v3(K.tile(4 * 256, F32), 4, 256)
    stb = v3(K.tile(4 * 256, BF16), 4, 256)
    ex2 = [K.tile(512, F32) for _ in range(2)]
    lg2 = [K.tile(512, F32) for _ in range(2)]
    eb2 = [K.tile(512, F32) for _ in range(2)]
    enb2 = [K.tile(512, F32) for _ in range(2)]
    QeT2 = [K.tile(512, BF16) for _ in range(2)]
    KeT2 = [K.tile(512, BF16) for _ in range(2)]
    ATm2 = [K.tile(512, BF16) for _ in range(2)]
    Ketm2 = [K.tile(512, BF16) for _ in range(2)]
    t12 = [K.tile(1024, F32) for _ in range(2)]
    sog2 = [K.tile(1024, F32) for _ in range(2)]
    gtm2 = [K.tile(1024, BF16) for _ in range(2)]
    junk = K.tile(256, BF16)
    rs2 = [K.tile(16, F32) for _ in range(2)]
    s1 = K.tile(256, F32)
    identb = io["identb"]

    s_gqk3 = io["s_fm"][2048:3072, :].rearrange("(h p) t -> p h t", p=128)
    K.dma("q_sp", gqk[0], s_gqk3[:, :, 0:128], [], [("gqk", 0)])
    s_tm3 = io["s_tm"].rearrange("(kt p) c -> p kt c", p=128)
    K.dma("q_sp", gv, s_tm3[:, :, 512:1536], [], ["gv"])
    K.dma("q_sp", og, s_tm3[:, :, 1536:2560], [], ["og"])
    K.dma("q_sp", alphaT[0:16, :], io["s_alphaT"][:, :], [], ["alphaT"])
    K.dma("q_sp", wa2[0:16, :], io["w_alpha2"][:, :], [], ["wa2"])
    K.dma("q_sp", ba[0:1, :], io["b_alpha"].rearrange("(a n) -> a n", a=1), [], ["ba"])
    K.dma("q_sp", tris, io["c_tris"][:, :], [], ["tris"])
    K.dma("q_sp", mask01, io["c_mask01"][:, :], [], ["mask01"])
    K.dma("q_sp", gnb, io["g_gla"].partition_broadcast(128), [], ["gnb"])
    K.op("dve", "memset", [], ["ones"], ap=ones, constant=1.0)
    K.op("dve", "memset", [], ["st"], ap=st, constant=0.0)
    K.op("pool", "memset", [], ["stb"], ap=stb, constant=0.0)

    def front(ti):
        ts_ = slice(ti * 128, (ti + 1) * 128)
        gqT = gqk[ti % 2][:, 0:4, :]
        gkT = gqk[ti % 2][:, 4:8, :]
        gkey = ("gqk", ti % 2)
        pq = ti % 2
        ex, lg, eb, enb, QeT, KeT, ATm, Ketm = ex2[pq], lg2[pq], eb2[pq], enb2[pq], QeT2[pq], KeT2[pq], ATm2[pq], Ketm2[pq]
        t1, sog, gtm, rs = t12[pq], sog2[pq], gtm2[pq], rs2[pq]
        if ti + 1 < NT:
            K.dma("q_sp", gqk[(ti + 1) % 2], s_gqk3[:, :, (ti + 1) * 128:(ti + 2) * 128], [], [("gqk", (ti + 1) % 2)])
        io["bg"].emit(3)
        K.op("pe", "matmul", ["alphaT", "wa2"], [("ps", 0)], out=K.bank(0), lhsT=alphaT[0:16, ts_], rhs=wa2[0:16, :],
             start=True, stop=False)
        K.op("pe", "matmul", ["ones", "ba"], [("ps", 0)], out=K.bank(0), lhsT=ones[0:1, :], rhs=ba[0:1, :],
             start=False, stop=True)
        K.op("act", "activation", [("ps", 0)], [("ex", pq)], out=ex, in_=K.bank(0), func=AF.Exp, scale=-1.0)
        K.op("act", "activation", [("ex", pq)], [("lg", pq)], out=lg, in_=ex, func=AF.Ln, bias=1.0)
        for h in range(4):
            K.op("pe", "matmul", [("lg", pq), "tris"], [("ps", 1)], out=K.bank(1)[:, h * 128:(h + 1) * 128],
                 lhsT=lg[:, h * 128:(h + 1) * 128], rhs=tris, start=True, stop=True)
        K.op("act", "activation", [("ps", 1)], [("eb", pq)], out=eb, in_=K.bank(1), func=AF.Exp)
        K.op("act", "activation", [("ps", 1)], [("enb", pq)], out=enb, in_=K.bank(1), func=AF.Exp, scale=-1.0)
        K.op("dve", "scalar_tensor_tensor", [("eb", pq), gkey], [("QeT", pq)], out=v3(QeT, 4, 128), in0=v3(eb, 4, 128), scalar=SCALE,
             in1=gqT, op0=ALU.mult, op1=ALU.mult)
        K.op("dve", "tensor_tensor", [("enb", pq), gkey], [("KeT", pq)], out=v3(KeT, 4, 128), in0=v3(enb, 4, 128), in1=gkT,
             op=ALU.mult)
        for h in range(4):
            K.op("pe", "matmul", [("KeT", pq), ("QeT", pq)], [("ps", 2)], out=K.bank(2)[:, h * 128:(h + 1) * 128],
                 lhsT=KeT[:, h * 128:(h + 1) * 128], rhs=QeT[:, h * 128:(h + 1) * 128], start=True, stop=True)
        K.op("dve", "tensor_tensor", [("ps", 2), "mask01"], [("ATm", pq)], out=v3(ATm, 4, 128), in0=v3(K.bank(2), 4, 128),
             in1=mask01.unsqueeze(1).broadcast_to([128, 4, 128]), op=ALU.mult)
        for h in range(4):
            K.op("pe", "transpose", [("KeT", pq), "identb"], [("ps", 5)], out=K.bank(5, 128, BF16, h * 64),
                 in_=KeT[:, h * 128:(h + 1) * 128], identity=identb)
        K.op("act", "activation", [("ps", 5)], [("Ketm", pq)], out=Ketm, in_=K.bank(5, 512, BF16, 0), func=AF.Copy)


    def mid(ti):
        ts_ = slice(ti * 128, (ti + 1) * 128)
        pq = ti % 2
        ex, lg, eb, enb, QeT, KeT, ATm, Ketm = ex2[pq], lg2[pq], eb2[pq], enb2[pq], QeT2[pq], KeT2[pq], ATm2[pq], Ketm2[pq]
        t1, sog, gtm, rs = t12[pq], sog2[pq], gtm2[pq], rs2[pq]
        for h in range(4):
            bk = 3 + h // 2
            o = K.bank(bk)[:, (h % 2) * 256:(h % 2) * 256 + 256]
            K.op("pe", "matmul", [("ATm", pq), "gv"], [("ps", bk)], out=o, lhsT=ATm[:, h * 128:(h + 1) * 128],
                 rhs=gv[:, ti, h * 256:(h + 1) * 256], start=True, stop=False)
            K.op("pe", "matmul", [("QeT", pq), "stb"], [("ps", bk)], out=o, lhsT=QeT[:, h * 128:(h + 1) * 128],
                 rhs=stb[:, h, :], start=False, stop=True)
        for h in range(4):
            bk = 6 + h // 2
            o = K.bank(bk)[:, (h % 2) * 256:(h % 2) * 256 + 256]
            K.op("pe", "matmul", [("Ketm", pq), "gv"], [("ps", bk)], out=o, lhsT=Ketm[:, h * 128:(h + 1) * 128],
                 rhs=gv[:, ti, h * 256:(h + 1) * 256], start=True, stop=True)
            ebl = eb[:, h * 128 + 127:h * 128 + 128]
            K.op("dve", "tensor_scalar", ["st", ("eb", pq)], ["s1"], out=s1, in0=st[:, h, :], scalar1=ebl, scalar2=None,
                 op0=ALU.mult)
            K.op("dve", "scalar_tensor_tensor", [("ps", bk), ("eb", pq), "s1"], ["st"], out=st[:, h, :], in0=o, scalar=ebl,
                 in1=s1, op0=ALU.mult, op1=ALU.add)
        K.op("act", "activation", ["st"], ["stb"], out=stb, in_=st, func=AF.Copy)


    def outp(ti):
        ts_ = slice(ti * 128, (ti + 1) * 128)
        pq = ti % 2
        ex, lg, eb, enb, QeT, KeT, ATm, Ketm = ex2[pq], lg2[pq], eb2[pq], enb2[pq], QeT2[pq], KeT2[pq], ATm2[pq], Ketm2[pq]
        t1, sog, gtm, rs = t12[pq], sog2[pq], gtm2[pq], rs2[pq]
        for h in range(4):
            bk = 3 + h // 2
            o = K.bank(bk)[:, (h % 2) * 256:(h % 2) * 256 + 256]
            K.op("act", "activation", [("ps", bk)], ["junk", ("rs", pq)], out=junk, in_=o, func=AF.Square,
                 accum_out=rs[:, h:h + 1])
        K.op("dve", "tensor_scalar", [("rs", pq)], [("rs", pq)], out=rs[:, 4:8], in0=rs[:, 0:4], scalar1=1.0 / 256, scalar2=EPS,
             op0=ALU.mult, op1=ALU.add)
        K.op("act", "activation", [("rs", pq)], [("rs", pq)], out=rs[:, 8:12], in_=rs[:, 4:8], func=AF.Sqrt)
        K.op("dve", "reciprocal", [("rs", pq)], [("rs", pq)], out=rs[:, 12:16], in_=rs[:, 8:12])
        K.op("act", "activation", ["og"], [("sog", pq)], out=sog, in_=og[:, ti, :], func=AF.Silu)
        for h in range(4):
            bk = 3 + h // 2
            o = K.bank(bk)[:, (h % 2) * 256:(h % 2) * 256 + 256]
            K.op("dve", "scalar_tensor_tensor", [("ps", bk), ("rs", pq), "gnb"], [("t1", pq)], out=t1[:, h * 256:(h + 1) * 256], in0=o,
                 scalar=rs[:, 12 + h:13 + h], in1=gnb, op0=ALU.mult, op1=ALU.mult)
        K.op("dve", "tensor_tensor", [("t1", pq), ("sog", pq)], [("gtm", pq)], out=gtm, in0=t1, in1=sog, op=ALU.mult)
        for fc in range(8):
            K.op("pe", "transpose", [("gtm", pq), "identb"], [("ps", 5)], out=K.bank(5, 128, BF16, fc * 64),
                 in_=gtm[:, fc * 128:(fc + 1) * 128], identity=identb)
        K.evac(glaT[:, :, ts_], v3(K.bank(5, 1024, BF16, 0), 8, 128), [("ps", 5)], [("glaT", ti)])

    front(0)
    for ti in range(NT):
        mid(ti)
        if ti + 1 < NT:
            front(ti + 1)
        outp(ti)
    gk = [("glaT", ti) for ti in range(NT)]
    for fc in range(8):
        K.dma("q_sp", io["s_mixT"][1024 + fc * 128:1024 + (fc + 1) * 128, :], glaT[:, fc, :], gk, [("s_mixT", 8 + fc)])
    S.barrier()


def phase_e(K, io):
    S = K.S
    K.reset()
    mixs = [v3(K.tile(KC * 128, BF16), KC, 128) for _ in range(2)]
    wout = v3(K.tile(KC * D, BF16), KC, D)
    xt = [K.tile(D, F32) for _ in range(2)]
    xn = [K.tile(D, F32) for _ in range(2)]
    junk = K.tile(D, BF16)
    hTf = [K.tile(KC * 128, F32) for _ in range(2)]
    wr = v3(K.tile(KC * 36, F32), KC, 36)
    brt = K.tile(36, F32)
    lgt = [K.tile(36, F32) for _ in range(2)]
    sm = [K.tile(8, F32) for _ in range(2)]
    sr = [K.tile(24, F32) for _ in range(2)]
    oh = [K.tile(4, F32) for _ in range(2)]
    esel = [K.tile(8, F32) for _ in range(2)]
    exs = [K.tile(8, F32) for _ in range(2)]
    msk = [K.tile(8, F32) for _ in range(2)]
    mx8 = [K.tile(8, F32) for _ in range(2)]
    C3 = v3(io["C"], NT, 32)
    gffn, identf = io["gffn"], io["identf"]
    gfb = K.tile(D, F32)
    h2t = [K.tile(D, BF16) for _ in range(2)]
    K.dma("q_sp", gfb, io["g_ffn"].partition_broadcast(128), [], ["gfb"])
    s_mix3 = io["s_mixT"].rearrange("(kc p) t -> p kc t", p=128)
    K.dma("q_sp", mixs[0], s_mix3[:, :, 0:128], [], [("mix", 0)])
    w3 = io["w_out"].rearrange("(kc p) c -> p kc c", p=128)
    for j in range(4):
        K.dma("q_pool", wout[:, j * 4:(j + 1) * 4, :], w3[:, j * 4:(j + 1) * 4, :], [], [("wout", j)])
    wk = [("wout", j) for j in range(4)]
    K.dma("q_sp", wr[:, :, 0:4], io["w_rg"].rearrange("(kc p) c -> p kc c", p=128), [], ["wr0"])
    K.dma("q_sp", wr[:, :, 4:36], io["w_re"].rearrange("(kc p) c -> p kc c", p=128), [], ["wr1"])
    K.dma("q_sp", brt[:, 0:4], io["b_rg"].partition_broadcast(128), [], ["br0"])
    K.dma("q_sp", brt[:, 4:36], io["b_re"].partition_broadcast(128), [], ["br1"])

    def stage1(ti):
        b = ti % 2
        ts_ = slice(ti * 128, (ti + 1) * 128)
        xk, smk = ("xt", b), ("sm", b)
        K.dma("q_sp", xt[b], io["x"][ts_, :], [], [xk])
        if ti + 1 < NT:
            K.dma("q_sp", mixs[(ti + 1) % 2], s_mix3[:, :, (ti + 1) * 128:(ti + 2) * 128], [], [("mix", (ti + 1) % 2)])
        io["bg"].emit(2)
        for c in range(4):
            for kc in range(KC):
                K.op("pe", "matmul", [("mix", b)] + wk, [("ps", c)], out=K.bank(c), lhsT=mixs[b][:, kc, :],
                     rhs=wout[:, kc, c * 512:(c + 1) * 512], start=(kc == 0), stop=(kc == KC - 1))
        for c in range(4):
            K.op("dve", "tensor_tensor", [("ps", c), xk], [xk], out=xt[b][:, c * 512:(c + 1) * 512], in0=K.bank(c),
                 in1=xt[b][:, c * 512:(c + 1) * 512], op=ALU.add)
        K.dma("q_sp", io["s_x1"][ts_, :], xt[b], [xk], [("s_x1", ti)])
        K.op("act", "activation", [xk], ["junk", smk], out=junk, in_=xt[b], func=AF.Square, accum_out=sm[b][:, 0:1])
        K.op("dve", "tensor_scalar", [smk], [smk], out=sm[b][:, 1:2], in0=sm[b][:, 0:1], scalar1=1.0 / D, scalar2=EPS,
             op0=ALU.mult, op1=ALU.add)
        K.op("act", "activation", [smk], [smk], out=sm[b][:, 2:3], in_=sm[b][:, 1:2], func=AF.Sqrt)
        K.op("dve", "reciprocal", [smk], [smk], out=sm[b][:, 3:4], in_=sm[b][:, 2:3])
        K.op("dve", "tensor_scalar", [xk, smk], [("xn", b)], out=xn[b], in0=xt[b], scalar1=sm[b][:, 3:4], scalar2=None,
             op0=ALU.mult)
        K.op("pool", "tensor_tensor", [("xn", b), "gfb"], [("h2t", b)], out=h2t[b], in0=xn[b], in1=gfb, op=ALU.mult)
        K.dma("q_sp", io["s_h2"][ts_, :], h2t[b], [("h2t", b)], [("s_h2", ti)])

    def stage2(ti):
        b = ti % 2
        hk, lk, rk = ("hTf", b), ("lgt", b), ("sr", b)
        r = sr[b]
        for q4 in range(4):
            bk = 4 + q4
            for j in range(4):
                kc = q4 * 4 + j
                K.op("pe", "transpose", [("xn", b), "identf"], [("ps", bk)], out=K.bank(bk, 128, F32, j * 128),
                     in_=xn[b][:, kc * 128:(kc + 1) * 128], identity=identf)
            for j in range(4):
                kc = q4 * 4 + j
                K.op("act", "activation", [("ps", bk), "gffn"], [hk], out=hTf[b][:, kc * 128:(kc + 1) * 128],
                     in_=K.bank(bk, 128, F32, j * 128), func=AF.Copy, scale=gffn[:, kc:kc + 1])
        for kc in range(KC):
            K.op("pe", "matmul", [hk, "wr0", "wr1"], [("ps", 4 + b)], out=K.bank(4 + b)[:, 0:36],
                 lhsT=hTf[b][:, kc * 128:(kc + 1) * 128], rhs=wr[:, kc, :], start=(kc == 0), stop=(kc == KC - 1))
        K.op("dve", "tensor_tensor", [("ps", 4 + b), "br0", "br1"], [lk], out=lgt[b], in0=K.bank(4 + b)[:, 0:36], in1=brt,
             op=ALU.add)
        L = lgt[b]
        K.op("dve", "tensor_reduce", [lk], [rk], out=r[:, 8:9], in_=L[:, 0:4], axis=AX.X, op=ALU.max)
        K.op("dve", "tensor_scalar", [rk], [rk], out=r[:, 9:10], in0=r[:, 8:9], scalar1=-1.0, scalar2=None, op0=ALU.mult)
        K.op("act", "activation", [lk, rk], [("oh", b), rk], out=oh[b], in_=L[:, 0:4], func=AF.Exp, bias=r[:, 9:10],
             accum_out=r[:, 10:11])
        K.op("dve", "tensor_scalar", [lk, rk], [("oh", b)], out=oh[b], in0=L[:, 0:4], scalar1=r[:, 8:9], scalar2=None,
             op0=ALU.is_ge)
        K.op("dve", "tensor_scalar", [lk, ("oh", b)], [("esel", b)], out=esel[b], in0=L[:, 4:12], scalar1=oh[b][:, 0:1],
             scalar2=None, op0=ALU.mult)
        for g in range(1, 4):
            K.op("dve", "scalar_tensor_tensor", [lk, ("oh", b), ("esel", b)], [("esel", b)], out=esel[b],
                 in0=L[:, 4 + 8 * g:12 + 8 * g], scalar=oh[b][:, g:g + 1], in1=esel[b], op0=ALU.mult, op1=ALU.add)
        K.op("dve", "max", [("esel", b)], [("mx8", b)], out=mx8[b], in_=esel[b])
        K.op("dve", "tensor_scalar", [("mx8", b)], [rk], out=r[:, 12:13], in0=mx8[b][:, 0:1], scalar1=-1.0, scalar2=None,
             op0=ALU.mult)
        K.op("act", "activation", [("esel", b), rk], [("exs", b)], out=exs[b], in_=esel[b], func=AF.Exp, bias=r[:, 12:13])
        K.op("dve", "tensor_scalar", [("esel", b), ("mx8", b)], [("msk", b)], out=msk[b], in0=esel[b],
             scalar1=mx8[b][:, 1:2], scalar2=None, op0=ALU.is_ge)
        K.op("act", "activation", [("mx8", b), rk], [rk], out=r[:, 13:14], in_=mx8[b][:, 1:2], func=AF.Exp,
             bias=r[:, 12:13])
        K.op("dve", "tensor_scalar", [rk], [rk], out=r[:, 14:15], in0=r[:, 13:14], scalar1=1.0, scalar2=r[:, 10:11],
             op0=ALU.add, op1=ALU.mult)
        K.op("dve", "reciprocal", [rk], [rk], out=r[:, 15:16], in_=r[:, 14:15])
        K.op("dve", "tensor_scalar", [("oh", b), rk], [rk], out=r[:, 16:20], in0=oh[b], scalar1=r[:, 15:16], scalar2=None,
             op0=ALU.mult)
        for g in range(4):
            K.op("dve", "scalar_tensor_tensor", [("exs", b), rk, ("msk", b)], ["C"], out=C3[:, ti, g * 8:(g + 1) * 8],
                 in0=exs[b], scalar=r[:, 16 + g:17 + g], in1=msk[b], op0=ALU.mult, op1=ALU.mult)

    stage1(0)
    for ti in range(NT):
        if ti + 1 < NT:
            stage1(ti + 1)
        stage2(ti)
    S.barrier()


def phase_f(K, io):
    S = K.S
    C3 = v3(io["C"], NT, 32)
    for half in range(2):
        K.reset()
        h2T = v3(K.tile(KC * 1024, BF16), KC, 1024)
        acc = v3(K.tile(8 * D, F32), 8, D)
        hid = [v3(K.tile(2 * 1024, BF16), 2, 1024) for _ in range(2)]
        sg = [K.tile(512, BF16) for _ in range(2)]
        mark = K.off
        wg = [v3(K.tile(KC * 256, BF16), KC, 256) for _ in range(2)]
        wu = [v3(K.tile(KC * 256, BF16), KC, 256) for _ in range(2)]
        wd = [v3(K.tile(2 * D, BF16), 2, D) for _ in range(2)]
        K.dma("q_sp", h2T, io["s_h2T"].rearrange("(kc p) t -> p kc t", p=128)[:, :, half * 1024:(half + 1) * 1024],
              [], ["h2T"])
        K.op("dve", "memset", [], ["acc%d" % i for i in range(8)], ap=acc, constant=0.0)
        units = [(e, fh) for e in range(32) for fh in range(2)]
        gbc = [0]

        def load_gu(u):
            e, fh = units[u]
            g, ee = e // 8, e % 8
            wb = u % 2
            fs = slice(fh * 256, (fh + 1) * 256)
            K.dma("q_pool", wg[wb], io["w_eg"][g, ee].rearrange("(kc p) f -> p kc f", p=128)[:, :, fs], [], [("wg", wb)])
            K.dma("q_pool", wu[wb], io["w_eu"][g, ee].rearrange("(kc p) f -> p kc f", p=128)[:, :, fs], [], [("wu", wb)])

        def load_d(u):
            e, fh = units[u]
            g, ee = e // 8, e % 8
            wb = u % 2
            fs = slice(fh * 256, (fh + 1) * 256)
            K.dma("q_pool", wd[wb], io["w_ed"][g, ee][fs, :].rearrange("(fc p) d -> p fc d", p=128), [], [("wd", wb)])

        def gate_up_piece(u, piece):
            wb = u % 2
            fc, tb = piece // 2, piece % 2
            b0 = (gbc[0] % 2) * 2
            gbc[0] += 1
            for (bk, w, wkey) in ((b0, wg[wb], ("wg", wb)), (b0 + 1, wu[wb], ("wu", wb))):
                for kc in range(KC):
                    K.op("pe", "matmul", [wkey, "h2T"], [("ps", bk)], out=K.bank(bk),
                         lhsT=w[:, kc, fc * 128:(fc + 1) * 128], rhs=h2T[:, kc, tb * 512:(tb + 1) * 512],
                         start=(kc == 0), stop=(kc == KC - 1))
            sb_ = gbc[0] % 2
            K.op("act", "activation", [("ps", b0)], [("sg", sb_)], out=sg[sb_], in_=K.bank(b0), func=AF.Silu)
            K.op("dve", "tensor_tensor", [("ps", b0 + 1), ("sg", sb_)], [("hid", wb, fc, tb)],
                 out=hid[wb][:, fc, tb * 512:(tb + 1) * 512], in0=K.bank(b0 + 1), in1=sg[sb_], op=ALU.mult)

        def down_tile(u, ti):
            e, fh = units[u]
            wb = u % 2
            hk = [("hid", wb, fc, ti // 4) for fc in range(2)]
            for c in range(4):
                bk = 4 + c
                for fc in range(2):
                    K.op("pe", "matmul", hk + [("wd", wb)], [("ps", bk)], out=K.bank(bk),
                         lhsT=hid[wb][:, fc, ti * 128:(ti + 1) * 128], rhs=wd[wb][:, fc, c * 512:(c + 1) * 512],
                         start=(fc == 0), stop=(fc == 1))
                K.op("dve", "scalar_tensor_tensor", [("ps", bk), "C", "acc%d" % ti], ["acc%d" % ti],
                     out=acc[:, ti, c * 512:(c + 1) * 512], in0=K.bank(bk),
                     scalar=C3[:, half * 8 + ti, e:e + 1], in1=acc[:, ti, c * 512:(c + 1) * 512],
                     op0=ALU.mult, op1=ALU.add)

        nu = len(units)
        load_gu(0)
        load_d(0)
        load_gu(1)
        load_d(1)
        for p in range(4):
            gate_up_piece(0, p)
        for u in range(nu):
            if u + 2 < nu:
                load_gu(u + 2)
            for ti in range(8):
                down_tile(u, ti)
                if ti % 2 == 1 and u + 1 < nu:
                    gate_up_piece(u + 1, ti // 2)
            if u + 2 < nu:
                load_d(u + 2)
        S.barrier()
        K.off = mark
        gfin = K.tile(D, F32)
        x1t = [K.tile(D, F32) for _ in range(2)]
        junk = K.tile(D, BF16)
        sm = [K.tile(8, F32) for _ in range(2)]
        K.dma("q_sp", gfin, io["g_final"].partition_broadcast(128), [], ["gfin"])
        for ti in range(8):
            b = ti % 2
            tg = half * 8 + ti
            xk, sk = ("x1t", b), ("smf", b)
            K.dma("q_sp", x1t[b], io["s_x1"][tg * 128:(tg + 1) * 128, :], [], [xk])
            K.op("dve", "tensor_tensor", [xk, "acc%d" % ti], [xk], out=x1t[b], in0=x1t[b], in1=acc[:, ti, :], op=ALU.add)
            K.op("act", "activation", [xk], ["junkf", sk], out=junk, in_=x1t[b], func=AF.Square, accum_out=sm[b][:, 0:1])
            K.op("dve", "tensor_scalar", [sk], [sk], out=sm[b][:, 1:2], in0=sm[b][:, 0:1], scalar1=1.0 / D, scalar2=EPS,
                 op0=ALU.mult, op1=ALU.add)
            K.op("act", "activation", [sk], [sk], out=sm[b][:, 2:3], in_=sm[b][:, 1:2], func=AF.Sqrt)
            K.op("dve", "reciprocal", [sk], [sk], out=sm[b][:, 3:4], in_=sm[b][:, 2:3])
            K.op("dve", "scalar_tensor_tensor", [xk, sk, "gfin"], [xk], out=x1t[b], in0=x1t[b], scalar=sm[b][:, 3:4],
                 in1=gfin, op0=ALU.mult, op1=ALU.mult)
            K.dma("q_sp", io["y"][tg * 128:(tg + 1) * 128, :], x1t[b], [xk], [("y", tg)])
        S.barrier()


I32 = mybir.dt.int32
NTILE = 48
TS = 256
NSLOT = NTILE * TS


class BgCast:
    def __init__(self, K, io, stg):
        self.K, self.io, self.stg = K, io, stg
        self.jobs = []
        for e in range(32):
            g, ee = e // 8, e % 8
            for (src, dst, kind) in ((io["w_eg"], io["s_wgb"], 0), (io["w_eu"], io["s_wub"], 0), (io["w_ed"], io["s_wdb"], 1)):
                for q in range(4):
                    self.jobs.append((src, dst, kind, e, g, ee, q))
        self.i = 0

    def emit(self, n):
        K = self.K
        for _ in range(n):
            if self.i >= len(self.jobs):
                return
            src, dst, kind, e, g, ee, q = self.jobs[self.i]
            b = self.i % len(self.stg)
            self.i += 1
            st = self.stg[b]
            if kind == 0:
                K.dma("q_pool", v3(st, 4, 512), src[g, ee].rearrange("(kc p) f -> p kc f", p=128)[:, q * 4:(q + 1) * 4, :],
                      [], [("stg", b)])
            else:
                K.dma("q_pool", st, src[g, ee][q * 128:(q + 1) * 128, :], [], [("stg", b)])
            K.dma("q_sp", dst[e * 128:(e + 1) * 128, q * 2048:(q + 1) * 2048], st, [("stg", b)], [("wb", self.i)])


def phase_w(K, io):
    io["bg"].emit(100000)
    K.S.barrier()


def phase_e2(K, io):
    S = K.S
    K.reset()
    C3 = v3(io["C"], NT, 32)
    Cf = io["C"]
    Mnz = K.tile(512, F32)
    M1 = K.tile(512, F32)
    M2 = K.tile(512, F32)
    slotmat = K.tile(512, F32)
    tmp = K.tile(512, F32)
    ones = K.tile(128, F32)
    stri = K.tile(128, F32)
    thr = K.tile(8, F32)
    jidx = K.tile(NTILE, F32)
    pcol = K.tile(1, F32)
    n_ = K.tile(32, F32)
    cmp3 = K.tile(256, F32)
    tiles = K.tile(32, F32)
    one32 = K.tile(32, F32)
    tend = K.tile(32, F32)
    tbase = K.tile(32, F32)
    rowmax = K.tile(NT, F32)
    sl = K.tile(2 * NT, F32)
    cmpj = K.tile(NTILE * 32, F32)
    eid = K.tile(NTILE, F32)
    widf = K.tile(NTILE, F32)
    K.dma("q_sp", stri, io["c_stri"][:, :], [], ["stri"])
    K.dma("q_sp", thr, io["c_thr"][:, :], [], ["thr"])
    K.dma("q_sp", jidx, io["c_jidx"][:, :], [], ["jidx"])
    K.dma("q_sp", pcol, io["c_pcol"][:, :], [], ["pcol"])
    K.op("dve", "memset", [], ["ones"], ap=ones, constant=1.0)
    K.op("dve", "memset", [], ["one32"], ap=one32, constant=1.0)
    K.op("dve", "tensor_scalar", ["C"], ["Mnz"], out=Mnz, in0=Cf, scalar1=0.0, scalar2=None, op0=ALU.is_gt)
    Mnz3 = v3(Mnz, NT, 32)
    for ti in range(NT):
        o = K.bank(0)[:, ti * 32:(ti + 1) * 32]
        K.op("pe", "matmul", ["stri", "Mnz"], [("ps", 0)], out=o, lhsT=stri, rhs=Mnz3[:, ti, :], start=True, stop=(ti == 0))
        for tj in range(ti):
            K.op("pe", "matmul", ["ones", "Mnz"], [("ps", 0)], out=o, lhsT=ones, rhs=Mnz3[:, tj, :], start=False,
                 stop=(tj == ti - 1))
    for ti in range(NT):
        K.op("pe", "matmul", ["ones", "Mnz"], [("ps", 1)], out=K.bank(1)[:, 0:32], lhsT=ones, rhs=Mnz3[:, ti, :],
             start=(ti == 0), stop=(ti == NT - 1))
    K.op("dve", "tensor_copy", [("ps", 1)], ["n"], out=n_, in_=K.bank(1)[:, 0:32])
    K.op("dve", "tensor_tensor", ["n", "thr"], ["cmp3"], out=v3(cmp3, 32, 8), in0=n_.unsqueeze(2).broadcast_to([128, 32, 8]),
         in1=thr.unsqueeze(1).broadcast_to([128, 32, 8]), op=ALU.is_gt)
    K.op("dve", "tensor_reduce", ["cmp3"], ["tiles"], out=tiles, in_=v3(cmp3, 32, 8), axis=AX.X, op=ALU.add)
    K.op("dve", "tensor_tensor_scan", ["tiles", "one32"], ["tend"], out=tend, data0=one32, data1=tiles, initial=0.0,
         op0=ALU.mult, op1=ALU.add)
    K.op("dve", "tensor_tensor", ["tend", "tiles"], ["tbase"], out=tbase, in0=tend, in1=tiles, op=ALU.subtract)
    K.op("dve", "scalar_tensor_tensor", ["tbase", ("ps", 0)], ["slotmat"], out=v3(slotmat, NT, 32),
         in0=tbase.unsqueeze(1).broadcast_to([128, NT, 32]), scalar=float(TS), in1=v3(K.bank(0), NT, 32),
         op0=ALU.mult, op1=ALU.add)
    K.op("dve", "tensor_reduce", ["C"], ["rowmax"], out=rowmax, in_=C3, axis=AX.X, op=ALU.max)
    K.op("dve", "tensor_tensor", ["C", "rowmax"], ["M1"], out=v3(M1, NT, 32), in0=C3,
         in1=rowmax.unsqueeze(2).broadcast_to([128, NT, 32]), op=ALU.is_ge)
    K.op("dve", "tensor_tensor", ["Mnz", "M1"], ["M2"], out=M2, in0=Mnz, in1=M1, op=ALU.subtract)
    W2 = io["w12"]
    K.op("dve", "tensor_copy", ["rowmax"], ["w12"], out=W2[:, 0:NT], in_=rowmax)
    for (m, mk, k) in ((M1, "M1", 0), (M2, "M2", 1)):
        K.op("dve", "tensor_tensor", [mk, "slotmat"], ["tmp"], out=tmp, in0=m, in1=slotmat, op=ALU.mult)
        K.op("dve", "tensor_reduce", ["tmp"], ["sl"], out=sl[:, k * NT:(k + 1) * NT], in_=v3(tmp, NT, 32), axis=AX.X,
             op=ALU.add)
    K.op("dve", "tensor_tensor", ["M2", "C"], ["tmp"], out=tmp, in0=M2, in1=Cf, op=ALU.mult)
    K.op("dve", "tensor_reduce", ["tmp"], ["w12"], out=W2[:, NT:2 * NT], in_=v3(tmp, NT, 32), axis=AX.X, op=ALU.add)
    K.op("dve", "tensor_copy", ["sl"], ["slot"], out=io["slot"], in_=sl)
    K.op("dve", "tensor_tensor", ["tend", "jidx"], ["cmpj"], out=v3(cmpj, NTILE, 32),
         in0=tend.unsqueeze(1).broadcast_to([128, NTILE, 32]), in1=jidx.unsqueeze(2).broadcast_to([128, NTILE, 32]),
         op=ALU.is_le)
    K.op("dve", "tensor_reduce", ["cmpj"], ["eid"], out=eid, in_=v3(cmpj, NTILE, 32), axis=AX.X, op=ALU.add)
    rowb = K.tile(2 * NTILE, F32)
    unus = K.tile(NTILE, F32)
    K.dma("q_sp", rowb, io["c_rowbase"][:, :], [], ["rowb"])
    K.op("dve", "tensor_scalar", ["eid"], ["unus"], out=unus, in0=eid, scalar1=31.5, scalar2=1.0e6, op0=ALU.is_gt,
         op1=ALU.mult)
    K.op("dve", "tensor_tensor", ["rowb", "unus"], ["rowb"], out=v3(rowb, NTILE, 2), in0=v3(rowb, NTILE, 2),
         in1=unus.unsqueeze(2).broadcast_to([128, NTILE, 2]), op=ALU.add)
    K.op("dve", "tensor_copy", ["rowb"], ["ridx"], out=io["ridx"], in_=rowb)
    K.op("dve", "tensor_scalar", ["eid", "pcol"], ["widf"], out=widf, in0=eid, scalar1=128.0, scalar2=pcol[:, 0:1],
         op0=ALU.mult, op1=ALU.add)
    K.op("dve", "tensor_copy", ["widf"], ["widx"], out=io["widx"], in_=widf)
    if "dbg_slot" in io:
        K.dma("q_sp", io["dbg_slot"][:, :], sl, ["sl"], ["dbg_slot"])
        K.dma("q_sp", io["dbg_w12"][:, :], W2, ["w12"], ["dbg_w12"])
        K.dma("q_sp", io["dbg_wid"][:, :], widf, ["widf"], ["dbg_wid"])
    S.barrier()


def phase_fs(K, io):
    S = K.S
    K.reset()
    slot, widx, W2, ridx = io["slot"], io["widx"], io["w12"], io["ridx"]
    regs = {}

    def breg(e, v):
        if v not in regs:
            regs[v] = e.to_reg(v)
        return regs[v]

    IOA = bass.IndirectOffsetOnAxis
    ht = [K.tile(D, BF16) for _ in range(2)]
    for ti in range(NT):
        b = ti % 2
        K.dma("q_sp", ht[b], io["s_h2"][ti * 128:(ti + 1) * 128, :], [], [("ht", b)])
        for k in range(2):
            S.op("pool", lambda e, b=b, k=k, ti=ti: e.indirect_dma_start(
                out=io["s_xs"], out_offset=IOA(ap=slot[:, k * NT + ti:k * NT + ti + 1], axis=0), in_=ht[b], in_offset=None),
                [("ht", b), "slot"], [("xs", ti, k)], dma="q_pool")
    S.barrier()
    K.reset()
    wg = [K.tile(8192, BF16) for _ in range(2)]
    wu = [K.tile(8192, BF16) for _ in range(2)]
    wd = [K.tile(8192, BF16) for _ in range(2)]
    xs = [K.tile(2 * D, BF16) for _ in range(2)]
    xT = [K.tile(KC * TS, BF16) for _ in range(2)]
    hid = [K.tile(4 * TS, BF16) for _ in range(2)]
    sg = [K.tile(TS, BF16) for _ in range(2)]
    yt = [K.tile(D, BF16) for _ in range(2)]
    identb = io["identb"]
    yb = 0
    for j in range(NTILE):
        b = j % 2
        for (wt, src, nm) in ((wg[b], io["s_wgb"], "wg"), (wu[b], io["s_wub"], "wu"), (wd[b], io["s_wdb"], "wd")):
            S.op("pool", lambda e, wt=wt, src=src, j=j: e.indirect_dma_start(
                out=wt, out_offset=None, in_=src, in_offset=IOA(ap=widx[:, j:j + 1], axis=0),
                bounds_check=breg(e, 4095), oob_is_err=False),
                ["widx"], [(nm, b)], dma="q_pool")
        xs3 = v3(xs[b], 2, D)
        for sh in range(2):
            S.op("pool", lambda e, o=xs3[:, sh, :], c=2 * j + sh: e.indirect_dma_start(
                out=o, out_offset=None, in_=io["s_xs"], in_offset=IOA(ap=ridx[:, c:c + 1], axis=0),
                bounds_check=breg(e, NSLOT - 1), oob_is_err=False),
                ["ridx"], [("xsb", b)] if sh == 0 else [("xsb2", b)], dma="q_pool")
        xT3 = v3(xT[b], KC, TS)
        for sh in range(2):
            for q2 in range(2):
                bk = sh * 2 + q2
                for i in range(8):
                    kc = q2 * 8 + i
                    K.op("pe", "transpose", [("xsb", b), ("xsb2", b), "identb"], [("ps", bk)],
                         out=K.bank(bk, 128, BF16, i * 64), in_=xs3[:, sh, kc * 128:(kc + 1) * 128], identity=identb)
                K.evac(xT3[:, q2 * 8:(q2 + 1) * 8, sh * 128:(sh + 1) * 128], v3(K.bank(bk, 1024, BF16, 0), 8, 128),
                       [("ps", bk)], [("xT", b)])
        wg3, wu3, wd3 = v3(wg[b], KC, 512), v3(wu[b], KC, 512), v3(wd[b], 4, D)
        hid3 = v3(hid[b], 4, TS)
        for fc in range(4):
            bk = 4 + fc % 2
            for (w3_, nm, o0) in ((wg3, "wg", 0), (wu3, "wu", 256)):
                for kc in range(KC):
                    K.op("pe", "matmul", [(nm, b), ("xT", b)], [("ps", bk)], out=K.bank(bk)[:, o0:o0 + TS],
                         lhsT=w3_[:, kc, fc * 128:(fc + 1) * 128], rhs=xT3[:, kc, :], start=(kc == 0 and o0 == 0),
                         stop=(kc == KC - 1 and o0 == 256), skip_group_check=True)
            sb_ = fc % 2
            K.op("act", "activation", [("ps", bk)], [("sg", sb_)], out=sg[sb_], in_=K.bank(bk)[:, 0:TS], func=AF.Silu)
            K.op("dve", "tensor_tensor", [("ps", bk), ("sg", sb_)], [("hid", b)], out=hid3[:, fc, :],
                 in0=K.bank(bk)[:, TS:2 * TS], in1=sg[sb_], op=ALU.mult)
        for sh in range(2):
            y_ = yt[yb % 2]
            yk = ("yt", yb % 2)
            yb += 1
            for c in range(4):
                bk = 6 + c % 2
                for fc in range(4):
                    K.op("pe", "matmul", [("hid", b), ("wd", b)], [("ps", bk)], out=K.bank(bk),
                         lhsT=hid3[:, fc, sh * 128:(sh + 1) * 128], rhs=wd3[:, fc, c * 512:(c + 1) * 512],
                         start=(fc == 0), stop=(fc == 3))
                K.evac(y_[:, c * 512:(c + 1) * 512], K.bank(bk), [("ps", bk)], [yk])
            K.dma("q_sp", io["s_ys"][j * TS + sh * 128:j * TS + (sh + 1) * 128, :], y_, [yk], [("ys", j, sh)])
    S.barrier()
    K.reset()
    gfin = K.tile(D, F32)
    x1t = [K.tile(D, F32) for _ in range(2)]
    y1 = [K.tile(D, BF16) for _ in range(2)]
    y2 = [K.tile(D, BF16) for _ in range(2)]
    junk = K.tile(D, BF16)
    sm = [K.tile(8, F32) for _ in range(2)]
    K.dma("q_sp", gfin, io["g_final"].partition_broadcast(128), [], ["gfin"])
    for ti in range(NT):
        b = ti % 2
        xk, sk = ("x1t", b), ("smf", b)
        K.dma("q_sp", x1t[b], io["s_x1"][ti * 128:(ti + 1) * 128, :], [], [xk])
        for (yy, nm, k) in ((y1[b], "y1", 0), (y2[b], "y2", 1)):
            S.op("pool", lambda e, yy=yy, k=k, ti=ti: e.indirect_dma_start(
                out=yy, out_offset=None, in_=io["s_ys"], in_offset=IOA(ap=slot[:, k * NT + ti:k * NT + ti + 1], axis=0)),
                ["slot"], [(nm, b)], dma="q_pool")
        K.op("dve", "scalar_tensor_tensor", [("y1", b), "w12", xk], [xk], out=x1t[b], in0=y1[b], scalar=W2[:, ti:ti + 1],
             in1=x1t[b], op0=ALU.mult, op1=ALU.add)
        K.op("dve", "scalar_tensor_tensor", [("y2", b), "w12", xk], [xk], out=x1t[b], in0=y2[b],
             scalar=W2[:, NT + ti:NT + ti + 1], in1=x1t[b], op0=ALU.mult, op1=ALU.add)
        K.op("act", "activation", [xk], ["junkf", sk], out=junk, in_=x1t[b], func=AF.Square, accum_out=sm[b][:, 0:1])
        K.op("dve", "tensor_scalar", [sk], [sk], out=sm[b][:, 1:2], in0=sm[b][:, 0:1], scalar1=1.0 / D, scalar2=EPS,
             op0=ALU.mult, op1=ALU.add)
        K.op("act", "activation", [sk], [sk], out=sm[b][:, 2:3], in_=sm[b][:, 1:2], func=AF.Sqrt)
        K.op("dve", "reciprocal", [sk], [sk], out=sm[b][:, 3:4], in_=sm[b][:, 2:3])
        K.op("dve", "scalar_tensor_tensor", [xk, sk, "gfin"], [xk], out=x1t[b], in0=x1t[b], scalar=sm[b][:, 3:4],
             in1=gfin, op0=ALU.mult, op1=ALU.mult)
        K.dma("q_sp", io["y"][ti * 128:(ti + 1) * 128, :], x1t[b], [xk], [("y", ti)])
    S.barrier()


def build_nc(upto="a", debug=False):
    nc = bass.Bass("TRN2", target_bir_lowering=False)
    io = {}

    def inp(name, shape, dt=F32):
        io[name] = nc.dram_tensor(name, list(shape), dt, kind="ExternalInput").ap()

    def scratch(name, shape, dt):
        kind = "ExternalOutput" if debug else "Internal"
        io[name] = nc.dram_tensor(name, list(shape), dt, kind=kind).ap()

    inp("x", [T, D])
    inp("w_in", [D, W_IN])
    inp("c_identf", [128, 128])
    inp("c_gmix", [128, KC])
    inp("b_gate", [24])
    for kv in "kv":
        inp("w_cmp_%s1" % kv, [4096, 256])
        inp("w_cmp_%s2" % kv, [256, 128])
        inp("c_pos%s" % kv, [128, 32])
    inp("c_dsel", [128, 2048])
    inp("c_dwin", [128, 640])
    inp("c_dcmp", [128, NT * 127])
    inp("c_rowvalid", [128, NT])
    inp("c_mulmask", [128, NT * 32])
    inp("c_addmask", [128, NT * 32])
    inp("c_overlap", [127, 32])
    inp("w_alpha2", [16, 512])
    inp("b_alpha", [512])
    inp("g_gla", [256])
    inp("c_tris", [128, 128])
    inp("w_out", [D, D])
    inp("c_gffn", [128, KC])
    inp("w_rg", [D, 4])
    inp("w_re", [D, 32])
    inp("b_rg", [4])
    inp("b_re", [32])
    inp("w_eg", [4, 8, D, 512])
    inp("w_eu", [4, 8, D, 512])
    inp("w_ed", [4, 8, 512, D])
    inp("g_final", [D])
    inp("c_mask01", [128, 128])
    scratch("s_fm", [FM_ROWS, T], BF16)
    scratch("s_tm", [T, TM_COLS], BF16)
    scratch("s_gate", [T, 24], F32)
    scratch("s_alphaT", [16, T], F32)
    scratch("s_mixT", [MIX_ROWS, T], BF16)
    scratch("s_x1", [T, D], F32)
    scratch("s_h2T", [D, T], BF16)
    scratch("s_h2", [T, D], BF16)
    for nm in ("s_wgb", "s_wub", "s_wdb"):
        io[nm] = nc.dram_tensor(nm, [4096, 8192], BF16, kind="Internal").ap()
    io["s_xs"] = nc.dram_tensor("s_xs", [NSLOT, D], BF16, kind="Internal").ap()
    io["s_ys"] = nc.dram_tensor("s_ys", [NSLOT, D], BF16, kind="Internal").ap()
    inp("g_ffn", [D])
    inp("c_stri", [128, 128])
    inp("c_thr", [128, 8])
    inp("c_jidx", [128, NTILE])
    inp("c_pcol", [128, 1])
    inp("c_rowbase", [128, 2 * NTILE])
    if debug:
        io["dbg_slot"] = nc.dram_tensor("dbg_slot", [128, 2 * NT], F32, kind="ExternalOutput").ap()
        io["dbg_w12"] = nc.dram_tensor("dbg_w12", [128, 2 * NT], F32, kind="ExternalOutput").ap()
        io["dbg_wid"] = nc.dram_tensor("dbg_wid", [128, NTILE], F32, kind="ExternalOutput").ap()
    io["y"] = nc.dram_tensor("y", [T, D], F32, kind="ExternalOutput").ap()

    with ExitStack() as st:
        S = Sched(nc)
        S.setup(st)
        big = st.enter_context(nc.sbuf_tensor("big", [128, SBUF_WORDS], F32))
        ps = st.enter_context(nc.psum_tensor("ps", [128, 4096], F32))
        K = Ctx(nc, S, big, ps)
        identf = K.tile(128, F32)
        gmix = K.tile(KC, F32)
        identb = K.tile(128, BF16)
        io["kcT"] = K.tile(256, BF16)
        io["vc"] = K.tile(256, BF16)
        io["C"] = K.tile(NT * 32, F32)
        io["w12"] = K.tile(2 * NT, F32)
        io["slot"] = K.tile(2 * NT, F32).bitcast(I32)
        io["widx"] = K.tile(NTILE, F32).bitcast(I32)
        io["ridx"] = K.tile(2 * NTILE, F32).bitcast(I32)
        io["bg"] = BgCast(K, io, [K.tile(2048, BF16) for _ in range(4)])
        gffn = K.tile(KC, F32)
        io["gffn"] = gffn
        K.base = K.off
        K.dma("q_sp", gffn, io["c_gffn"][:, :], [], ["gffn"])
        K.dma("q_sp", identf, io["c_identf"][:, :], [], ["identf"])
        K.dma("q_sp", gmix, io["c_gmix"][:, :], [], ["gmix"])
        io["identf"] = identf
        io["gmix"] = gmix
        io["identb"] = identb
        K.op("dve", "tensor_copy", ["identf"], ["identb"], out=identb, in_=identf)
        S.barrier()
        if "a" in upto:
            phase_a(K, io)
        if "b" in upto:
            phase_b(K, io)
        if "c" in upto:
            phase_c(K, io)
        if "d" in upto:
            phase_d(K, io)
        if "e" in upto:
            phase_e(K, io)
        if "f" in upto:
            phase_f(K, io)
        if "w" in upto:
            phase_w(K, io)
        if "g" in upto:
            phase_e2(K, io)
        if "s" in upto:
            phase_fs(K, io)
        S.barrier()
        with nc.Block() as block:
            S.emit(block)
    return nc, S


def _make_consts():
    f = np.float32
    c = {}
    q = np.arange(128)[:, None]
    u = np.arange(2048)[None, :]
    d = (q + 1920 - u).astype(f)
    c["c_dsel"] = np.where(d < 0, BIGD, d).astype(f)
    cc = np.arange(640)[None, :]
    d = (q + 512 - cc).astype(f)
    c["c_dwin"] = np.where((d < 0) | (d >= 512), BIGD, d).astype(f)
    n = np.arange(127)[None, None, :]
    qt = np.arange(NT)[None, :, None]
    d = (qt * 128 + q[:, :, None] - (16 * n + 31)).astype(f)
    c["c_dcmp"] = np.where(d < 0, BIGD, d).astype(f).reshape(128, NT * 127)
    t = (np.arange(NT)[None, :] * 128 + q)
    c["c_rowvalid"] = (t >= 31).astype(f)
    j = np.arange(32)[None, None, :]
    tt = t[:, :, None]
    cur = tt // 64
    forced = (j == 0) | ((j <= cur) & (j > cur - 2))
    causal = (j * 64 <= tt)
    c["c_mulmask"] = ((~forced) & causal).astype(f).reshape(128, NT * 32)
    c["c_addmask"] = np.where(forced, BIG, np.where(causal, 0.0, -BIG)).astype(f).reshape(128, NT * 32)
    cs = np.arange(127)[:, None] * 16
    ss = np.arange(32)[None, :] * 64
    c["c_overlap"] = ((cs < ss + 64) & (cs + 32 > ss)).astype(f)
    c["c_identf"] = np.eye(128, dtype=f)
    tri = (np.arange(128)[:, None] <= np.arange(128)[None, :])
    c["c_tris"] = (tri * (-1.0 / 16.0)).astype(f)
    c["c_mask01"] = tri.astype(f)
    c["c_stri"] = (np.arange(128)[:, None] < np.arange(128)[None, :]).astype(f)
    c["c_thr"] = np.tile((np.arange(8) * 256.0)[None, :], (128, 1)).astype(f)
    c["c_jidx"] = np.tile(np.arange(48, dtype=f)[None, :], (128, 1)).astype(f)
    c["c_pcol"] = np.arange(128, dtype=f)[:, None]
    c["c_rowbase"] = (np.arange(96, dtype=f)[None, :] * 128 + np.arange(128, dtype=f)[:, None]).astype(f)
    return {k: np.ascontiguousarray(v) for k, v in c.items()}


_CONSTS = _make_consts()


def host_inputs(inputs, b):
    f = np.float32
    m = {}
    m["x"] = np.ascontiguousarray(inputs["x"][b], dtype=f)
    m["w_in"] = np.ascontiguousarray(inputs["w_in"][0], dtype=f)
    m["c_identf"] = np.eye(128, dtype=f)
    m["c_gmix"] = np.ascontiguousarray(inputs["g_mix_norm"][0].reshape(KC, 128).T, dtype=f)
    m["b_gate"] = np.ascontiguousarray(inputs["b_nsa_gate"][0], dtype=f)
    m["w_cmp_k1"] = np.ascontiguousarray(inputs["w_cmp_k1"][0], dtype=f)
    m["w_cmp_k2"] = np.ascontiguousarray(inputs["w_cmp_k2"][0], dtype=f)
    m["w_cmp_v1"] = np.ascontiguousarray(inputs["w_cmp_v1"][0], dtype=f)
    m["w_cmp_v2"] = np.ascontiguousarray(inputs["w_cmp_v2"][0], dtype=f)
    m["c_posk"] = np.ascontiguousarray(inputs["cmp_pos_k"][0].T, dtype=f)
    m["c_posv"] = np.ascontiguousarray(inputs["cmp_pos_v"][0].T, dtype=f)
    m["w_alpha2"] = np.ascontiguousarray(inputs["w_alpha2"][0], dtype=f)
    m["b_alpha"] = np.ascontiguousarray(inputs["b_alpha"][0], dtype=f)
    m["g_gla"] = np.ascontiguousarray(inputs["g_gla_norm"][0], dtype=f)
    m["w_out"] = np.ascontiguousarray(inputs["w_out"][0], dtype=f)
    m["c_gffn"] = np.ascontiguousarray(inputs["g_ffn_norm"][0].reshape(KC, 128).T, dtype=f)
    m["w_rg"] = np.ascontiguousarray(inputs["w_router_group"][0], dtype=f)
    m["w_re"] = np.ascontiguousarray(inputs["w_router_expert"][0].reshape(D, 32), dtype=f)
    m["b_rg"] = np.ascontiguousarray(inputs["b_router_group"][0], dtype=f)
    m["b_re"] = np.ascontiguousarray(inputs["b_router_expert"][0].reshape(32), dtype=f)
    m["w_eg"] = np.ascontiguousarray(inputs["w_expert_gate"][0], dtype=f)
    m["w_eu"] = np.ascontiguousarray(inputs["w_expert_up"][0], dtype=f)
    m["w_ed"] = np.ascontiguousarray(inputs["w_expert_down"][0], dtype=f)
    m["g_final"] = np.ascontiguousarray(inputs["g_final_norm"], dtype=f)
    m["g_ffn"] = np.ascontiguousarray(inputs["g_ffn_norm"][0], dtype=f)
    m.update(_CONSTS)
    return m


def kernel(**inputs):
    nc, _ = build_nc("abcdewgs", debug=False)
    in_maps = [host_inputs(inputs, b) for b in range(8)]
    res = run_bass_kernel_spmd(nc, in_maps, core_ids=list(range(8)))
    return np.stack([np.asarray(r["y"], dtype=np.float32) for r in res.results], axis=0)
```

```python
import numpy as np
import ml_dtypes
from contextlib import ExitStack
import concourse.bass as bass
import concourse.mybir as mybir
from concourse.bass_utils import run_bass_kernel_spmd

F32 = mybir.dt.float32
BF16 = mybir.dt.bfloat16
AF = mybir.ActivationFunctionType
ALU = mybir.AluOpType
AX = mybir.AxisListType

D = 2048
T = 2048
NT = 16
KC = 16
EPS = 1e-6
W_IN = 5672
SBUF_WORDS = 49152


class _Stream:
    def __init__(self, name, issuer, sems, is_dma):
        self.name = name
        self.issuer = issuer
        self.sems = sems
        self.is_dma = is_dma
        self.count = 0

    def target(self, c):
        if not self.is_dma:
            return (self.sems[0], c)
        k = len(self.sems)
        return (self.sems[(c - 1) % k], 16 * ((c - 1) // k + 1))


class Sched:
    ENGINES = ("pe", "dve", "act", "pool", "sp")

    def __init__(self, nc, ring=8):
        self.nc = nc
        self.ring = ring
        self.items = {e: [] for e in self.ENGINES}
        self.streams = {}
        self.waited = {e: {} for e in self.ENGINES}
        self.last_write = {}
        self.readers = {}
        self.n_ops = 0

    def setup(self, stack):
        nc = self.nc
        for e in self.ENGINES:
            s = stack.enter_context(nc.semaphore("s_" + e))
            self.streams[e] = _Stream(e, e, [s], False)
        for q, issuer in (("q_sp", "sp"), ("q_pool", "pool"), ("q_act", "act")):
            sems = [stack.enter_context(nc.semaphore("s_%s_%d" % (q, i))) for i in range(self.ring)]
            self.streams[q] = _Stream(q, issuer, sems, True)

    def _need(self, eng, dep, waits):
        if dep is None:
            return
        sname, c = dep
        st = self.streams[sname]
        if not st.is_dma:
            if sname == eng and eng == "pe":
                return
            if self.waited[eng].get(sname, 0) >= c:
                return
            waits[sname] = max(waits.get(sname, 0), c)
        else:
            w = self.waited[eng].setdefault(sname, set())
            if c in w:
                return
            waits.setdefault(sname, set()).add(c)

    def op(self, eng, fn, reads=(), writes=(), dma=None):
        waits = {}
        for k in reads:
            self._need(eng, self.last_write.get(k), waits)
        for k in writes:
            self._need(eng, self.last_write.get(k), waits)
            for rn, rc in list(self.readers.get(k, {}).items()):
                if rn == eng and dma is None and eng == "pe":
                    continue
                if isinstance(rc, set):
                    for cc in rc:
                        self._need(eng, (rn, cc), waits)
                else:
                    self._need(eng, (rn, rc), waits)
        sname = dma if dma is not None else eng
        st = self.streams[sname]
        assert st.issuer == eng
        st.count += 1
        c = st.count
        wl = []
        if st.is_dma and c > len(st.sems):
            sem, val = st.target(c)
            wl.append((sem, val - 16))
        for n, v in waits.items():
            s2 = self.streams[n]
            if s2.is_dma:
                for cc in sorted(v):
                    wl.append(s2.target(cc))
                    self.waited[eng][n].add(cc)
            else:
                wl.append(s2.target(v))
                self.waited[eng][n] = v
        me = (sname, c)
        for k in reads:
            if st.is_dma:
                self.readers.setdefault(k, {}).setdefault(sname, set()).add(c)
            else:
                self.readers.setdefault(k, {})[sname] = c
        for k in writes:
            self.last_write[k] = me
            self.readers[k] = {}
        sem, _ = st.target(c)
        self.items[eng].append((wl, fn, sem, 16 if st.is_dma else 1))
        self.n_ops += 1
        return me

    def barrier(self):
        for eng in self.ENGINES:
            wl = []
            for n, st in self.streams.items():
                if st.count == 0:
                    continue
                if st.is_dma:
                    w = self.waited[eng].setdefault(n, set())
                    for c in range(max(1, st.count - len(st.sems) + 1), st.count + 1):
                        if c not in w:
                            wl.append(st.target(c))
                            w.add(c)
                else:
                    if n == eng:
                        continue
                    if self.waited[eng].get(n, 0) < st.count:
                        wl.append(st.target(st.count))
                        self.waited[eng][n] = st.count
            if wl:
                self.items[eng].append((wl, None, None, 0))
        self.last_write = {}
        self.readers = {}

    def emit(self, block):
        def run(engname):
            def f(e):
                for wl, fn, sem, inc in self.items[engname]:
                    for (ws, wv) in wl:
                        e.wait_ge(ws, wv)
                    if fn is not None:
                        fn(e).then_inc(sem, inc)
            return f

        block.tensor(run("pe"))
        block.vector(run("dve"))
        block.scalar(run("act"))
        block.gpsimd(run("pool"))
        block.sync(run("sp"))


class Ctx:
    def __init__(self, nc, S, big, ps):
        self.nc = nc
        self.S = S
        self.big = big
        self.ps = ps
        self.base = 0
        self.off = 0
        self.rr = 0

    def persist(self, free, dt):
        ap = self.tile(free, dt)
        self.base = self.off
        return ap

    def reset(self):
        self.off = self.base

    def tile(self, free, dt):
        words = free if dt == F32 else (free + 1) // 2
        words = (words + 7) // 8 * 8
        a = self.big[:, self.off:self.off + words]
        self.off += words
        assert self.off <= SBUF_WORDS, "SBUF overflow %d" % self.off
        if dt == F32:
            a = a[:, 0:free]
        if dt != F32:
            a = a.bitcast(dt)
            if a.shape[1] != free:
                a = a[:, 0:free]
        return a

    def bank(self, b, n=512, dt=F32, off=0):
        if dt == F32:
            return self.ps[:, b * 512 + off:b * 512 + off + n]
        return self.ps[:, b * 512 + off:b * 512 + off + (n + 1) // 2].bitcast(dt)

    def op(self, eng, method, reads, writes, **kw):
        return self.S.op(eng, lambda e: getattr(e, method)(**kw), reads, writes)

    def dma(self, q, out, in_, reads, writes):
        eng = {"q_sp": "sp", "q_pool": "pool", "q_act": "act"}[q]
        return self.S.op(eng, lambda e: e.dma_start(out=out, in_=in_), reads, writes, dma=q)

    def evac(self, out, in_, reads, writes, scale=None):
        self.rr += 1
        if self.rr % 2 == 0:
            if scale is None:
                return self.op("act", "activation", reads, writes, out=out, in_=in_, func=AF.Copy)
            return self.op("act", "activation", reads, writes, out=out, in_=in_, func=AF.Copy, scale=scale)
        if scale is None:
            return self.op("dve", "tensor_copy", reads, writes, out=out, in_=in_)
        return self.op("dve", "tensor_scalar", reads, writes, out=out, in0=in_, scalar1=scale, scalar2=None,
                       op0=ALU.mult)


def v3(ap, a, b):
    return ap.rearrange("p (a b) -> p a b", a=a, b=b)


FM_PARTS = [("q", 0, 1024, 0), ("kcmp", 1024, 256, 1024), ("vcmp", 1280, 256, 1280), ("ksel", 1536, 256, 1536),
            ("kwin", 2048, 256, 1792), ("gq", 2584, 512, 2048), ("gk", 3096, 512, 2560)]
FM_ROWS = 3072
ALPHA_COL = 4632
TM_PARTS = [("vsel", 1792, 256, 0), ("vwin", 2304, 256, 256), ("gv0", 3608, 512, 512), ("gv1", 4120, 512, 1024),
            ("og0", 4648, 512, 1536), ("og1", 5160, 512, 2048)]
TM_COLS = 2560
GATE_COL = 2560


def phase_a(K, io):
    S = K.S
    K.reset()
    hT = K.tile(KC * T, BF16)
    hT3 = v3(hT, KC, T)
    mark = K.off
    xt = [K.tile(D, F32) for _ in range(2)]
    junk = K.tile(D, BF16)
    st = [K.tile(8, F32) for _ in range(2)]
    for ti in range(NT):
        b = ti % 2
        xk, sk = ("xt", b), ("st", b)
        K.dma("q_sp", xt[b], io["x"][ti * 128:(ti + 1) * 128, :], [], [xk])
        K.op("act", "activation", [xk], ["junk", sk], out=junk, in_=xt[b], func=AF.Square, accum_out=st[b][:, 0:1])
        K.op("dve", "tensor_scalar", [sk], [sk], out=st[b][:, 1:2], in0=st[b][:, 0:1], scalar1=1.0 / D, scalar2=EPS,
             op0=ALU.mult, op1=ALU.add)
        K.op("act", "activation", [sk], [sk], out=st[b][:, 2:3], in_=st[b][:, 1:2], func=AF.Sqrt)
        K.op("dve", "reciprocal", [sk], [sk], out=st[b][:, 3:4], in_=st[b][:, 2:3])
        K.op("dve", "tensor_scalar", [xk, sk], [xk], out=xt[b], in0=xt[b], scalar1=st[b][:, 3:4], scalar2=None,
             op0=ALU.mult)
        for q4 in range(4):
            bk = 4 * b + q4
            pk = ("ps", bk)
            for j in range(4):
                kc = q4 * 4 + j
                K.op("pe", "transpose", [xk, "identf"], [pk], out=K.bank(bk, 128, F32, j * 128),
                     in_=xt[b][:, kc * 128:(kc + 1) * 128], identity=io["identf"])
            for j in range(4):
                kc = q4 * 4 + j
                K.evac(hT3[:, kc, ti * 128:(ti + 1) * 128], K.bank(bk, 128, F32, j * 128), [pk, "gmix"], [("hT", ti)],
                       scale=io["gmix"][:, kc:kc + 1])
    S.barrier()
    K.off = mark
    w_in3 = io["w_in"].rearrange("(kc p) c -> p kc c", p=128)
    wfm = [K.tile(KC * 128, BF16) for _ in range(3)]
    ofm = [K.tile(T, BF16) for _ in range(2)]
    ofa = K.tile(T, F32)
    chunks = []
    for (nm, c0, n, r0) in FM_PARTS:
        for j in range(n // 128):
            chunks.append((c0 + j * 128, 128, r0 + j * 128, False))
    chunks.append((ALPHA_COL, 16, 0, True))
    hkeys = [("hT", ti) for ti in range(NT)]
    pb = 0
    for ci, (c0, n, r0, is_alpha) in enumerate(chunks):
        io["bg"].emit(2)
        wb = ci % 3
        wk = ("wfm", wb)
        w3 = v3(wfm[wb], KC, 128)
        K.dma("q_pool", w3[:, :, 0:n], w_in3[:, :, c0:c0 + n], [], [wk])
        ob = ci % 2
        ok = ("ofa",) if is_alpha else ("ofm", ob)
        for tb in range(4):
            bk = pb % 8
            pb += 1
            pk = ("ps", bk)
            for kc in range(KC):
                K.op("pe", "matmul", [wk] + hkeys[tb * 4:tb * 4 + 4], [pk], out=K.bank(bk)[0:n, :],
                     lhsT=w3[:, kc, 0:n], rhs=hT3[:, kc, tb * 512:(tb + 1) * 512], start=(kc == 0), stop=(kc == KC - 1))
            dst = ofa[0:n, tb * 512:(tb + 1) * 512] if is_alpha else ofm[ob][0:n, tb * 512:(tb + 1) * 512]
            K.evac(dst, K.bank(bk)[0:n, :], [pk], [ok])
        if is_alpha:
            K.dma("q_sp", io["s_alphaT"][:, :], ofa[0:16, :], [ok], [("s_alphaT",)])
        else:
            K.dma("q_sp", io["s_fm"][r0:r0 + n, :], ofm[ob][0:n, :], [ok], [("s_fm", r0)])
    wtm = [K.tile(KC * 512, BF16) for _ in range(2)]
    otm = [K.tile(NT * 512, BF16) for _ in range(2)]
    ogt = K.tile(NT * 24, F32)
    bg = K.tile(24, F32)
    K.dma("q_sp", bg, io["b_gate"].partition_broadcast(128), [], ["bg"])
    s_tm3 = io["s_tm"].rearrange("(ti p) c -> p ti c", p=128)
    groups = [(c0, n, t0, False) for (nm, c0, n, t0) in TM_PARTS] + [(GATE_COL, 24, 0, True)]
    for gi, (c0, n, t0, is_gate) in enumerate(groups):
        wb = gi % 2
        wk = ("wtm", wb)
        w3 = v3(wtm[wb], KC, 512)
        K.dma("q_pool", w3[:, :, 0:n], w_in3[:, :, c0:c0 + n], [], [wk])
        ok = ("ogt",) if is_gate else ("otm", wb)
        o3 = v3(ogt, NT, 24) if is_gate else v3(otm[wb], NT, 512)
        for ti in range(NT):
            if ti % 4 == 0:
                io["bg"].emit(1)
            bk = pb % 8
            pb += 1
            pk = ("ps", bk)
            for kc in range(KC):
                K.op("pe", "matmul", [wk, ("hT", ti)], [pk], out=K.bank(bk)[:, 0:n],
                     lhsT=hT3[:, kc, ti * 128:(ti + 1) * 128], rhs=w3[:, kc, 0:n], start=(kc == 0), stop=(kc == KC - 1))
            if is_gate:
                K.op("dve", "tensor_tensor", [pk, "bg"], [ok], out=o3[:, ti, :], in0=K.bank(bk)[:, 0:n], in1=bg,
                     op=ALU.add)
            else:
                K.evac(o3[:, ti, 0:n], K.bank(bk)[:, 0:n], [pk], [ok])
        if is_gate:
            K.dma("q_sp", io["s_gate"].rearrange("(ti p) c -> p ti c", p=128), o3, [ok], [("s_gate",)])
        else:
            K.dma("q_sp", s_tm3[:, :, t0:t0 + n], o3[:, :, 0:n], [ok], [("s_tm", t0)])
    S.barrier()


SCALE = 128.0 ** -0.5
SLOPES = [2.0 ** (-(h + 1)) for h in range(8)]
BIG = 1.0e30
BIGD = 30000.0
MIX_ROWS = 2048


def phase_b(K, io):
    S = K.S
    K.reset()
    kT = K.tile(4 * T, BF16)
    kT3 = v3(kT, 4, T)
    w1 = [K.tile(32 * 256, BF16) for _ in range(2)]
    w2 = [K.tile(2 * 128, BF16) for _ in range(2)]
    pos = [K.tile(32, F32) for _ in range(2)]
    kp = [K.tile(32 * 127, BF16) for _ in range(2)]
    gel = [K.tile(2 * 127, BF16) for _ in range(2)]
    tx2 = K.tile(127, F32)
    tu = K.tile(127, F32)
    K.dma("q_sp", kT3, io["s_fm"][1024:1536, :].rearrange("(a p) t -> p a t", p=128), [], ["kT"])
    for kv, (n1, n2, npz) in enumerate((("w_cmp_k1", "w_cmp_k2", "c_posk"), ("w_cmp_v1", "w_cmp_v2", "c_posv"))):
        K.dma("q_pool", v3(w1[kv], 32, 256), io[n1].rearrange("(l d) j -> d l j", d=128), [], [("w1", kv)])
        K.dma("q_pool", v3(w2[kv], 2, 128), io[n2].rearrange("(jc j) d -> j jc d", j=128), [], [("w2", kv)])
        K.dma("q_sp", pos[kv], io[npz][:, :], [], [("pos", kv)])
    cnt = 0
    for kv in range(2):
        io["bg"].emit(10)
        w13 = v3(w1[kv], 32, 256)
        w23 = v3(w2[kv], 2, 128)
        for g in range(2):
            pb_ = cnt % 2
            cnt += 1
            kp3 = v3(kp[pb_], 32, 127)
            gl3 = v3(gel[pb_], 2, 127)
            for l in range(32):
                eng = "dve"
                K.op(eng, "tensor_scalar", ["kT", ("pos", kv)], [("kp", pb_)], out=kp3[:, l, :],
                     in0=kT3[:, kv * 2 + g, l:l + 2017:16], scalar1=pos[kv][:, l:l + 1], scalar2=None, op0=ALU.add)
            for jc in range(2):
                bk = jc
                pk = ("ps", bk)
                x = K.bank(bk)[:, 0:127]
                for l in range(32):
                    K.op("pe", "matmul", [("w1", kv), ("kp", pb_)], [pk], out=x, lhsT=w13[:, l, jc * 128:(jc + 1) * 128],
                         rhs=kp3[:, l, :], start=(l == 0), stop=(l == 31))
                K.op("act", "activation", [pk], ["tx2"], out=tx2, in_=x, func=AF.Square)
                K.op("dve", "tensor_scalar", ["tx2"], ["tu"], out=tu, in0=tx2, scalar1=0.044715, scalar2=1.0,
                     op0=ALU.mult, op1=ALU.add)
                K.op("dve", "tensor_tensor", ["tu", pk], ["tu"], out=tu, in0=tu, in1=x, op=ALU.mult)
                K.op("act", "activation", ["tu"], ["tx2"], out=tx2, in_=tu, func=AF.Tanh, scale=0.7978845608028654)
                K.op("dve", "tensor_scalar", ["tx2"], ["tu"], out=tu, in0=tx2, scalar1=1.0, scalar2=0.5,
                     op0=ALU.add, op1=ALU.mult)
                K.op("dve", "tensor_tensor", ["tu", pk], [("gel", pb_)], out=gl3[:, jc, :], in0=tu, in1=x, op=ALU.mult)
            bk = 2 + (cnt % 2)
            pk = ("ps", bk)
            if kv == 0:
                o = K.bank(bk)[:, 0:127]
                for jc in range(2):
                    K.op("pe", "matmul", [("w2", kv), ("gel", pb_)], [pk], out=o, lhsT=w23[:, jc, :], rhs=gl3[:, jc, :],
                         start=(jc == 0), stop=(jc == 1))
                K.evac(io["kcT"][:, g * 128:g * 128 + 127], o, [pk], ["kcT"])
            else:
                o = K.bank(bk)[0:127, 0:128]
                for jc in range(2):
                    K.op("pe", "matmul", [("w2", kv), ("gel", pb_)], [pk], out=o, lhsT=gl3[:, jc, :], rhs=w23[:, jc, :],
                         start=(jc == 0), stop=(jc == 1))
                K.evac(io["vc"][0:127, g * 128:(g + 1) * 128], o, [pk], ["vc"])
    S.barrier()


def phase_c(K, io):
    S = K.S
    K.reset()
    qTs = [v3(K.tile(8 * 128, BF16), 8, 128) for _ in range(2)]
    kselT = v3(K.tile(2 * T, BF16), 2, T)
    kwinT = v3(K.tile(2 * T, BF16), 2, T)
    vsel = v3(K.tile(NT * 256, BF16), NT, 256)
    vwin = v3(K.tile(NT * 256, BF16), NT, 256)
    gsig = v3(K.tile(NT * 24, F32), NT, 24)
    Dsel = K.tile(2048, F32)
    Dwin = K.tile(640, F32)
    Dcmp = v3(K.tile(NT * 127, F32), NT, 127)
    rowvalid = K.tile(NT, F32)
    mulmask = v3(K.tile(NT * 32, F32), NT, 32)
    addmask = v3(K.tile(NT * 32, F32), NT, 32)
    overlap = K.tile(32, F32)
    selbias = K.tile(2 * 32, F32)
    s_sel = [K.tile(2048, F32) for _ in range(4)]
    p_sel = [K.tile(2048, BF16) for _ in range(4)]
    pt_sel = [K.tile(2048, BF16) for _ in range(4)]
    s_win = [K.tile(640, F32) for _ in range(4)]
    p_win = [K.tile(640, BF16) for _ in range(4)]
    pt_win = [K.tile(640, BF16) for _ in range(4)]
    stat = [K.tile(8, F32) for _ in range(8)]
    sc = [K.tile(127, F32) for _ in range(4)]
    pc = [K.tile(127, F32) for _ in range(4)]
    pg = [K.tile(128, BF16) for _ in range(4)]
    pnT = [K.tile(128, F32) for _ in range(4)]
    pcT = [K.tile(128, BF16) for _ in range(4)]
    cst = [K.tile(8, F32) for _ in range(4)]
    osb = [K.tile(512, BF16) for _ in range(2)]
    imp2 = K.tile(32, F32)
    mx8 = K.tile(8, F32)
    identf, identb = io["identf"], io["identb"]
    kcT, vc = io["kcT"], io["vc"]
    R4 = range(4)

    s_q3 = io["s_fm"][0:1024, :].rearrange("(h p) t -> p h t", p=128)
    K.dma("q_sp", kselT, io["s_fm"][1536:1792, :].rearrange("(g p) t -> p g t", p=128), [], ["kselT"])
    K.dma("q_sp", kwinT, io["s_fm"][1792:2048, :].rearrange("(g p) t -> p g t", p=128), [], ["kwinT"])
    s_tm3 = io["s_tm"].rearrange("(kt p) c -> p kt c", p=128)
    K.dma("q_sp", vsel, s_tm3[:, :, 0:256], [], ["vsel"])
    K.dma("q_sp", vwin, s_tm3[:, :, 256:512], [], ["vwin"])
    K.dma("q_sp", gsig, io["s_gate"].rearrange("(ti p) c -> p ti c", p=128), [], ["gsig"])
    K.dma("q_sp", Dsel, io["c_dsel"][:, :], [], ["Dsel"])
    K.dma("q_sp", Dwin, io["c_dwin"][:, :], [], ["Dwin"])
    K.dma("q_sp", Dcmp, io["c_dcmp"].rearrange("p (a b) -> p a b", b=127), [], ["Dcmp"])
    K.dma("q_sp", rowvalid, io["c_rowvalid"][:, :], [], ["cm0"])
    K.dma("q_sp", mulmask, io["c_mulmask"].rearrange("p (a b) -> p a b", b=32), [], ["cm1"])
    K.dma("q_sp", addmask, io["c_addmask"].rearrange("p (a b) -> p a b", b=32), [], ["cm2"])
    K.dma("q_sp", overlap[0:127, :], io["c_overlap"][:, :], [], ["cm3"])
    K.op("act", "activation", ["gsig"], ["gsig"], out=gsig, in_=gsig, func=AF.Sigmoid)
    trr = [0]

    def transposes(p_ap, pt_ap, nkt, pk_, ptk):
        for k0 in range(0, nkt, 8):
            n = min(8, nkt - k0)
            bk = trr[0] % 4
            trr[0] += 1
            for j in range(n):
                K.op("pe", "transpose", [pk_, "identb"], [("ps", bk)], out=K.bank(bk, 128, BF16, j * 64),
                     in_=p_ap[:, (k0 + j) * 128:(k0 + j + 1) * 128], identity=identb)
            K.op("act", "activation", [("ps", bk)], [ptk], out=pt_ap[:, k0 * 128:(k0 + n) * 128],
                 in_=K.bank(bk, n * 128, BF16, 0), func=AF.Copy)

    it = 0
    K.dma("q_sp", qTs[0], s_q3[:, :, 0:128], [], [("qT", 0)])
    for qt in range(NT):
        qs = slice(qt * 128, (qt + 1) * 128)
        qT = qTs[qt % 2]
        qk_ = ("qT", qt % 2)
        ql = slice(0, 128)
        if qt + 1 < NT:
            K.dma("q_sp", qTs[(qt + 1) % 2], s_q3[:, :, (qt + 1) * 128:(qt + 2) * 128], [], [("qT", (qt + 1) % 2)])
        io["bg"].emit(13)
        nk = (qt + 1) * 128
        nc_s = (nk + 511) // 512
        off = 1920 - qt * 128
        k0w = max(0, qt * 128 - 512)
        nkw = qt * 128 + 128 - k0w
        nc_w = (nkw + 511) // 512
        coff = k0w - (qt * 128 - 512)
        nb = 2 * (qt + 1)
        for g in range(2):
            H = [g * 4 + hh for hh in R4]
            coefs = [-SLOPES[h] / SCALE for h in H]
            sb = selbias[:, g * 32:(g + 1) * 32]
            for hh in R4:
                K.op("pe", "matmul", [qk_, "kcT"], [("ps", hh)], out=K.bank(hh)[:, 0:127], lhsT=qT[:, H[hh], ql],
                     rhs=kcT[:, g * 128:g * 128 + 127], start=True, stop=True)
            for hh in R4:
                K.op("dve", "scalar_tensor_tensor", ["Dcmp", ("ps", hh)], [("sc", hh)], out=sc[hh], in0=Dcmp[:, qt, :],
                     scalar=coefs[hh], in1=K.bank(hh)[:, 0:127], op0=ALU.mult, op1=ALU.add)
            for hh in R4:
                K.op("dve", "tensor_reduce", [("sc", hh)], [("cst", hh)], out=cst[hh][:, 0:1], in_=sc[hh], axis=AX.X,
                     op=ALU.max)
            for hh in R4:
                K.op("dve", "tensor_scalar", [("cst", hh)], [("cst", hh)], out=cst[hh][:, 1:2], in0=cst[hh][:, 0:1],
                     scalar1=-SCALE, scalar2=None, op0=ALU.mult)
            for hh in R4:
                K.op("act", "activation", [("sc", hh), ("cst", hh)], [("pc", hh), ("cst", hh)], out=pc[hh], in_=sc[hh],
                     func=AF.Exp, scale=SCALE, bias=cst[hh][:, 1:2], accum_out=cst[hh][:, 2:3])
            for hh in R4:
                K.op("dve", "reciprocal", [("cst", hh)], [("cst", hh)], out=cst[hh][:, 3:4], in_=cst[hh][:, 2:3])
            for hh in R4:
                K.op("dve", "tensor_scalar", [("cst", hh), "cm0"], [("cst", hh)], out=cst[hh][:, 4:5],
                     in0=cst[hh][:, 3:4], scalar1=rowvalid[:, qt:qt + 1], scalar2=None, op0=ALU.mult)
            for hh in R4:
                K.op("dve", "tensor_scalar", [("pc", hh), ("cst", hh)], [("pc", hh)], out=pc[hh], in0=pc[hh],
                     scalar1=cst[hh][:, 4:5], scalar2=None, op0=ALU.mult)
            for hh in R4:
                K.op("dve", "tensor_scalar", [("pc", hh), "gsig"], [("pg", hh)], out=pg[hh][:, 0:127], in0=pc[hh],
                     scalar1=gsig[:, qt, H[hh] * 3:H[hh] * 3 + 1], scalar2=None, op0=ALU.mult)
            for hh in R4:
                bk = 4 + hh % 2
                o0 = (hh // 2) * 192
                K.op("pe", "transpose", [("pc", hh), "identf"], [("ps", bk)], out=K.bank(bk, 128, F32, o0)[0:127, :],
                     in_=pc[hh], identity=identf)
                K.op("pe", "transpose", [("pg", hh), "identb"], [("ps", bk)],
                     out=K.bank(bk, 128, BF16, o0 + 128)[0:127, :], in_=pg[hh][:, 0:127], identity=identb)
            for hh in R4:
                bk = 4 + hh % 2
                o0 = (hh // 2) * 192
                K.op("act", "activation", [("ps", bk)], [("pnT", hh)], out=pnT[hh][0:127, :],
                     in_=K.bank(bk, 128, F32, o0)[0:127, :], func=AF.Copy)
                K.op("dve", "tensor_copy", [("ps", bk)], [("pcT", hh)], out=pcT[hh][0:127, :],
                     in_=K.bank(bk, 128, BF16, o0 + 128)[0:127, :])
            for hh in R4:
                K.op("pe", "matmul", [("pnT", hh), "cm3"], [("ps", 6)], out=K.bank(6)[:, 0:32], lhsT=pnT[hh][0:127, :],
                     rhs=overlap[0:127, :], start=(hh == 0), stop=(hh == 3))
            K.op("dve", "tensor_tensor", [("ps", 6), "cm1"], ["imp2"], out=imp2, in0=K.bank(6)[:, 0:32],
                 in1=mulmask[:, qt, :], op=ALU.mult)
            K.op("dve", "tensor_tensor", ["imp2", "cm2"], ["imp2"], out=imp2, in0=imp2, in1=addmask[:, qt, :], op=ALU.add)
            K.op("dve", "max", ["imp2"], ["mx8"], out=mx8, in_=imp2)
            K.op("dve", "tensor_scalar", ["imp2", "mx8"], ["imp2"], out=imp2, in0=imp2, scalar1=mx8[:, 7:8], scalar2=None,
                 op0=ALU.is_ge)
            K.op("dve", "tensor_scalar", ["imp2"], [("selbias", g)], out=sb, in0=imp2, scalar1=-1.0, scalar2=BIG,
                 op0=ALU.add, op1=ALU.mult)

            def qk_sel(hh):
                for c in range(nc_s):
                    w = min(512, nk - c * 512)
                    bk = (hh % 2) * 4 + c
                    K.op("pe", "matmul", [qk_, "kselT"], [("ps", bk)], out=K.bank(bk)[:, 0:w], lhsT=qT[:, H[hh], ql],
                         rhs=kselT[:, g, c * 512:c * 512 + w], start=True, stop=True)

            def p1_sel(hh):
                for c in range(nc_s):
                    w = min(512, nk - c * 512)
                    bk = (hh % 2) * 4 + c
                    K.op("dve", "scalar_tensor_tensor", ["Dsel", ("ps", bk)], [("s_sel", hh)],
                         out=s_sel[hh][:, c * 512:c * 512 + w], in0=Dsel[:, off + c * 512:off + c * 512 + w],
                         scalar=coefs[hh], in1=K.bank(bk)[:, 0:w], op0=ALU.mult, op1=ALU.add)

            def qk_win(hh):
                for c in range(nc_w):
                    w = min(512, nkw - c * 512)
                    bk = hh * 2 + c
                    K.op("pe", "matmul", [qk_, "kwinT"], [("ps", bk)], out=K.bank(bk)[:, 0:w], lhsT=qT[:, H[hh], ql],
                         rhs=kwinT[:, g, k0w + c * 512:k0w + c * 512 + w], start=True, stop=True)

            def p1_win(hh):
                for c in range(nc_w):
                    w = min(512, nkw - c * 512)
                    bk = hh * 2 + c
                    K.op("dve", "scalar_tensor_tensor", ["Dwin", ("ps", bk)], [("s_win", hh)],
                         out=s_win[hh][:, c * 512:c * 512 + w], in0=Dwin[:, coff + c * 512:coff + c * 512 + w],
                         scalar=coefs[hh], in1=K.bank(bk)[:, 0:w], op0=ALU.mult, op1=ALU.add)

            qk_sel(0)
            qk_sel(1)
            p1_sel(0)
            qk_sel(2)
            p1_sel(1)
            qk_sel(3)
            p1_sel(2)
            p1_sel(3)
            for hh in R4:
                ss = s_sel[hh][:, 0:nk]
                K.op("dve", "tensor_tensor", [("s_sel", hh), ("selbias", g)], [("s_sel", hh)], out=v3(ss, nb, 64),
                     in0=v3(ss, nb, 64), in1=sb[:, 0:nb].unsqueeze(2).broadcast_to([128, nb, 64]), op=ALU.add)
            for hh in R4:
                K.op("dve", "tensor_reduce", [("s_sel", hh)], [("stat", hh)], out=stat[hh][:, 0:1],
                     in_=s_sel[hh][:, 0:nk], axis=AX.X, op=ALU.max)
            for hh in R4:
                K.op("dve", "tensor_scalar", [("stat", hh)], [("stat", hh)], out=stat[hh][:, 1:2], in0=stat[hh][:, 0:1],
                     scalar1=-SCALE, scalar2=None, op0=ALU.mult)
            for hh in R4:
                K.op("act", "activation", [("s_sel", hh), ("stat", hh)], [("p_sel", hh), ("stat", hh)],
                     out=p_sel[hh][:, 0:nk], in_=s_sel[hh][:, 0:nk], func=AF.Exp, scale=SCALE, bias=stat[hh][:, 1:2],
                     accum_out=stat[hh][:, 2:3])
            for hh in R4:
                qk_win(hh)
            for hh in R4:
                p1_win(hh)
            for hh in R4:
                K.op("dve", "tensor_reduce", [("s_win", hh)], [("stat", 4 + hh)], out=stat[4 + hh][:, 0:1],
                     in_=s_win[hh][:, 0:nkw], axis=AX.X, op=ALU.max)
            for hh in R4:
                K.op("dve", "tensor_scalar", [("stat", 4 + hh)], [("stat", 4 + hh)], out=stat[4 + hh][:, 1:2],
                     in0=stat[4 + hh][:, 0:1], scalar1=-SCALE, scalar2=None, op0=ALU.mult)
            for hh in R4:
                K.op("act", "activation", [("s_win", hh), ("stat", 4 + hh)], [("p_win", hh), ("stat", 4 + hh)],
                     out=p_win[hh][:, 0:nkw], in_=s_win[hh][:, 0:nkw], func=AF.Exp, scale=SCALE,
                     bias=stat[4 + hh][:, 1:2], accum_out=stat[4 + hh][:, 2:3])
            for (base, pbuf, pname, n_, gi) in ((0, p_sel, "p_sel", nk, 1), (4, p_win, "p_win", nkw, 2)):
                for hh in R4:
                    K.op("dve", "reciprocal", [("stat", base + hh)], [("stat", base + hh)], out=stat[base + hh][:, 3:4],
                         in_=stat[base + hh][:, 2:3])
                for hh in R4:
                    K.op("dve", "tensor_scalar", [("stat", base + hh), "gsig"], [("stat", base + hh)],
                         out=stat[base + hh][:, 4:5], in0=stat[base + hh][:, 3:4],
                         scalar1=gsig[:, qt, H[hh] * 3 + gi:H[hh] * 3 + gi + 1], scalar2=None, op0=ALU.mult)
                for hh in R4:
                    K.op("dve", "tensor_scalar", [(pname, hh), ("stat", base + hh)], [(pname, hh)],
                         out=pbuf[hh][:, 0:n_], in0=pbuf[hh][:, 0:n_], scalar1=stat[base + hh][:, 4:5], scalar2=None,
                         op0=ALU.mult)
            for hh in R4:
                transposes(p_sel[hh], pt_sel[hh], qt + 1, ("p_sel", hh), ("pt_sel", hh))
            for hh in R4:
                transposes(p_win[hh], pt_win[hh], nkw // 128, ("p_win", hh), ("pt_win", hh))
            ob = it % 2
            it += 1
            for hh in R4:
                bk = 4 + hh
                o = K.bank(bk)[:, 0:128]
                nmm = 1 + (qt + 1) + nkw // 128
                i = 1
                K.op("pe", "matmul", ["vc", ("pcT", hh)], [("ps", bk)], out=o, lhsT=vc[0:127, g * 128:(g + 1) * 128],
                     rhs=pcT[hh][0:127, :], start=True, stop=False)
                for kt in range(qt + 1):
                    i += 1
                    K.op("pe", "matmul", ["vsel", ("pt_sel", hh)], [("ps", bk)], out=o,
                         lhsT=vsel[:, kt, g * 128:(g + 1) * 128], rhs=pt_sel[hh][:, kt * 128:(kt + 1) * 128],
                         start=False, stop=False)
                for j in range(nkw // 128):
                    i += 1
                    K.op("pe", "matmul", ["vwin", ("pt_win", hh)], [("ps", bk)], out=o,
                         lhsT=vwin[:, k0w // 128 + j, g * 128:(g + 1) * 128], rhs=pt_win[hh][:, j * 128:(j + 1) * 128],
                         start=False, stop=(i == nmm))
                K.evac(osb[ob][:, hh * 128:(hh + 1) * 128], o, [("ps", bk)], [("osb", ob)])
            K.dma("q_sp", io["s_mixT"][g * 512:(g + 1) * 512, qs].rearrange("(h p) t -> p h t", p=128),
                  v3(osb[ob], 4, 128), [("osb", ob)], [("s_mixT", qt, g)])
    S.barrier()


def phase_d(K, io):
    S = K.S
    K.reset()
    gqk = [v3(K.tile(8 * 128, BF16), 8, 128) for _ in range(2)]
    gv = v3(K.tile(NT * 1024, BF16), NT, 1024)
    og = v3(K.tile(NT * 1024, BF16), NT, 1024)
    glaT = v3(K.tile(8 * T, BF16), 8, T)
    alphaT = K.tile(T, F32)
    wa2 = K.tile(512, F32)
    ba = K.tile(512, F32)
    ones = K.tile(128, F32)
    tris = K.tile(128, F32)
    mask01 = K.tile(128, F32)
    gnb = K.tile(256, F32)
    st = v3(K.tile(4 * 256, F32), 4, 256)
    stb = v3(K.tile(4 * 256, BF16), 4, 256)
    ex2 = [K.tile(512, F32) for _ in range(2)]
    lg2 = [K.tile(512, F32) for _ in range(2)]
    eb2 = [K.tile(512, F32) for _ in range(2)]
    enb2 = [K.tile(512, F32) for _ in range(2)]
    QeT2 = [K.tile(512, BF16) for _ in range(2)]
    KeT2 = [K.tile(512, BF16) for _ in range(2)]
    ATm2 = [K.tile(512, BF16) for _ in range(2)]
    Ketm2 = [K.tile(512, BF16) for _ in range(2)]
    t12 = [K.tile(1024, F32) for _ in range(2)]
    sog2 = [K.tile(1024, F32) for _ in range(2)]
    gtm2 = [K.tile(1024, BF16) for _ in range(2)]
    junk = K.tile(256, BF16)
    rs2 = [K.tile(16, F32) for _ in range(2)]
    s1 = K.tile(256, F32)
    identb = io["identb"]

    s_gqk3 = io["s_fm"][2048:3072, :].rearrange("(h p) t -> p h t", p=128)
    K.dma("q_sp", gqk[0], s_gqk3[:, :, 0:128], [], [("gqk", 0)])
    s_tm3 = io["s_tm"].rearrange("(kt p) c -> p kt c", p=128)
    K.dma("q_sp", gv, s_tm3[:, :, 512:1536], [], ["gv"])
    K.dma("q_sp", og, s_tm3[:, :, 1536:2560], [], ["og"])
    K.dma("q_sp", alphaT[0:16, :], io["s_alphaT"][:, :], [], ["alphaT"])
    K.dma("q_sp", wa2[0:16, :], io["w_alpha2"][:, :], [], ["wa2"])
    K.dma("q_sp", ba[0:1, :], io["b_alpha"].rearrange("(a n) -> a n", a=1), [], ["ba"])
    K.dma("q_sp", tris, io["c_tris"][:, :], [], ["tris"])
    K.dma("q_sp", mask01, io["c_mask01"][:, :], [], ["mask01"])
    K.dma("q_sp", gnb, io["g_gla"].partition_broadcast(128), [], ["gnb"])
    K.op("dve", "memset", [], ["ones"], ap=ones, constant=1.0)
    K.op("dve", "memset", [], ["st"], ap=st, constant=0.0)
    K.op("pool", "memset", [], ["stb"], ap=stb, constant=0.0)

    def front(ti):
        ts_ = slice(ti * 128, (ti + 1) * 128)
        gqT = gqk[ti % 2][:, 0:4, :]
        gkT = gqk[ti % 2][:, 4:8, :]
        gkey = ("gqk", ti % 2)
        pq = ti % 2
        ex, lg, eb, enb, QeT, KeT, ATm, Ketm = ex2[pq], lg2[pq], eb2[pq], enb2[pq], QeT2[pq], KeT2[pq], ATm2[pq], Ketm2[pq]
        t1, sog, gtm, rs = t12[pq], sog2[pq], gtm2[pq], rs2[pq]
        if ti + 1 < NT:
            K.dma("q_sp", gqk[(ti + 1) % 2], s_gqk3[:, :, (ti + 1) * 128:(ti + 2) * 128], [], [("gqk", (ti + 1) % 2)])
        io["bg"].emit(3)
        K.op("pe", "matmul", ["alphaT", "wa2"], [("ps", 0)], out=K.bank(0), lhsT=alphaT[0:16, ts_], rhs=wa2[0:16, :],
             start=True, stop=False)
        K.op("pe", "matmul", ["ones", "ba"], [("ps", 0)], out=K.bank(0), lhsT=ones[0:1, :], rhs=ba[0:1, :],
             start=False, stop=True)
        K.op("act", "activation", [("ps", 0)], [("ex", pq)], out=ex, in_=K.bank(0), func=AF.Exp, scale=-1.0)
        K.op("act", "activation", [("ex", pq)], [("lg", pq)], out=lg, in_=ex, func=AF.Ln, bias=1.0)
        for h in range(4):
            K.op("pe", "matmul", [("lg", pq), "tris"], [("ps", 1)], out=K.bank(1)[:, h * 128:(h + 1) * 128],
                 lhsT=lg[:, h * 128:(h + 1) * 128], rhs=tris, start=True, stop=True)
        K.op("act", "activation", [("ps", 1)], [("eb", pq)], out=eb, in_=K.bank(1), func=AF.Exp)
        K.op("act", "activation", [("ps", 1)], [("enb", pq)], out=enb, in_=K.bank(1), func=AF.Exp, scale=-1.0)
        K.op("dve", "scalar_tensor_tensor", [("eb", pq), gkey], [("QeT", pq)], out=v3(QeT, 4, 128), in0=v3(eb, 4, 128), scalar=SCALE,
             in1=gqT, op0=ALU.mult, op1=ALU.mult)
        K.op("dve", "tensor_tensor", [("enb", pq), gkey], [("KeT", pq)], out=v3(KeT, 4, 128), in0=v3(enb, 4, 128), in1=gkT,
             op=ALU.mult)
        for h in range(4):
            K.op("pe", "matmul", [("KeT", pq), ("QeT", pq)], [("ps", 2)], out=K.bank(2)[:, h * 128:(h + 1) * 128],
                 lhsT=KeT[:, h * 128:(h + 1) * 128], rhs=QeT[:, h * 128:(h + 1) * 128], start=True, stop=True)
        K.op("dve", "tensor_tensor", [("ps", 2), "mask01"], [("ATm", pq)], out=v3(ATm, 4, 128), in0=v3(K.bank(2), 4, 128),
             in1=mask01.unsqueeze(1).broadcast_to([128, 4, 128]), op=ALU.mult)
        for h in range(4):
            K.op("pe", "transpose", [("KeT", pq), "identb"], [("ps", 5)], out=K.bank(5, 128, BF16, h * 64),
                 in_=KeT[:, h * 128:(h + 1) * 128], identity=identb)
        K.op("act", "activation", [("ps", 5)], [("Ketm", pq)], out=Ketm, in_=K.bank(5, 512, BF16, 0), func=AF.Copy)


    def mid(ti):
        ts_ = slice(ti * 128, (ti + 1) * 128)
        pq = ti % 2
        ex, lg, eb, enb, QeT, KeT, ATm, Ketm = ex2[pq], lg2[pq], eb2[pq], enb2[pq], QeT2[pq], KeT2[pq], ATm2[pq], Ketm2[pq]
        t1, sog, gtm, rs = t12[pq], sog2[pq], gtm2[pq], rs2[pq]
        for h in range(4):
            bk = 3 + h // 2
            o = K.bank(bk)[:, (h % 2) * 256:(h % 2) * 256 + 256]
            K.op("pe", "matmul", [("ATm", pq), "gv"], [("ps", bk)], out=o, lhsT=ATm[:, h * 128:(h + 1) * 128],
                 rhs=gv[:, ti, h * 256:(h + 1) * 256], start=True, stop=False)
            K.op("pe", "matmul", [("QeT", pq), "stb"], [("ps", bk)], out=o, lhsT=QeT[:, h * 128:(h + 1) * 128],
                 rhs=stb[:, h, :], start=False, stop=True)
        for h in range(4):
            bk = 6 + h // 2
            o = K.bank(bk)[:, (h % 2) * 256:(h % 2) * 256 + 256]
            K.op("pe", "matmul", [("Ketm", pq), "gv"], [("ps", bk)], out=o, lhsT=Ketm[:, h * 128:(h + 1) * 128],
                 rhs=gv[:, ti, h * 256:(h + 1) * 256], start=True, stop=True)
            ebl = eb[:, h * 128 + 127:h * 128 + 128]
            K.op("dve", "tensor_scalar", ["st", ("eb", pq)], ["s1"], out=s1, in0=st[:, h, :], scalar1=ebl, scalar2=None,
                 op0=ALU.mult)
            K.op("dve", "scalar_tensor_tensor", [("ps", bk), ("eb", pq), "s1"], ["st"], out=st[:, h, :], in0=o, scalar=ebl,
                 in1=s1, op0=ALU.mult, op1=ALU.add)
        K.op("act", "activation", ["st"], ["stb"], out=stb, in_=st, func=AF.Copy)


    def outp(ti):
        ts_ = slice(ti * 128, (ti + 1) * 128)
        pq = ti % 2
        ex, lg, eb, enb, QeT, KeT, ATm, Ketm = ex2[pq], lg2[pq], eb2[pq], enb2[pq], QeT2[pq], KeT2[pq], ATm2[pq], Ketm2[pq]
        t1, sog, gtm, rs = t12[pq], sog2[pq], gtm2[pq], rs2[pq]
        for h in range(4):
            bk = 3 + h // 2
            o = K.bank(bk)[:, (h % 2) * 256:(h % 2) * 256 + 256]
            K.op("act", "activation", [("ps", bk)], ["junk", ("rs", pq)], out=junk, in_=o, func=AF.Square,
                 accum_out=rs[:, h:h + 1])
        K.op("dve", "tensor_scalar", [("rs", pq)], [("rs", pq)], out=rs[:, 4:8], in0=rs[:, 0:4], scalar1=1.0 / 256, scalar2=EPS,
             op0=ALU.mult, op1=ALU.add)
        K.op("act", "activation", [("rs", pq)], [("rs", pq)], out=rs[:, 8:12], in_=rs[:, 4:8], func=AF.Sqrt)
        K.op("dve", "reciprocal", [("rs", pq)], [("rs", pq)], out=rs[:, 12:16], in_=rs[:, 8:12])
        K.op("act", "activation", ["og"], [("sog", pq)], out=sog, in_=og[:, ti, :], func=AF.Silu)
        for h in range(4):
            bk = 3 + h // 2
            o = K.bank(bk)[:, (h % 2) * 256:(h % 2) * 256 + 256]
            K.op("dve", "scalar_tensor_tensor", [("ps", bk), ("rs", pq), "gnb"], [("t1", pq)], out=t1[:, h * 256:(h + 1) * 256], in0=o,
                 scalar=rs[:, 12 + h:13 + h], in1=gnb, op0=ALU.mult, op1=ALU.mult)
        K.op("dve", "tensor_tensor", [("t1", pq), ("sog", pq)], [("gtm", pq)], out=gtm, in0=t1, in1=sog, op=ALU.mult)
        for fc in range(8):
            K.op("pe", "transpose", [("gtm", pq), "identb"], [("ps", 5)], out=K.bank(5, 128, BF16, fc * 64),
                 in_=gtm[:, fc * 128:(fc + 1) * 128], identity=identb)
        K.evac(glaT[:, :, ts_], v3(K.bank(5, 1024, BF16, 0), 8, 128), [("ps", 5)], [("glaT", ti)])

    front(0)
    for ti in range(NT):
        mid(ti)
        if ti + 1 < NT:
            front(ti + 1)
        outp(ti)
    gk = [("glaT", ti) for ti in range(NT)]
    for fc in range(8):
        K.dma("q_sp", io["s_mixT"][1024 + fc * 128:1024 + (fc + 1) * 128, :], glaT[:, fc, :], gk, [("s_mixT", 8 + fc)])
    S.barrier()


def phase_e(K, io):
    S = K.S
    K.reset()
    mixs = [v3(K.tile(KC * 128, BF16), KC, 128) for _ in range(2)]
    wout = v3(K.tile(KC * D, BF16), KC, D)
    xt = [K.tile(D, F32) for _ in range(2)]
    xn = [K.tile(D, F32) for _ in range(2)]
    junk = K.tile(D, BF16)
    hTf = [K.tile(KC * 128, F32) for _ in range(2)]
    wr = v3(K.tile(KC * 36, F32), KC, 36)
    brt = K.tile(36, F32)
    lgt = [K.tile(36, F32) for _ in range(2)]
    sm = [K.tile(8, F32) for _ in range(2)]
    sr = [K.tile(24, F32) for _ in range(2)]
    oh = [K.tile(4, F32) for _ in range(2)]
    esel = [K.tile(8, F32) for _ in range(2)]
    exs = [K.tile(8, F32) for _ in range(2)]
    msk = [K.tile(8, F32) for _ in range(2)]
    mx8 = [K.tile(8, F32) for _ in range(2)]
    C3 = v3(io["C"], NT, 32)
    gffn, identf = io["gffn"], io["identf"]
    gfb = K.tile(D, F32)
    h2t = [K.tile(D, BF16) for _ in range(2)]
    K.dma("q_sp", gfb, io["g_ffn"].partition_broadcast(128), [], ["gfb"])
    s_mix3 = io["s_mixT"].rearrange("(kc p) t -> p kc t", p=128)
    K.dma("q_sp", mixs[0], s_mix3[:, :, 0:128], [], [("mix", 0)])
    w3 = io["w_out"].rearrange("(kc p) c -> p kc c", p=128)
    for j in range(4):
        K.dma("q_pool", wout[:, j * 4:(j + 1) * 4, :], w3[:, j * 4:(j + 1) * 4, :], [], [("wout", j)])
    wk = [("wout", j) for j in range(4)]
    K.dma("q_sp", wr[:, :, 0:4], io["w_rg"].rearrange("(kc p) c -> p kc c", p=128), [], ["wr0"])
    K.dma("q_sp", wr[:, :, 4:36], io["w_re"].rearrange("(kc p) c -> p kc c", p=128), [], ["wr1"])
    K.dma("q_sp", brt[:, 0:4], io["b_rg"].partition_broadcast(128), [], ["br0"])
    K.dma("q_sp", brt[:, 4:36], io["b_re"].partition_broadcast(128), [], ["br1"])

    def stage1(ti):
        b = ti % 2
        ts_ = slice(ti * 128, (ti + 1) * 128)
        xk, smk = ("xt", b), ("sm", b)
        K.dma("q_sp", xt[b], io["x"][ts_, :], [], [xk])
        if ti + 1 < NT:
            K.dma("q_sp", mixs[(ti + 1) % 2], s_mix3[:, :, (ti + 1) * 128:(ti + 2) * 128], [], [("mix", (ti + 1) % 2)])
        io["bg"].emit(2)
        for c in range(4):
            for kc in range(KC):
                K.op("pe", "matmul", [("mix", b)] + wk, [("ps", c)], out=K.bank(c), lhsT=mixs[b][:, kc, :],
                     rhs=wout[:, kc, c * 512:(c + 1) * 512], start=(kc == 0), stop=(kc == KC - 1))
        for c in range(4):
            K.op("dve", "tensor_tensor", [("ps", c), xk], [xk], out=xt[b][:, c * 512:(c + 1) * 512], in0=K.bank(c),
                 in1=xt[b][:, c * 512:(c + 1) * 512], op=ALU.add)
        K.dma("q_sp", io["s_x1"][ts_, :], xt[b], [xk], [("s_x1", ti)])
        K.op("act", "activation", [xk], ["junk", smk], out=junk, in_=xt[b], func=AF.Square, accum_out=sm[b][:, 0:1])
        K.op("dve", "tensor_scalar", [smk], [smk], out=sm[b][:, 1:2], in0=sm[b][:, 0:1], scalar1=1.0 / D, scalar2=EPS,
             op0=ALU.mult, op1=ALU.add)
        K.op("act", "activation", [smk], [smk], out=sm[b][:, 2:3], in_=sm[b][:, 1:2], func=AF.Sqrt)
        K.op("dve", "reciprocal", [smk], [smk], out=sm[b][:, 3:4], in_=sm[b][:, 2:3])
        K.op("dve", "tensor_scalar", [xk, smk], [("xn", b)], out=xn[b], in0=xt[b], scalar1=sm[b][:, 3:4], scalar2=None,
             op0=ALU.mult)
        K.op("pool", "tensor_tensor", [("xn", b), "gfb"], [("h2t", b)], out=h2t[b], in0=xn[b], in1=gfb, op=ALU.mult)
        K.dma("q_sp", io["s_h2"][ts_, :], h2t[b], [("h2t", b)], [("s_h2", ti)])

    def stage2(ti):
        b = ti % 2
        hk, lk, rk = ("hTf", b), ("lgt", b), ("sr", b)
        r = sr[b]
        for q4 in range(4):
            bk = 4 + q4
            for j in range(4):
                kc = q4 * 4 + j
                K.op("pe", "transpose", [("xn", b), "identf"], [("ps", bk)], out=K.bank(bk, 128, F32, j * 128),
                     in_=xn[b][:, kc * 128:(kc + 1) * 128], identity=identf)
            for j in range(4):
                kc = q4 * 4 + j
                K.op("act", "activation", [("ps", bk), "gffn"], [hk], out=hTf[b][:, kc * 128:(kc + 1) * 128],
                     in_=K.bank(bk, 128, F32, j * 128), func=AF.Copy, scale=gffn[:, kc:kc + 1])
        for kc in range(KC):
            K.op("pe", "matmul", [hk, "wr0", "wr1"], [("ps", 4 + b)], out=K.bank(4 + b)[:, 0:36],
                 lhsT=hTf[b][:, kc * 128:(kc + 1) * 128], rhs=wr[:, kc, :], start=(kc == 0), stop=(kc == KC - 1))
        K.op("dve", "tensor_tensor", [("ps", 4 + b), "br0", "br1"], [lk], out=lgt[b], in0=K.bank(4 + b)[:, 0:36], in1=brt,
             op=ALU.add)
        L = lgt[b]
        K.op("dve", "tensor_reduce", [lk], [rk], out=r[:, 8:9], in_=L[:, 0:4], axis=AX.X, op=ALU.max)
        K.op("dve", "tensor_scalar", [rk], [rk], out=r[:, 9:10], in0=r[:, 8:9], scalar1=-1.0, scalar2=None, op0=ALU.mult)
        K.op("act", "activation", [lk, rk], [("oh", b), rk], out=oh[b], in_=L[:, 0:4], func=AF.Exp, bias=r[:, 9:10],
             accum_out=r[:, 10:11])
        K.op("dve", "tensor_scalar", [lk, rk], [("oh", b)], out=oh[b], in0=L[:, 0:4], scalar1=r[:, 8:9], scalar2=None,
             op0=ALU.is_ge)
        K.op("dve", "tensor_scalar", [lk, ("oh", b)], [("esel", b)], out=esel[b], in0=L[:, 4:12], scalar1=oh[b][:, 0:1],
             scalar2=None, op0=ALU.mult)
        for g in range(1, 4):
            K.op("dve", "scalar_tensor_tensor", [lk, ("oh", b), ("esel", b)], [("esel", b)], out=esel[b],
                 in0=L[:, 4 + 8 * g:12 + 8 * g], scalar=oh[b][:, g:g + 1], in1=esel[b], op0=ALU.mult, op1=ALU.add)
        K.op("dve", "max", [("esel", b)], [("mx8", b)], out=mx8[b], in_=esel[b])
        K.op("dve", "tensor_scalar", [("mx8", b)], [rk], out=r[:, 12:13], in0=mx8[b][:, 0:1], scalar1=-1.0, scalar2=None,
             op0=ALU.mult)
        K.op("act", "activation", [("esel", b), rk], [("exs", b)], out=exs[b], in_=esel[b], func=AF.Exp, bias=r[:, 12:13])
        K.op("dve", "tensor_scalar", [("esel", b), ("mx8", b)], [("msk", b)], out=msk[b], in0=esel[b],
             scalar1=mx8[b][:, 1:2], scalar2=None, op0=ALU.is_ge)
        K.op("act", "activation", [("mx8", b), rk], [rk], out=r[:, 13:14], in_=mx8[b][:, 1:2], func=AF.Exp,
             bias=r[:, 12:13])
        K.op("dve", "tensor_scalar", [rk], [rk], out=r[:, 14:15], in0=r[:, 13:14], scalar1=1.0, scalar2=r[:, 10:11],
             op0=ALU.add, op1=ALU.mult)
        K.op("dve", "reciprocal", [rk], [rk], out=r[:, 15:16], in_=r[:, 14:15])
        K.op("dve", "tensor_scalar", [("oh", b), rk], [rk], out=r[:, 16:20], in0=oh[b], scalar1=r[:, 15:16], scalar2=None,
             op0=ALU.mult)
        for g in range(4):
            K.op("dve", "scalar_tensor_tensor", [("exs", b), rk, ("msk", b)], ["C"], out=C3[:, ti, g * 8:(g + 1) * 8],
                 in0=exs[b], scalar=r[:, 16 + g:17 + g], in1=msk[b], op0=ALU.mult, op1=ALU.mult)

    stage1(0)
    for ti in range(NT):
        if ti + 1 < NT:
            stage1(ti + 1)
        stage2(ti)
    S.barrier()


def phase_f(K, io):
    S = K.S
    C3 = v3(io["C"], NT, 32)
    for half in range(2):
        K.reset()
        h2T = v3(K.tile(KC * 1024, BF16), KC, 1024)
        acc = v3(K.tile(8 * D, F32), 8, D)
        hid = [v3(K.tile(2 * 1024, BF16), 2, 1024) for _ in range(2)]
        sg = [K.tile(512, BF16) for _ in range(2)]
        mark = K.off
        wg = [v3(K.tile(KC * 256, BF16), KC, 256) for _ in range(2)]
        wu = [v3(K.tile(KC * 256, BF16), KC, 256) for _ in range(2)]
        wd = [v3(K.tile(2 * D, BF16), 2, D) for _ in range(2)]
        K.dma("q_sp", h2T, io["s_h2T"].rearrange("(kc p) t -> p kc t", p=128)[:, :, half * 1024:(half + 1) * 1024],
              [], ["h2T"])
        K.op("dve", "memset", [], ["acc%d" % i for i in range(8)], ap=acc, constant=0.0)
        units = [(e, fh) for e in range(32) for fh in range(2)]
        gbc = [0]

        def load_gu(u):
            e, fh = units[u]
            g, ee = e // 8, e % 8
            wb = u % 2
            fs = slice(fh * 256, (fh + 1) * 256)
            K.dma("q_pool", wg[wb], io["w_eg"][g, ee].rearrange("(kc p) f -> p kc f", p=128)[:, :, fs], [], [("wg", wb)])
            K.dma("q_pool", wu[wb], io["w_eu"][g, ee].rearrange("(kc p) f -> p kc f", p=128)[:, :, fs], [], [("wu", wb)])

        def load_d(u):
            e, fh = units[u]
            g, ee = e // 8, e % 8
            wb = u % 2
            fs = slice(fh * 256, (fh + 1) * 256)
            K.dma("q_pool", wd[wb], io["w_ed"][g, ee][fs, :].rearrange("(fc p) d -> p fc d", p=128), [], [("wd", wb)])

        def gate_up_piece(u, piece):
            wb = u % 2
            fc, tb = piece // 2, piece % 2
            b0 = (gbc[0] % 2) * 2
            gbc[0] += 1
            for (bk, w, wkey) in ((b0, wg[wb], ("wg", wb)), (b0 + 1, wu[wb], ("wu", wb))):
                for kc in range(KC):
                    K.op("pe", "matmul", [wkey, "h2T"], [("ps", bk)], out=K.bank(bk),
                         lhsT=w[:, kc, fc * 128:(fc + 1) * 128], rhs=h2T[:, kc, tb * 512:(tb + 1) * 512],
                         start=(kc == 0), stop=(kc == KC - 1))
            sb_ = gbc[0] % 2
            K.op("act", "activation", [("ps", b0)], [("sg", sb_)], out=sg[sb_], in_=K.bank(b0), func=AF.Silu)
            K.op("dve", "tensor_tensor", [("ps", b0 + 1), ("sg", sb_)], [("hid", wb, fc, tb)],
                 out=hid[wb][:, fc, tb * 512:(tb + 1) * 512], in0=K.bank(b0 + 1), in1=sg[sb_], op=ALU.mult)

        def down_tile(u, ti):
            e, fh = units[u]
            wb = u % 2
            hk = [("hid", wb, fc, ti // 4) for fc in range(2)]
            for c in range(4):
                bk = 4 + c
                for fc in range(2):
                    K.op("pe", "matmul", hk + [("wd", wb)], [("ps", bk)], out=K.bank(bk),
                         lhsT=hid[wb][:, fc, ti * 128:(ti + 1) * 128], rhs=wd[wb][:, fc, c * 512:(c + 1) * 512],
                         start=(fc == 0), stop=(fc == 1))
                K.op("dve", "scalar_tensor_tensor", [("ps", bk), "C", "acc%d" % ti], ["acc%d" % ti],
                     out=acc[:, ti, c * 512:(c + 1) * 512], in0=K.bank(bk),
                     scalar=C3[:, half * 8 + ti, e:e + 1], in1=acc[:, ti, c * 512:(c + 1) * 512],
                     op0=ALU.mult, op1=ALU.add)

        nu = len(units)
        load_gu(0)
        load_d(0)
        load_gu(1)
        load_d(1)
        for p in range(4):
            gate_up_piece(0, p)
        for u in range(nu):
            if u + 2 < nu:
                load_gu(u + 2)
            for ti in range(8):
                down_tile(u, ti)
                if ti % 2 == 1 and u + 1 < nu:
                    gate_up_piece(u + 1, ti // 2)
            if u + 2 < nu:
                load_d(u + 2)
        S.barrier()
        K.off = mark
        gfin = K.tile(D, F32)
        x1t = [K.tile(D, F32) for _ in range(2)]
        junk = K.tile(D, BF16)
        sm = [K.tile(8, F32) for _ in range(2)]
        K.dma("q_sp", gfin, io["g_final"].partition_broadcast(128), [], ["gfin"])
        for ti in range(8):
            b = ti % 2
            tg = half * 8 + ti
            xk, sk = ("x1t", b), ("smf", b)
            K.dma("q_sp", x1t[b], io["s_x1"][tg * 128:(tg + 1) * 128, :], [], [xk])
            K.op("dve", "tensor_tensor", [xk, "acc%d" % ti], [xk], out=x1t[b], in0=x1t[b], in1=acc[:, ti, :], op=ALU.add)
            K.op("act", "activation", [xk], ["junkf", sk], out=junk, in_=x1t[b], func=AF.Square, accum_out=sm[b][:, 0:1])
            K.op("dve", "tensor_scalar", [sk], [sk], out=sm[b][:, 1:2], in0=sm[b][:, 0:1], scalar1=1.0 / D, scalar2=EPS,
                 op0=ALU.mult, op1=ALU.add)
            K.op("act", "activation", [sk], [sk], out=sm[b][:, 2:3], in_=sm[b][:, 1:2], func=AF.Sqrt)
            K.op("dve", "reciprocal", [sk], [sk], out=sm[b][:, 3:4], in_=sm[b][:, 2:3])
            K.op("dve", "scalar_tensor_tensor", [xk, sk, "gfin"], [xk], out=x1t[b], in0=x1t[b], scalar=sm[b][:, 3:4],
                 in1=gfin, op0=ALU.mult, op1=ALU.mult)
            K.dma("q_sp", io["y"][tg * 128:(tg + 1) * 128, :], x1t[b], [xk], [("y", tg)])
        S.barrier()


I32 = mybir.dt.int32
NTILE = 48
TS = 256
NSLOT = NTILE * TS


class BgCast:
    def __init__(self, K, io, stg):
        self.K, self.io, self.stg = K, io, stg
        self.jobs = []
        for e in range(32):
            g, ee = e // 8, e % 8
            for (src, dst, kind) in ((io["w_eg"], io["s_wgb"], 0), (io["w_eu"], io["s_wub"], 0), (io["w_ed"], io["s_wdb"], 1)):
                for q in range(4):
                    self.jobs.append((src, dst, kind, e, g, ee, q))
        self.i = 0

    def emit(self, n):
        K = self.K
        for _ in range(n):
            if self.i >= len(self.jobs):
                return
            src, dst, kind, e, g, ee, q = self.jobs[self.i]
            b = self.i % len(self.stg)
            self.i += 1
            st = self.stg[b]
            if kind == 0:
                K.dma("q_pool", v3(st, 4, 512), src[g, ee].rearrange("(kc p) f -> p kc f", p=128)[:, q * 4:(q + 1) * 4, :],
                      [], [("stg", b)])
            else:
                K.dma("q_pool", st, src[g, ee][q * 128:(q + 1) * 128, :], [], [("stg", b)])
            K.dma("q_sp", dst[e * 128:(e + 1) * 128, q * 2048:(q + 1) * 2048], st, [("stg", b)], [("wb", self.i)])


def phase_w(K, io):
    io["bg"].emit(100000)
    K.S.barrier()


def phase_e2(K, io):
    S = K.S
    K.reset()
    C3 = v3(io["C"], NT, 32)
    Cf = io["C"]
    Mnz = K.tile(512, F32)
    M1 = K.tile(512, F32)
    M2 = K.tile(512, F32)
    slotmat = K.tile(512, F32)
    tmp = K.tile(512, F32)
    ones = K.tile(128, F32)
    stri = K.tile(128, F32)
    thr = K.tile(8, F32)
    jidx = K.tile(NTILE, F32)
    pcol = K.tile(1, F32)
    n_ = K.tile(32, F32)
    cmp3 = K.tile(256, F32)
    tiles = K.tile(32, F32)
    one32 = K.tile(32, F32)
    tend = K.tile(32, F32)
    tbase = K.tile(32, F32)
    rowmax = K.tile(NT, F32)
    sl = K.tile(2 * NT, F32)
    cmpj = K.tile(NTILE * 32, F32)
    eid = K.tile(NTILE, F32)
    widf = K.tile(NTILE, F32)
    K.dma("q_sp", stri, io["c_stri"][:, :], [], ["stri"])
    K.dma("q_sp", thr, io["c_thr"][:, :], [], ["thr"])
    K.dma("q_sp", jidx, io["c_jidx"][:, :], [], ["jidx"])
    K.dma("q_sp", pcol, io["c_pcol"][:, :], [], ["pcol"])
    K.op("dve", "memset", [], ["ones"], ap=ones, constant=1.0)
    K.op("dve", "memset", [], ["one32"], ap=one32, constant=1.0)
    K.op("dve", "tensor_scalar", ["C"], ["Mnz"], out=Mnz, in0=Cf, scalar1=0.0, scalar2=None, op0=ALU.is_gt)
    Mnz3 = v3(Mnz, NT, 32)
    for ti in range(NT):
        o = K.bank(0)[:, ti * 32:(ti + 1) * 32]
        K.op("pe", "matmul", ["stri", "Mnz"], [("ps", 0)], out=o, lhsT=stri, rhs=Mnz3[:, ti, :], start=True, stop=(ti == 0))
        for tj in range(ti):
            K.op("pe", "matmul", ["ones", "Mnz"], [("ps", 0)], out=o, lhsT=ones, rhs=Mnz3[:, tj, :], start=False,
                 stop=(tj == ti - 1))
    for ti in range(NT):
        K.op("pe", "matmul", ["ones", "Mnz"], [("ps", 1)], out=K.bank(1)[:, 0:32], lhsT=ones, rhs=Mnz3[:, ti, :],
             start=(ti == 0), stop=(ti == NT - 1))
    K.op("dve", "tensor_copy", [("ps", 1)], ["n"], out=n_, in_=K.bank(1)[:, 0:32])
    K.op("dve", "tensor_tensor", ["n", "thr"], ["cmp3"], out=v3(cmp3, 32, 8), in0=n_.unsqueeze(2).broadcast_to([128, 32, 8]),
         in1=thr.unsqueeze(1).broadcast_to([128, 32, 8]), op=ALU.is_gt)
    K.op("dve", "tensor_reduce", ["cmp3"], ["tiles"], out=tiles, in_=v3(cmp3, 32, 8), axis=AX.X, op=ALU.add)
    K.op("dve", "tensor_tensor_scan", ["tiles", "one32"], ["tend"], out=tend, data0=one32, data1=tiles, initial=0.0,
         op0=ALU.mult, op1=ALU.add)
    K.op("dve", "tensor_tensor", ["tend", "tiles"], ["tbase"], out=tbase, in0=tend, in1=tiles, op=ALU.subtract)
    K.op("dve", "scalar_tensor_tensor", ["tbase", ("ps", 0)], ["slotmat"], out=v3(slotmat, NT, 32),
         in0=tbase.unsqueeze(1).broadcast_to([128, NT, 32]), scalar=float(TS), in1=v3(K.bank(0), NT, 32),
         op0=ALU.mult, op1=ALU.add)
    K.op("dve", "tensor_reduce", ["C"], ["rowmax"], out=rowmax, in_=C3, axis=AX.X, op=ALU.max)
    K.op("dve", "tensor_tensor", ["C", "rowmax"], ["M1"], out=v3(M1, NT, 32), in0=C3,
         in1=rowmax.unsqueeze(2).broadcast_to([128, NT, 32]), op=ALU.is_ge)
    K.op("dve", "tensor_tensor", ["Mnz", "M1"], ["M2"], out=M2, in0=Mnz, in1=M1, op=ALU.subtract)
    W2 = io["w12"]
    K.op("dve", "tensor_copy", ["rowmax"], ["w12"], out=W2[:, 0:NT], in_=rowmax)
    for (m, mk, k) in ((M1, "M1", 0), (M2, "M2", 1)):
        K.op("dve", "tensor_tensor", [mk, "slotmat"], ["tmp"], out=tmp, in0=m, in1=slotmat, op=ALU.mult)
        K.op("dve", "tensor_reduce", ["tmp"], ["sl"], out=sl[:, k * NT:(k + 1) * NT], in_=v3(tmp, NT, 32), axis=AX.X,
             op=ALU.add)
    K.op("dve", "tensor_tensor", ["M2", "C"], ["tmp"], out=tmp, in0=M2, in1=Cf, op=ALU.mult)
    K.op("dve", "tensor_reduce", ["tmp"], ["w12"], out=W2[:, NT:2 * NT], in_=v3(tmp, NT, 32), axis=AX.X, op=ALU.add)
    K.op("dve", "tensor_copy", ["sl"], ["slot"], out=io["slot"], in_=sl)
    K.op("dve", "tensor_tensor", ["tend", "jidx"], ["cmpj"], out=v3(cmpj, NTILE, 32),
         in0=tend.unsqueeze(1).broadcast_to([128, NTILE, 32]), in1=jidx.unsqueeze(2).broadcast_to([128, NTILE, 32]),
         op=ALU.is_le)
    K.op("dve", "tensor_reduce", ["cmpj"], ["eid"], out=eid, in_=v3(cmpj, NTILE, 32), axis=AX.X, op=ALU.add)
    rowb = K.tile(2 * NTILE, F32)
    unus = K.tile(NTILE, F32)
    K.dma("q_sp", rowb, io["c_rowbase"][:, :], [], ["rowb"])
    K.op("dve", "tensor_scalar", ["eid"], ["unus"], out=unus, in0=eid, scalar1=31.5, scalar2=1.0e6, op0=ALU.is_gt,
         op1=ALU.mult)
    K.op("dve", "tensor_tensor", ["rowb", "unus"], ["rowb"], out=v3(rowb, NTILE, 2), in0=v3(rowb, NTILE, 2),
         in1=unus.unsqueeze(2).broadcast_to([128, NTILE, 2]), op=ALU.add)
    K.op("dve", "tensor_copy", ["rowb"], ["ridx"], out=io["ridx"], in_=rowb)
    K.op("dve", "tensor_scalar", ["eid", "pcol"], ["widf"], out=widf, in0=eid, scalar1=128.0, scalar2=pcol[:, 0:1],
         op0=ALU.mult, op1=ALU.add)
    K.op("dve", "tensor_copy", ["widf"], ["widx"], out=io["widx"], in_=widf)
    if "dbg_slot" in io:
        K.dma("q_sp", io["dbg_slot"][:, :], sl, ["sl"], ["dbg_slot"])
        K.dma("q_sp", io["dbg_w12"][:, :], W2, ["w12"], ["dbg_w12"])
        K.dma("q_sp", io["dbg_wid"][:, :], widf, ["widf"], ["dbg_wid"])
    S.barrier()


def phase_fs(K, io):
    S = K.S
    K.reset()
    slot, widx, W2, ridx = io["slot"], io["widx"], io["w12"], io["ridx"]
    regs = {}

    def breg(e, v):
        if v not in regs:
            regs[v] = e.to_reg(v)
        return regs[v]

    IOA = bass.IndirectOffsetOnAxis
    ht = [K.tile(D, BF16) for _ in range(2)]
    for ti in range(NT):
        b = ti % 2
        K.dma("q_sp", ht[b], io["s_h2"][ti * 128:(ti + 1) * 128, :], [], [("ht", b)])
        for k in range(2):
            S.op("pool", lambda e, b=b, k=k, ti=ti: e.indirect_dma_start(
                out=io["s_xs"], out_offset=IOA(ap=slot[:, k * NT + ti:k * NT + ti + 1], axis=0), in_=ht[b], in_offset=None),
                [("ht", b), "slot"], [("xs", ti, k)], dma="q_pool")
    S.barrier()
    K.reset()
    wg = [K.tile(8192, BF16) for _ in range(2)]
    wu = [K.tile(8192, BF16) for _ in range(2)]
    wd = [K.tile(8192, BF16) for _ in range(2)]
    xs = [K.tile(2 * D, BF16) for _ in range(2)]
    xT = [K.tile(KC * TS, BF16) for _ in range(2)]
    hid = [K.tile(4 * TS, BF16) for _ in range(2)]
    sg = [K.tile(TS, BF16) for _ in range(2)]
    yt = [K.tile(D, BF16) for _ in range(2)]
    identb = io["identb"]
    yb = 0
    for j in range(NTILE):
        b = j % 2
        for (wt, src, nm) in ((wg[b], io["s_wgb"], "wg"), (wu[b], io["s_wub"], "wu"), (wd[b], io["s_wdb"], "wd")):
            S.op("pool", lambda e, wt=wt, src=src, j=j: e.indirect_dma_start(
                out=wt, out_offset=None, in_=src, in_offset=IOA(ap=widx[:, j:j + 1], axis=0),
                bounds_check=breg(e, 4095), oob_is_err=False),
                ["widx"], [(nm, b)], dma="q_pool")
        xs3 = v3(xs[b], 2, D)
        for sh in range(2):
            S.op("pool", lambda e, o=xs3[:, sh, :], c=2 * j + sh: e.indirect_dma_start(
                out=o, out_offset=None, in_=io["s_xs"], in_offset=IOA(ap=ridx[:, c:c + 1], axis=0),
                bounds_check=breg(e, NSLOT - 1), oob_is_err=False),
                ["ridx"], [("xsb", b)] if sh == 0 else [("xsb2", b)], dma="q_pool")
        xT3 = v3(xT[b], KC, TS)
        for sh in range(2):
            for q2 in range(2):
                bk = sh * 2 + q2
                for i in range(8):
                    kc = q2 * 8 + i
                    K.op("pe", "transpose", [("xsb", b), ("xsb2", b), "identb"], [("ps", bk)],
                         out=K.bank(bk, 128, BF16, i * 64), in_=xs3[:, sh, kc * 128:(kc + 1) * 128], identity=identb)
                K.evac(xT3[:, q2 * 8:(q2 + 1) * 8, sh * 128:(sh + 1) * 128], v3(K.bank(bk, 1024, BF16, 0), 8, 128),
                       [("ps", bk)], [("xT", b)])
        wg3, wu3, wd3 = v3(wg[b], KC, 512), v3(wu[b], KC, 512), v3(wd[b], 4, D)
        hid3 = v3(hid[b], 4, TS)
        for fc in range(4):
            bk = 4 + fc % 2
            for (w3_, nm, o0) in ((wg3, "wg", 0), (wu3, "wu", 256)):
                for kc in range(KC):
                    K.op("pe", "matmul", [(nm, b), ("xT", b)], [("ps", bk)], out=K.bank(bk)[:, o0:o0 + TS],
                         lhsT=w3_[:, kc, fc * 128:(fc + 1) * 128], rhs=xT3[:, kc, :], start=(kc == 0 and o0 == 0),
                         stop=(kc == KC - 1 and o0 == 256), skip_group_check=True)
            sb_ = fc % 2
            K.op("act", "activation", [("ps", bk)], [("sg", sb_)], out=sg[sb_], in_=K.bank(bk)[:, 0:TS], func=AF.Silu)
            K.op("dve", "tensor_tensor", [("ps", bk), ("sg", sb_)], [("hid", b)], out=hid3[:, fc, :],
                 in0=K.bank(bk)[:, TS:2 * TS], in1=sg[sb_], op=ALU.mult)
        for sh in range(2):
            y_ = yt[yb % 2]
            yk = ("yt", yb % 2)
            yb += 1
            for c in range(4):
                bk = 6 + c % 2
                for fc in range(4):
                    K.op("pe", "matmul", [("hid", b), ("wd", b)], [("ps", bk)], out=K.bank(bk),
                         lhsT=hid3[:, fc, sh * 128:(sh + 1) * 128], rhs=wd3[:, fc, c * 512:(c + 1) * 512],
                         start=(fc == 0), stop=(fc == 3))
                K.evac(y_[:, c * 512:(c + 1) * 512], K.bank(bk), [("ps", bk)], [yk])
            K.dma("q_sp", io["s_ys"][j * TS + sh * 128:j * TS + (sh + 1) * 128, :], y_, [yk], [("ys", j, sh)])
    S.barrier()
    K.reset()
    gfin = K.tile(D, F32)
    x1t = [K.tile(D, F32) for _ in range(2)]
    y1 = [K.tile(D, BF16) for _ in range(2)]
    y2 = [K.tile(D, BF16) for _ in range(2)]
    junk = K.tile(D, BF16)
    sm = [K.tile(8, F32) for _ in range(2)]
    K.dma("q_sp", gfin, io["g_final"].partition_broadcast(128), [], ["gfin"])
    for ti in range(NT):
        b = ti % 2
        xk, sk = ("x1t", b), ("smf", b)
        K.dma("q_sp", x1t[b], io["s_x1"][ti * 128:(ti + 1) * 128, :], [], [xk])
        for (yy, nm, k) in ((y1[b], "y1", 0), (y2[b], "y2", 1)):
            S.op("pool", lambda e, yy=yy, k=k, ti=ti: e.indirect_dma_start(
                out=yy, out_offset=None, in_=io["s_ys"], in_offset=IOA(ap=slot[:, k * NT + ti:k * NT + ti + 1], axis=0)),
                ["slot"], [(nm, b)], dma="q_pool")
        K.op("dve", "scalar_tensor_tensor", [("y1", b), "w12", xk], [xk], out=x1t[b], in0=y1[b], scalar=W2[:, ti:ti + 1],
             in1=x1t[b], op0=ALU.mult, op1=ALU.add)
        K.op("dve", "scalar_tensor_tensor", [("y2", b), "w12", xk], [xk], out=x1t[b], in0=y2[b],
             scalar=W2[:, NT + ti:NT + ti + 1], in1=x1t[b], op0=ALU.mult, op1=ALU.add)
        K.op("act", "activation", [xk], ["junkf", sk], out=junk, in_=x1t[b], func=AF.Square, accum_out=sm[b][:, 0:1])
        K.op("dve", "tensor_scalar", [sk], [sk], out=sm[b][:, 1:2], in0=sm[b][:, 0:1], scalar1=1.0 / D, scalar2=EPS,
             op0=ALU.mult, op1=ALU.add)
        K.op("act", "activation", [sk], [sk], out=sm[b][:, 2:3], in_=sm[b][:, 1:2], func=AF.Sqrt)
        K.op("dve", "reciprocal", [sk], [sk], out=sm[b][:, 3:4], in_=sm[b][:, 2:3])
        K.op("dve", "scalar_tensor_tensor", [xk, sk, "gfin"], [xk], out=x1t[b], in0=x1t[b], scalar=sm[b][:, 3:4],
             in1=gfin, op0=ALU.mult, op1=ALU.mult)
        K.dma("q_sp", io["y"][ti * 128:(ti + 1) * 128, :], x1t[b], [xk], [("y", ti)])
    S.barrier()


def build_nc(upto="a", debug=False):
    nc = bass.Bass("TRN2", target_bir_lowering=False)
    io = {}

    def inp(name, shape, dt=F32):
        io[name] = nc.dram_tensor(name, list(shape), dt, kind="ExternalInput").ap()

    def scratch(name, shape, dt):
        kind = "ExternalOutput" if debug else "Internal"
        io[name] = nc.dram_tensor(name, list(shape), dt, kind=kind).ap()

    inp("x", [T, D])
    inp("w_in", [D, W_IN])
    inp("c_identf", [128, 128])
    inp("c_gmix", [128, KC])
    inp("b_gate", [24])
    for kv in "kv":
        inp("w_cmp_%s1" % kv, [4096, 256])
        inp("w_cmp_%s2" % kv, [256, 128])
        inp("c_pos%s" % kv, [128, 32])
    inp("c_dsel", [128, 2048])
    inp("c_dwin", [128, 640])
    inp("c_dcmp", [128, NT * 127])
    inp("c_rowvalid", [128, NT])
    inp("c_mulmask", [128, NT * 32])
    inp("c_addmask", [128, NT * 32])
    inp("c_overlap", [127, 32])
    inp("w_alpha2", [16, 512])
    inp("b_alpha", [512])
    inp("g_gla", [256])
    inp("c_tris", [128, 128])
    inp("w_out", [D, D])
    inp("c_gffn", [128, KC])
    inp("w_rg", [D, 4])
    inp("w_re", [D, 32])
    inp("b_rg", [4])
    inp("b_re", [32])
    inp("w_eg", [4, 8, D, 512])
    inp("w_eu", [4, 8, D, 512])
    inp("w_ed", [4, 8, 512, D])
    inp("g_final", [D])
    inp("c_mask01", [128, 128])
    scratch("s_fm", [FM_ROWS, T], BF16)
    scratch("s_tm", [T, TM_COLS], BF16)
    scratch("s_gate", [T, 24], F32)
    scratch("s_alphaT", [16, T], F32)
    scratch("s_mixT", [MIX_ROWS, T], BF16)
    scratch("s_x1", [T, D], F32)
    scratch("s_h2T", [D, T], BF16)
    scratch("s_h2", [T, D], BF16)
    for nm in ("s_wgb", "s_wub", "s_wdb"):
        io[nm] = nc.dram_tensor(nm, [4096, 8192], BF16, kind="Internal").ap()
    io["s_xs"] = nc.dram_tensor("s_xs", [NSLOT, D], BF16, kind="Internal").ap()
    io["s_ys"] = nc.dram_tensor("s_ys", [NSLOT, D], BF16, kind="Internal").ap()
    inp("g_ffn", [D])
    inp("c_stri", [128, 128])
    inp("c_thr", [128, 8])
    inp("c_jidx", [128, NTILE])
    inp("c_pcol", [128, 1])
    inp("c_rowbase", [128, 2 * NTILE])
    if debug:
        io["dbg_slot"] = nc.dram_tensor("dbg_slot", [128, 2 * NT], F32, kind="ExternalOutput").ap()
        io["dbg_w12"] = nc.dram_tensor("dbg_w12", [128, 2 * NT], F32, kind="ExternalOutput").ap()
        io["dbg_wid"] = nc.dram_tensor("dbg_wid", [128, NTILE], F32, kind="ExternalOutput").ap()
    io["y"] = nc.dram_tensor("y", [T, D], F32, kind="ExternalOutput").ap()

    with ExitStack() as st:
        S = Sched(nc)
        S.setup(st)
        big = st.enter_context(nc.sbuf_tensor("big", [128, SBUF_WORDS], F32))
        ps = st.enter_context(nc.psum_tensor("ps", [128, 4096], F32))
        K = Ctx(nc, S, big, ps)
        identf = K.tile(128, F32)
        gmix = K.tile(KC, F32)
        identb = K.tile(128, BF16)
        io["kcT"] = K.tile(256, BF16)
        io["vc"] = K.tile(256, BF16)
        io["C"] = K.tile(NT * 32, F32)
        io["w12"] = K.tile(2 * NT, F32)
        io["slot"] = K.tile(2 * NT, F32).bitcast(I32)
        io["widx"] = K.tile(NTILE, F32).bitcast(I32)
        io["ridx"] = K.tile(2 * NTILE, F32).bitcast(I32)
        io["bg"] = BgCast(K, io, [K.tile(2048, BF16) for _ in range(4)])
        gffn = K.tile(KC, F32)
        io["gffn"] = gffn
        K.base = K.off
        K.dma("q_sp", gffn, io["c_gffn"][:, :], [], ["gffn"])
        K.dma("q_sp", identf, io["c_identf"][:, :], [], ["identf"])
        K.dma("q_sp", gmix, io["c_gmix"][:, :], [], ["gmix"])
        io["identf"] = identf
        io["gmix"] = gmix
        io["identb"] = identb
        K.op("dve", "tensor_copy", ["identf"], ["identb"], out=identb, in_=identf)
        S.barrier()
        if "a" in upto:
            phase_a(K, io)
        if "b" in upto:
            phase_b(K, io)
        if "c" in upto:
            phase_c(K, io)
        if "d" in upto:
            phase_d(K, io)
        if "e" in upto:
            phase_e(K, io)
        if "f" in upto:
            phase_f(K, io)
        if "w" in upto:
            phase_w(K, io)
        if "g" in upto:
            phase_e2(K, io)
        if "s" in upto:
            phase_fs(K, io)
        S.barrier()
        with nc.Block() as block:
            S.emit(block)
    return nc, S


def _make_consts():
    f = np.float32
    c = {}
    q = np.arange(128)[:, None]
    u = np.arange(2048)[None, :]
    d = (q + 1920 - u).astype(f)
    c["c_dsel"] = np.where(d < 0, BIGD, d).astype(f)
    cc = np.arange(640)[None, :]
    d = (q + 512 - cc).astype(f)
    c["c_dwin"] = np.where((d < 0) | (d >= 512), BIGD, d).astype(f)
    n = np.arange(127)[None, None, :]
    qt = np.arange(NT)[None, :, None]
    d = (qt * 128 + q[:, :, None] - (16 * n + 31)).astype(f)
    c["c_dcmp"] = np.where(d < 0, BIGD, d).astype(f).reshape(128, NT * 127)
    t = (np.arange(NT)[None, :] * 128 + q)
    c["c_rowvalid"] = (t >= 31).astype(f)
    j = np.arange(32)[None, None, :]
    tt = t[:, :, None]
    cur = tt // 64
    forced = (j == 0) | ((j <= cur) & (j > cur - 2))
    causal = (j * 64 <= tt)
    c["c_mulmask"] = ((~forced) & causal).astype(f).reshape(128, NT * 32)
    c["c_addmask"] = np.where(forced, BIG, np.where(causal, 0.0, -BIG)).astype(f).reshape(128, NT * 32)
    cs = np.arange(127)[:, None] * 16
    ss = np.arange(32)[None, :] * 64
    c["c_overlap"] = ((cs < ss + 64) & (cs + 32 > ss)).astype(f)
    c["c_identf"] = np.eye(128, dtype=f)
    tri = (np.arange(128)[:, None] <= np.arange(128)[None, :])
    c["c_tris"] = (tri * (-1.0 / 16.0)).astype(f)
    c["c_mask01"] = tri.astype(f)
    c["c_stri"] = (np.arange(128)[:, None] < np.arange(128)[None, :]).astype(f)
    c["c_thr"] = np.tile((np.arange(8) * 256.0)[None, :], (128, 1)).astype(f)
    c["c_jidx"] = np.tile(np.arange(48, dtype=f)[None, :], (128, 1)).astype(f)
    c["c_pcol"] = np.arange(128, dtype=f)[:, None]
    c["c_rowbase"] = (np.arange(96, dtype=f)[None, :] * 128 + np.arange(128, dtype=f)[:, None]).astype(f)
    return {k: np.ascontiguousarray(v) for k, v in c.items()}


_CONSTS = _make_consts()


def host_inputs(inputs, b):
    f = np.float32
    m = {}
    m["x"] = np.ascontiguousarray(inputs["x"][b], dtype=f)
    m["w_in"] = np.ascontiguousarray(inputs["w_in"][0], dtype=f)
    m["c_identf"] = np.eye(128, dtype=f)
    m["c_gmix"] = np.ascontiguousarray(inputs["g_mix_norm"][0].reshape(KC, 128).T, dtype=f)
    m["b_gate"] = np.ascontiguousarray(inputs["b_nsa_gate"][0], dtype=f)
    m["w_cmp_k1"] = np.ascontiguousarray(inputs["w_cmp_k1"][0], dtype=f)
    m["w_cmp_k2"] = np.ascontiguousarray(inputs["w_cmp_k2"][0], dtype=f)
    m["w_cmp_v1"] = np.ascontiguousarray(inputs["w_cmp_v1"][0], dtype=f)
    m["w_cmp_v2"] = np.ascontiguousarray(inputs["w_cmp_v2"][0], dtype=f)
    m["c_posk"] = np.ascontiguousarray(inputs["cmp_pos_k"][0].T, dtype=f)
    m["c_posv"] = np.ascontiguousarray(inputs["cmp_pos_v"][0].T, dtype=f)
    m["w_alpha2"] = np.ascontiguousarray(inputs["w_alpha2"][0], dtype=f)
    m["b_alpha"] = np.ascontiguousarray(inputs["b_alpha"][0], dtype=f)
    m["g_gla"] = np.ascontiguousarray(inputs["g_gla_norm"][0], dtype=f)
    m["w_out"] = np.ascontiguousarray(inputs["w_out"][0], dtype=f)
    m["c_gffn"] = np.ascontiguousarray(inputs["g_ffn_norm"][0].reshape(KC, 128).T, dtype=f)
    m["w_rg"] = np.ascontiguousarray(inputs["w_router_group"][0], dtype=f)
    m["w_re"] = np.ascontiguousarray(inputs["w_router_expert"][0].reshape(D, 32), dtype=f)
    m["b_rg"] = np.ascontiguousarray(inputs["b_router_group"][0], dtype=f)
    m["b_re"] = np.ascontiguousarray(inputs["b_router_expert"][0].reshape(32), dtype=f)
    m["w_eg"] = np.ascontiguousarray(inputs["w_expert_gate"][0], dtype=f)
    m["w_eu"] = np.ascontiguousarray(inputs["w_expert_up"][0], dtype=f)
    m["w_ed"] = np.ascontiguousarray(inputs["w_expert_down"][0], dtype=f)
    m["g_final"] = np.ascontiguousarray(inputs["g_final_norm"], dtype=f)
    m["g_ffn"] = np.ascontiguousarray(inputs["g_ffn_norm"][0], dtype=f)
    m.update(_CONSTS)
    return m


def kernel(**inputs):
    nc, _ = build_nc("abcdewgs", debug=False)
    in_maps = [host_inputs(inputs, b) for b in range(8)]
    res = run_bass_kernel_spmd(nc, in_maps, core_ids=list(range(8)))
    return np.stack([np.asarray(r["y"], dtype=np.float32) for r in res.results], axis=0)
```

```python
import numpy as np
import ml_dtypes
from contextlib import ExitStack
import concourse.bass as bass
import concourse.mybir as mybir
from concourse.bass_utils import run_bass_kernel_spmd

F32 = mybir.dt.float32
BF16 = mybir.dt.bfloat16
AF = mybir.ActivationFunctionType
ALU = mybir.AluOpType
AX = mybir.AxisListType

D = 2048
T = 2048
NT = 16
KC = 16
EPS = 1e-6
W_IN = 5672
SBUF_WORDS = 49152


class _Stream:
    def __init__(self, name, issuer, sems, is_dma):
        self.name = name
        self.issuer = issuer
        self.sems = sems
        self.is_dma = is_dma
        self.count = 0

    def target(self, c):
        if not self.is_dma:
            return (self.sems[0], c)
        k = len(self.sems)
        return (self.sems[(c - 1) % k], 16 * ((c - 1) // k + 1))


class Sched:
    ENGINES = ("pe", "dve", "act", "pool", "sp")

    def __init__(self, nc, ring=8):
        self.nc = nc
        self.ring = ring
        self.items = {e: [] for e in self.ENGINES}
        self.streams = {}
        self.waited = {e: {} for e in self.ENGINES}
        self.last_write = {}
        self.readers = {}
        self.n_ops = 0

    def setup(self, stack):
        nc = self.nc
        for e in self.ENGINES:
            s = stack.enter_context(nc.semaphore("s_" + e))
            self.streams[e] = _Stream(e, e, [s], False)
        for q, issuer in (("q_sp", "sp"), ("q_pool", "pool"), ("q_act", "act")):
            sems = [stack.enter_context(nc.semaphore("s_%s_%d" % (q, i))) for i in range(self.ring)]
            self.streams[q] = _Stream(q, issuer, sems, True)

    def _need(self, eng, dep, waits):
        if dep is None:
            return
        sname, c = dep
        st = self.streams[sname]
        if not st.is_dma:
            if sname == eng and eng == "pe":
                return
            if self.waited[eng].get(sname, 0) >= c:
                return
            waits[sname] = max(waits.get(sname, 0), c)
        else:
            w = self.waited[eng].setdefault(sname, set())
            if c in w:
                return
            waits.setdefault(sname, set()).add(c)

    def op(self, eng, fn, reads=(), writes=(), dma=None):
        waits = {}
        for k in reads:
            self._need(eng, self.last_write.get(k), waits)
        for k in writes:
            self._need(eng, self.last_write.get(k), waits)
            for rn, rc in list(self.readers.get(k, {}).items()):
                if rn == eng and dma is None and eng == "pe":
                    continue
                if isinstance(rc, set):
                    for cc in rc:
                        self._need(eng, (rn, cc), waits)
                else:
                    self._need(eng, (rn, rc), waits)
        sname = dma if dma is not None else eng
        st = self.streams[sname]
        assert st.issuer == eng
        st.count += 1
        c = st.count
        wl = []
        if st.is_dma and c > len(st.sems):
            sem, val = st.target(c)
            wl.append((sem, val - 16))
        for n, v in waits.items():
            s2 = self.streams[n]
            if s2.is_dma:
                for cc in sorted(v):
                    wl.append(s2.target(cc))
                    self.waited[eng][n].add(cc)
            else:
                wl.append(s2.target(v))
                self.waited[eng][n] = v
        me = (sname, c)
        for k in reads:
            if st.is_dma:
                self.readers.setdefault(k, {}).setdefault(sname, set()).add(c)
            else:
                self.readers.setdefault(k, {})[sname] = c
        for k in writes:
            self.last_write[k] = me
            self.readers[k] = {}
        sem, _ = st.target(c)
        self.items[eng].append((wl, fn, sem, 16 if st.is_dma else 1))
        self.n_ops += 1
        return me

    def barrier(self):
        for eng in self.ENGINES:
            wl = []
            for n, st in self.streams.items():
                if st.count == 0:
                    continue
                if st.is_dma:
                    w = self.waited[eng].setdefault(n, set())
                    for c in range(max(1, st.count - len(st.sems) + 1), st.count + 1):
                        if c not in w:
                            wl.append(st.target(c))
                            w.add(c)
                else:
                    if n == eng:
                        continue
                    if self.waited[eng].get(n, 0) < st.count:
                        wl.append(st.target(st.count))
                        self.waited[eng][n] = st.count
            if wl:
                self.items[eng].append((wl, None, None, 0))
        self.last_write = {}
        self.readers = {}

    def emit(self, block):
        def run(engname):
            def f(e):
                for wl, fn, sem, inc in self.items[engname]:
                    for (ws, wv) in wl:
                        e.wait_ge(ws, wv)
                    if fn is not None:
                        fn(e).then_inc(sem, inc)
            return f

        block.tensor(run("pe"))
        block.vector(run("dve"))
        block.scalar(run("act"))
        block.gpsimd(run("pool"))
        block.sync(run("sp"))


class Ctx:
    def __init__(self, nc, S, big, ps):
        self.nc = nc
        self.S = S
        self.big = big
        self.ps = ps
        self.base = 0
        self.off = 0
        self.rr = 0

    def persist(self, free, dt):
        ap = self.tile(free, dt)
        self.base = self.off
        return ap

    def reset(self):
        self.off = self.base

    def tile(self, free, dt):
        words = free if dt == F32 else (free + 1) // 2
        words = (words + 7) // 8 * 8
        a = self.big[:, self.off:self.off + words]
        self.off += words
        assert self.off <= SBUF_WORDS, "SBUF overflow %d" % self.off
        if dt == F32:
            a = a[:, 0:free]
        if dt != F32:
            a = a.bitcast(dt)
            if a.shape[1] != free:
                a = a[:, 0:free]
        return a

    def bank(self, b, n=512, dt=F32, off=0):
        if dt == F32:
            return self.ps[:, b * 512 + off:b * 512 + off + n]
        return self.ps[:, b * 512 + off:b * 512 + off + (n + 1) // 2].bitcast(dt)

    def op(self, eng, method, reads, writes, **kw):
        return self.S.op(eng, lambda e: getattr(e, method)(**kw), reads, writes)

    def dma(self, q, out, in_, reads, writes):
        eng = {"q_sp": "sp", "q_pool": "pool", "q_act": "act"}[q]
        return self.S.op(eng, lambda e: e.dma_start(out=out, in_=in_), reads, writes, dma=q)

    def evac(self, out, in_, reads, writes, scale=None):
        self.rr += 1
        if self.rr % 2 == 0:
            if scale is None:
                return self.op("act", "activation", reads, writes, out=out, in_=in_, func=AF.Copy)
            return self.op("act", "activation", reads, writes, out=out, in_=in_, func=AF.Copy, scale=scale)
        if scale is None:
            return self.op("dve", "tensor_copy", reads, writes, out=out, in_=in_)
        return self.op("dve", "tensor_scalar", reads, writes, out=out, in0=in_, scalar1=scale, scalar2=None,
                       op0=ALU.mult)


def v3(ap, a, b):
    return ap.rearrange("p (a b) -> p a b", a=a, b=b)


FM_PARTS = [("q", 0, 1024, 0), ("kcmp", 1024, 256, 1024), ("vcmp", 1280, 256, 1280), ("ksel", 1536, 256, 1536),
            ("kwin", 2048, 256, 1792), ("gq", 2584, 512, 2048), ("gk", 3096, 512, 2560)]
FM_ROWS = 3072
ALPHA_COL = 4632
TM_PARTS = [("vsel", 1792, 256, 0), ("vwin", 2304, 256, 256), ("gv0", 3608, 512, 512), ("gv1", 4120, 512, 1024),
            ("og0", 4648, 512, 1536), ("og1", 5160, 512, 2048)]
TM_COLS = 2560
GATE_COL = 2560


def phase_a(K, io):
    S = K.S
    K.reset()
    hT = K.tile(KC * T, BF16)
    hT3 = v3(hT, KC, T)
    mark = K.off
    xt = [K.tile(D, F32) for _ in range(2)]
    junk = K.tile(D, BF16)
    st = [K.tile(8, F32) for _ in range(2)]
    for ti in range(NT):
        b = ti % 2
        xk, sk = ("xt", b), ("st", b)
        K.dma("q_sp", xt[b], io["x"][ti * 128:(ti + 1) * 128, :], [], [xk])
        K.op("act", "activation", [xk], ["junk", sk], out=junk, in_=xt[b], func=AF.Square, accum_out=st[b][:, 0:1])
        K.op("dve", "tensor_scalar", [sk], [sk], out=st[b][:, 1:2], in0=st[b][:, 0:1], scalar1=1.0 / D, scalar2=EPS,
             op0=ALU.mult, op1=ALU.add)
        K.op("act", "activation", [sk], [sk], out=st[b][:, 2:3], in_=st[b][:, 1:2], func=AF.Sqrt)
        K.op("dve", "reciprocal", [sk], [sk], out=st[b][:, 3:4], in_=st[b][:, 2:3])
        K.op("dve", "tensor_scalar", [xk, sk], [xk], out=xt[b], in0=xt[b], scalar1=st[b][:, 3:4], scalar2=None,
             op0=ALU.mult)
        for q4 in range(4):
            bk = 4 * b + q4
            pk = ("ps", bk)
            for j in range(4):
                kc = q4 * 4 + j
                K.op("pe", "transpose", [xk, "identf"], [pk], out=K.bank(bk, 128, F32, j * 128),
                     in_=xt[b][:, kc * 128:(kc + 1) * 128], identity=io["identf"])
            for j in range(4):
                kc = q4 * 4 + j
                K.evac(hT3[:, kc, ti * 128:(ti + 1) * 128], K.bank(bk, 128, F32, j * 128), [pk, "gmix"], [("hT", ti)],
                       scale=io["gmix"][:, kc:kc + 1])
    S.barrier()
    K.off = mark
    w_in3 = io["w_in"].rearrange("(kc p) c -> p kc c", p=128)
    wfm = [K.tile(KC * 128, BF16) for _ in range(3)]
    ofm = [K.tile(T, BF16) for _ in range(2)]
    ofa = K.tile(T, F32)
    chunks = []
    for (nm, c0, n, r0) in FM_PARTS:
        for j in range(n // 128):
            chunks.append((c0 + j * 128, 128, r0 + j * 128, False))
    chunks.append((ALPHA_COL, 16, 0, True))
    hkeys = [("hT", ti) for ti in range(NT)]
    pb = 0
    for ci, (c0, n, r0, is_alpha) in enumerate(chunks):
        io["bg"].emit(2)
        wb = ci % 3
        wk = ("wfm", wb)
        w3 = v3(wfm[wb], KC, 128)
        K.dma("q_pool", w3[:, :, 0:n], w_in3[:, :, c0:c0 + n], [], [wk])
        ob = ci % 2
        ok = ("ofa",) if is_alpha else ("ofm", ob)
        for tb in range(4):
            bk = pb % 8
            pb += 1
            pk = ("ps", bk)
            for kc in range(KC):
                K.op("pe", "matmul", [wk] + hkeys[tb * 4:tb * 4 + 4], [pk], out=K.bank(bk)[0:n, :],
                     lhsT=w3[:, kc, 0:n], rhs=hT3[:, kc, tb * 512:(tb + 1) * 512], start=(kc == 0), stop=(kc == KC - 1))
            dst = ofa[0:n, tb * 512:(tb + 1) * 512] if is_alpha else ofm[ob][0:n, tb * 512:(tb + 1) * 512]
            K.evac(dst, K.bank(bk)[0:n, :], [pk], [ok])
        if is_alpha:
            K.dma("q_sp", io["s_alphaT"][:, :], ofa[0:16, :], [ok], [("s_alphaT",)])
        else:
            K.dma("q_sp", io["s_fm"][r0:r0 + n, :], ofm[ob][0:n, :], [ok], [("s_fm", r0)])
    wtm = [K.tile(KC * 512, BF16) for _ in range(2)]
    otm = [K.tile(NT * 512, BF16) for _ in range(2)]
    ogt = K.tile(NT * 24, F32)
    bg = K.tile(24, F32)
    K.dma("q_sp", bg, io["b_gate"].partition_broadcast(128), [], ["bg"])
    s_tm3 = io["s_tm"].rearrange("(ti p) c -> p ti c", p=128)
    groups = [(c0, n, t0, False) for (nm, c0, n, t0) in TM_PARTS] + [(GATE_COL, 24, 0, True)]
    for gi, (c0, n, t0, is_gate) in enumerate(groups):
        wb = gi % 2
        wk = ("wtm", wb)
        w3 = v3(wtm[wb], KC, 512)
        K.dma("q_pool", w3[:, :, 0:n], w_in3[:, :, c0:c0 + n], [], [wk])
        ok = ("ogt",) if is_gate else ("otm", wb)
        o3 = v3(ogt, NT, 24) if is_gate else v3(otm[wb], NT, 512)
        for ti in range(NT):
            if ti % 4 == 0:
                io["bg"].emit(1)
            bk = pb % 8
            pb += 1
            pk = ("ps", bk)
            for kc in range(KC):
                K.op("pe", "matmul", [wk, ("hT", ti)], [pk], out=K.bank(bk)[:, 0:n],
                     lhsT=hT3[:, kc, ti * 128:(ti + 1) * 128], rhs=w3[:, kc, 0:n], start=(kc == 0), stop=(kc == KC - 1))
            if is_gate:
                K.op("dve", "tensor_tensor", [pk, "bg"], [ok], out=o3[:, ti, :], in0=K.bank(bk)[:, 0:n], in1=bg,
                     op=ALU.add)
            else:
                K.evac(o3[:, ti, 0:n], K.bank(bk)[:, 0:n], [pk], [ok])
        if is_gate:
            K.dma("q_sp", io["s_gate"].rearrange("(ti p) c -> p ti c", p=128), o3, [ok], [("s_gate",)])
        else:
            K.dma("q_sp", s_tm3[:, :, t0:t0 + n], o3[:, :, 0:n], [ok], [("s_tm", t0)])
    S.barrier()


SCALE = 128.0 ** -0.5
SLOPES = [2.0 ** (-(h + 1)) for h in range(8)]
BIG = 1.0e30
BIGD = 30000.0
MIX_ROWS = 2048


def phase_b(K, io):
    S = K.S
    K.reset()
    kT = K.tile(4 * T, BF16)
    kT3 = v3(kT, 4, T)
    w1 = [K.tile(32 * 256, BF16) for _ in range(2)]
    w2 = [K.tile(2 * 128, BF16) for _ in range(2)]
    pos = [K.tile(32, F32) for _ in range(2)]
    kp = [K.tile(32 * 127, BF16) for _ in range(2)]
    gel = [K.tile(2 * 127, BF16) for _ in range(2)]
    tx2 = K.tile(127, F32)
    tu = K.tile(127, F32)
    K.dma("q_sp", kT3, io["s_fm"][1024:1536, :].rearrange("(a p) t -> p a t", p=128), [], ["kT"])
    for kv, (n1, n2, npz) in enumerate((("w_cmp_k1", "w_cmp_k2", "c_posk"), ("w_cmp_v1", "w_cmp_v2", "c_posv"))):
        K.dma("q_pool", v3(w1[kv], 32, 256), io[n1].rearrange("(l d) j -> d l j", d=128), [], [("w1", kv)])
        K.dma("q_pool", v3(w2[kv], 2, 128), io[n2].rearrange("(jc j) d -> j jc d", j=128), [], [("w2", kv)])
        K.dma("q_sp", pos[kv], io[npz][:, :], [], [("pos", kv)])
    cnt = 0
    for kv in range(2):
        io["bg"].emit(10)
        w13 = v3(w1[kv], 32, 256)
        w23 = v3(w2[kv], 2, 128)
        for g in range(2):
            pb_ = cnt % 2
            cnt += 1
            kp3 = v3(kp[pb_], 32, 127)
            gl3 = v3(gel[pb_], 2, 127)
            for l in range(32):
                eng = "dve"
                K.op(eng, "tensor_scalar", ["kT", ("pos", kv)], [("kp", pb_)], out=kp3[:, l, :],
                     in0=kT3[:, kv * 2 + g, l:l + 2017:16], scalar1=pos[kv][:, l:l + 1], scalar2=None, op0=ALU.add)
            for jc in range(2):
                bk = jc
                pk = ("ps", bk)
                x = K.bank(bk)[:, 0:127]
                for l in range(32):
                    K.op("pe", "matmul", [("w1", kv), ("kp", pb_)], [pk], out=x, lhsT=w13[:, l, jc * 128:(jc + 1) * 128],
                         rhs=kp3[:, l, :], start=(l == 0), stop=(l == 31))
                K.op("act", "activation", [pk], ["tx2"], out=tx2, in_=x, func=AF.Square)
                K.op("dve", "tensor_scalar", ["tx2"], ["tu"], out=tu, in0=tx2, scalar1=0.044715, scalar2=1.0,
                     op0=ALU.mult, op1=ALU.add)
                K.op("dve", "tensor_tensor", ["tu", pk], ["tu"], out=tu, in0=tu, in1=x, op=ALU.mult)
                K.op("act", "activation", ["tu"], ["tx2"], out=tx2, in_=tu, func=AF.Tanh, scale=0.7978845608028654)
                K.op("dve", "tensor_scalar", ["tx2"], ["tu"], out=tu, in0=tx2, scalar1=1.0, scalar2=0.5,
                     op0=ALU.add, op1=ALU.mult)
                K.op("dve", "tensor_tensor", ["tu", pk], [("gel", pb_)], out=gl3[:, jc, :], in0=tu, in1=x, op=ALU.mult)
            bk = 2 + (cnt % 2)
            pk = ("ps", bk)
            if kv == 0:
                o = K.bank(bk)[:, 0:127]
                for jc in range(2):
                    K.op("pe", "matmul", [("w2", kv), ("gel", pb_)], [pk], out=o, lhsT=w23[:, jc, :], rhs=gl3[:, jc, :],
                         start=(jc == 0), stop=(jc == 1))
                K.evac(io["kcT"][:, g * 128:g * 128 + 127], o, [pk], ["kcT"])
            else:
                o = K.bank(bk)[0:127, 0:128]
                for jc in range(2):
                    K.op("pe", "matmul", [("w2", kv), ("gel", pb_)], [pk], out=o, lhsT=gl3[:, jc, :], rhs=w23[:, jc, :],
                         start=(jc == 0), stop=(jc == 1))
                K.evac(io["vc"][0:127, g * 128:(g + 1) * 128], o, [pk], ["vc"])
    S.barrier()


def phase_c(K, io):
    S = K.S
    K.reset()
    qTs = [v3(K.tile(8 * 128, BF16), 8, 128) for _ in range(2)]
    kselT = v3(K.tile(2 * T, BF16), 2, T)
    kwinT = v3(K.tile(2 * T, BF16), 2, T)
    vsel = v3(K.tile(NT * 256, BF16), NT, 256)
    vwin = v3(K.tile(NT * 256, BF16), NT, 256)
    gsig = v3(K.tile(NT * 24, F32), NT, 24)
    Dsel = K.tile(2048, F32)
    Dwin = K.tile(640, F32)
    Dcmp = v3(K.tile(NT * 127, F32), NT, 127)
    rowvalid = K.tile(NT, F32)
    mulmask = v3(K.tile(NT * 32, F32), NT, 32)
    addmask = v3(K.tile(NT * 32, F32), NT, 32)
    overlap = K.tile(32, F32)
    selbias = K.tile(2 * 32, F32)
    s_sel = [K.tile(2048, F32) for _ in range(4)]
    p_sel = [K.tile(2048, BF16) for _ in range(4)]
    pt_sel = [K.tile(2048, BF16) for _ in range(4)]
    s_win = [K.tile(640, F32) for _ in range(4)]
    p_win = [K.tile(640, BF16) for _ in range(4)]
    pt_win = [K.tile(640, BF16) for _ in range(4)]
    stat = [K.tile(8, F32) for _ in range(8)]
    sc = [K.tile(127, F32) for _ in range(4)]
    pc = [K.tile(127, F32) for _ in range(4)]
    pg = [K.tile(128, BF16) for _ in range(4)]
    pnT = [K.tile(128, F32) for _ in range(4)]
    pcT = [K.tile(128, BF16) for _ in range(4)]
    cst = [K.tile(8, F32) for _ in range(4)]
    osb = [K.tile(512, BF16) for _ in range(2)]
    imp2 = K.tile(32, F32)
    mx8 = K.tile(8, F32)
    identf, identb = io["identf"], io["identb"]
    kcT, vc = io["kcT"], io["vc"]
    R4 = range(4)

    s_q3 = io["s_fm"][0:1024, :].rearrange("(h p) t -> p h t", p=128)
    K.dma("q_sp", kselT, io["s_fm"][1536:1792, :].rearrange("(g p) t -> p g t", p=128), [], ["kselT"])
    K.dma("q_sp", kwinT, io["s_fm"][1792:2048, :].rearrange("(g p) t -> p g t", p=128), [], ["kwinT"])
    s_tm3 = io["s_tm"].rearrange("(kt p) c -> p kt c", p=128)
    K.dma("q_sp", vsel, s_tm3[:, :, 0:256], [], ["vsel"])
    K.dma("q_sp", vwin, s_tm3[:, :, 256:512], [], ["vwin"])
    K.dma("q_sp", gsig, io["s_gate"].rearrange("(ti p) c -> p ti c", p=128), [], ["gsig"])
    K.dma("q_sp", Dsel, io["c_dsel"][:, :], [], ["Dsel"])
    K.dma("q_sp", Dwin, io["c_dwin"][:, :], [], ["Dwin"])
    K.dma("q_sp", Dcmp, io["c_dcmp"].rearrange("p (a b) -> p a b", b=127), [], ["Dcmp"])
    K.dma("q_sp", rowvalid, io["c_rowvalid"][:, :], [], ["cm0"])
    K.dma("q_sp", mulmask, io["c_mulmask"].rearrange("p (a b) -> p a b", b=32), [], ["cm1"])
    K.dma("q_sp", addmask, io["c_addmask"].rearrange("p (a b) -> p a b", b=32), [], ["cm2"])
    K.dma("q_sp", overlap[0:127, :], io["c_overlap"][:, :], [], ["cm3"])
    K.op("act", "activation", ["gsig"], ["gsig"], out=gsig, in_=gsig, func=AF.Sigmoid)
    trr = [0]

    def transposes(p_ap, pt_ap, nkt, pk_, ptk):
        for k0 in range(0, nkt, 8):
            n = min(8, nkt - k0)
            bk = trr[0] % 4
            trr[0] += 1
            for j in range(n):
                K.op("pe", "transpose", [pk_, "identb"], [("ps", bk)], out=K.bank(bk, 128, BF16, j * 64),
                     in_=p_ap[:, (k0 + j) * 128:(k0 + j + 1) * 128], identity=identb)
            K.op("act", "activation", [("ps", bk)], [ptk], out=pt_ap[:, k0 * 128:(k0 + n) * 128],
                 in_=K.bank(bk, n * 128, BF16, 0), func=AF.Copy)

    it = 0
    K.dma("q_sp", qTs[0], s_q3[:, :, 0:128], [], [("qT", 0)])
    for qt in range(NT):
        qs = slice(qt * 128, (qt + 1) * 128)
        qT = qTs[qt % 2]
        qk_ = ("qT", qt % 2)
        ql = slice(0, 128)
        if qt + 1 < NT:
            K.dma("q_sp", qTs[(qt + 1) % 2], s_q3[:, :, (qt + 1) * 128:(qt + 2) * 128], [], [("qT", (qt + 1) % 2)])
        io["bg"].emit(13)
        nk = (qt + 1) * 128
        nc_s = (nk + 511) // 512
        off = 1920 - qt * 128
        k0w = max(0, qt * 128 - 512)
        nkw = qt * 128 + 128 - k0w
        nc_w = (nkw + 511) // 512
        coff = k0w - (qt * 128 - 512)
        nb = 2 * (qt + 1)
        for g in range(2):
            H = [g * 4 + hh for hh in R4]
            coefs = [-SLOPES[h] / SCALE for h in H]
            sb = selbias[:, g * 32:(g + 1) * 32]
            for hh in R4:
                K.op("pe", "matmul", [qk_, "kcT"], [("ps", hh)], out=K.bank(hh)[:, 0:127], lhsT=qT[:, H[hh], ql],
                     rhs=kcT[:, g * 128:g * 128 + 127], start=True, stop=True)
            for hh in R4:
                K.op("dve", "scalar_tensor_tensor", ["Dcmp", ("ps", hh)], [("sc", hh)], out=sc[hh], in0=Dcmp[:, qt, :],
                     scalar=coefs[hh], in1=K.bank(hh)[:, 0:127], op0=ALU.mult, op1=ALU.add)
            for hh in R4:
                K.op("dve", "tensor_reduce", [("sc", hh)], [("cst", hh)], out=cst[hh][:, 0:1], in_=sc[hh], axis=AX.X,
                     op=ALU.max)
            for hh in R4:
                K.op("dve", "tensor_scalar", [("cst", hh)], [("cst", hh)], out=cst[hh][:, 1:2], in0=cst[hh][:, 0:1],
                     scalar1=-SCALE, scalar2=None, op0=ALU.mult)
            for hh in R4:
                K.op("act", "activation", [("sc", hh), ("cst", hh)], [("pc", hh), ("cst", hh)], out=pc[hh], in_=sc[hh],
                     func=AF.Exp, scale=SCALE, bias=cst[hh][:, 1:2], accum_out=cst[hh][:, 2:3])
            for hh in R4:
                K.op("dve", "reciprocal", [("cst", hh)], [("cst", hh)], out=cst[hh][:, 3:4], in_=cst[hh][:, 2:3])
            for hh in R4:
                K.op("dve", "tensor_scalar", [("cst", hh), "cm0"], [("cst", hh)], out=cst[hh][:, 4:5],
                     in0=cst[hh][:, 3:4], scalar1=rowvalid[:, qt:qt + 1], scalar2=None, op0=ALU.mult)
            for hh in R4:
                K.op("dve", "tensor_scalar", [("pc", hh), ("cst", hh)], [("pc", hh)], out=pc[hh], in0=pc[hh],
                     scalar1=cst[hh][:, 4:5], scalar2=None, op0=ALU.mult)
            for hh in R4:
                K.op("dve", "tensor_scalar", [("pc", hh), "gsig"], [("pg", hh)], out=pg[hh][:, 0:127], in0=pc[hh],
                     scalar1=gsig[:, qt, H[hh] * 3:H[hh] * 3 + 1], scalar2=None, op0=ALU.mult)
            for hh in R4:
                bk = 4 + hh % 2
                o0 = (hh // 2) * 192
                K.op("pe", "transpose", [("pc", hh), "identf"], [("ps", bk)], out=K.bank(bk, 128, F32, o0)[0:127, :],
                     in_=pc[hh], identity=identf)
                K.op("pe", "transpose", [("pg", hh), "identb"], [("ps", bk)],
                     out=K.bank(bk, 128, BF16, o0 + 128)[0:127, :], in_=pg[hh][:, 0:127], identity=identb)
            for hh in R4:
                bk = 4 + hh % 2
                o0 = (hh // 2) * 192
                K.op("act", "activation", [("ps", bk)], [("pnT", hh)], out=pnT[hh][0:127, :],
                     in_=K.bank(bk, 128, F32, o0)[0:127, :], func=AF.Copy)
                K.op("dve", "tensor_copy", [("ps", bk)], [("pcT", hh)], out=pcT[hh][0:127, :],
                     in_=K.bank(bk, 128, BF16, o0 + 128)[0:127, :])
            for hh in R4:
                K.op("pe", "matmul", [("pnT", hh), "cm3"], [("ps", 6)], out=K.bank(6)[:, 0:32], lhsT=pnT[hh][0:127, :],
                     rhs=overlap[0:127, :], start=(hh == 0), stop=(hh == 3))
            K.op("dve", "tensor_tensor", [("ps", 6), "cm1"], ["imp2"], out=imp2, in0=K.bank(6)[:, 0:32],
                 in1=mulmask[:, qt, :], op=ALU.mult)
            K.op("dve", "tensor_tensor", ["imp2", "cm2"], ["imp2"], out=imp2, in0=imp2, in1=addmask[:, qt, :], op=ALU.add)
            K.op("dve", "max", ["imp2"], ["mx8"], out=mx8, in_=imp2)
            K.op("dve", "tensor_scalar", ["imp2", "mx8"], ["imp2"], out=imp2, in0=imp2, scalar1=mx8[:, 7:8], scalar2=None,
                 op0=ALU.is_ge)
            K.op("dve", "tensor_scalar", ["imp2"], [("selbias", g)], out=sb, in0=imp2, scalar1=-1.0, scalar2=BIG,
                 op0=ALU.add, op1=ALU.mult)

            def qk_sel(hh):
                for c in range(nc_s):
                    w = min(512, nk - c * 512)
                    bk = (hh % 2) * 4 + c
                    K.op("pe", "matmul", [qk_, "kselT"], [("ps", bk)], out=K.bank(bk)[:, 0:w], lhsT=qT[:, H[hh], ql],
                         rhs=kselT[:, g, c * 512:c * 512 + w], start=True, stop=True)

            def p1_sel(hh):
                for c in range(nc_s):
                    w = min(512, nk - c * 512)
                    bk = (hh % 2) * 4 + c
                    K.op("dve", "scalar_tensor_tensor", ["Dsel", ("ps", bk)], [("s_sel", hh)],
                         out=s_sel[hh][:, c * 512:c * 512 + w], in0=Dsel[:, off + c * 512:off + c * 512 + w],
                         scalar=coefs[hh], in1=K.bank(bk)[:, 0:w], op0=ALU.mult, op1=ALU.add)

            def qk_win(hh):
                for c in range(nc_w):
                    w = min(512, nkw - c * 512)
                    bk = hh * 2 + c
                    K.op("pe", "matmul", [qk_, "kwinT"], [("ps", bk)], out=K.bank(bk)[:, 0:w], lhsT=qT[:, H[hh], ql],
                         rhs=kwinT[:, g, k0w + c * 512:k0w + c * 512 + w], start=True, stop=True)

            def p1_win(hh):
                for c in range(nc_w):
                    w = min(512, nkw - c * 512)
                    bk = hh * 2 + c
                    K.op("dve", "scalar_tensor_tensor", ["Dwin", ("ps", bk)], [("s_win", hh)],
                         out=s_win[hh][:, c * 512:c * 512 + w], in0=Dwin[:, coff + c * 512:coff + c * 512 + w],
                         scalar=coefs[hh], in1=K.bank(bk)[:, 0:w], op0=ALU.mult, op1=ALU.add)

            qk_sel(0)
            qk_sel(1)
            p1_sel(0)
            qk_sel(2)
            p1_sel(1)
            qk_sel(3)
            p1_sel(2)
            p1_sel(3)
            for hh in R4:
                ss = s_sel[hh][:, 0:nk]
                K.op("dve", "tensor_tensor", [("s_sel", hh), ("selbias", g)], [("s_sel", hh)], out=v3(ss, nb, 64),
                     in0=v3(ss, nb, 64), in1=sb[:, 0:nb].unsqueeze(2).broadcast_to([128, nb, 64]), op=ALU.add)
            for hh in R4:
                K.op("dve", "tensor_reduce", [("s_sel", hh)], [("stat", hh)], out=stat[hh][:, 0:1],
                     in_=s_sel[hh][:, 0:nk], axis=AX.X, op=ALU.max)
            for hh in R4:
                K.op("dve", "tensor_scalar", [("stat", hh)], [("stat", hh)], out=stat[hh][:, 1:2], in0=stat[hh][:, 0:1],
                     scalar1=-SCALE, scalar2=None, op0=ALU.mult)
            for hh in R4:
                K.op("act", "activation", [("s_sel", hh), ("stat", hh)], [("p_sel", hh), ("stat", hh)],
                     out=p_sel[hh][:, 0:nk], in_=s_sel[hh][:, 0:nk], func=AF.Exp, scale=SCALE, bias=stat[hh][:, 1:2],
                     accum_out=stat[hh][:, 2:3])
            for hh in R4:
                qk_win(hh)
            for hh in R4:
                p1_win(hh)
            for hh in R4:
                K.op("dve", "tensor_reduce", [("s_win", hh)], [("stat", 4 + hh)], out=stat[4 + hh][:, 0:1],
                     in_=s_win[hh][:, 0:nkw], axis=AX.X, op=ALU.max)
            for hh in R4:
                K.op("dve", "tensor_scalar", [("stat", 4 + hh)], [("stat", 4 + hh)], out=stat[4 + hh][:, 1:2],
                     in0=stat[4 + hh][:, 0:1], scalar1=-SCALE, scalar2=None, op0=ALU.mult)
            for hh in R4:
                K.op("act", "activation", [("s_win", hh), ("stat", 4 + hh)], [("p_win", hh), ("stat", 4 + hh)],
                     out=p_win[hh][:, 0:nkw], in_=s_win[hh][:, 0:nkw], func=AF.Exp, scale=SCALE,
                     bias=stat[4 + hh][:, 1:2], accum_out=stat[4 + hh][:, 2:3])
            for (base, pbuf, pname, n_, gi) in ((0, p_sel, "p_sel", nk, 1), (4, p_win, "p_win", nkw, 2)):
                for hh in R4:
                    K.op("dve", "reciprocal", [("stat", base + hh)], [("stat", base + hh)], out=stat[base + hh][:, 3:4],
                         in_=stat[base + hh][:, 2:3])
                for hh in R4:
                    K.op("dve", "tensor_scalar", [("stat", base + hh), "gsig"], [("stat", base + hh)],
                         out=stat[base + hh][:, 4:5], in0=stat[base + hh][:, 3:4],
                         scalar1=gsig[:, qt, H[hh] * 3 + gi:H[hh] * 3 + gi + 1], scalar2=None, op0=ALU.mult)
                for hh in R4:
                    K.op("dve", "tensor_scalar", [(pname, hh), ("stat", base + hh)], [(pname, hh)],
                         out=pbuf[hh][:, 0:n_], in0=pbuf[hh][:, 0:n_], scalar1=stat[base + hh][:, 4:5], scalar2=None,
                         op0=ALU.mult)
            for hh in R4:
                transposes(p_sel[hh], pt_sel[hh], qt + 1, ("p_sel", hh), ("pt_sel", hh))
            for hh in R4:
                transposes(p_win[hh], pt_win[hh], nkw // 128, ("p_win", hh), ("pt_win", hh))
            ob = it % 2
            it += 1
            for hh in R4:
                bk = 4 + hh
                o = K.bank(bk)[:, 0:128]
                nmm = 1 + (qt + 1) + nkw // 128
                i = 1
                K.op("pe", "matmul", ["vc", ("pcT", hh)], [("ps", bk)], out=o, lhsT=vc[0:127, g * 128:(g + 1) * 128],
                     rhs=pcT[hh][0:127, :], start=True, stop=False)
                for kt in range(qt + 1):
                    i += 1
                    K.op("pe", "matmul", ["vsel", ("pt_sel", hh)], [("ps", bk)], out=o,
                         lhsT=vsel[:, kt, g * 128:(g + 1) * 128], rhs=pt_sel[hh][:, kt * 128:(kt + 1) * 128],
                         start=False, stop=False)
                for j in range(nkw // 128):
                    i += 1
                    K.op("pe", "matmul", ["vwin", ("pt_win", hh)], [("ps", bk)], out=o,
                         lhsT=vwin[:, k0w // 128 + j, g * 128:(g + 1) * 128], rhs=pt_win[hh][:, j * 128:(j + 1) * 128],
                         start=False, stop=(i == nmm))
                K.evac(osb[ob][:, hh * 128:(hh + 1) * 128], o, [("ps", bk)], [("osb", ob)])
            K.dma("q_sp", io["s_mixT"][g * 512:(g + 1) * 512, qs].rearrange("(h p) t -> p h t", p=128),
                  v3(osb[ob], 4, 128), [("osb", ob)], [("s_mixT", qt, g)])
    S.barrier()


def phase_d(K, io):
    S = K.S
    K.reset()
    gqk = [v3(K.tile(8 * 128, BF16), 8, 128) for _ in range(2)]
    gv = v3(K.tile(NT * 1024, BF16), NT, 1024)
    og = v3(K.tile(NT * 1024, BF16), NT, 1024)
    glaT = v3(K.tile(8 * T, BF16), 8, T)
    alphaT = K.tile(T, F32)
    wa2 = K.tile(512, F32)
    ba = K.tile(512, F32)
    ones = K.tile(128, F32)
    tris = K.tile(128, F32)
    mask01 = K.tile(128, F32)
    gnb = K.tile(256, F32)
    st = v3(K.tile(4 * 256, F32), 4, 256)
    stb = v3(K.tile(4 * 256, BF16), 4, 256)
    ex2 = [K.tile(512, F32) for _ in range(2)]
    lg2 = [K.tile(512, F32) for _ in range(2)]
    eb2 = [K.tile(512, F32) for _ in range(2)]
    enb2 = [K.tile(512, F32) for _ in range(2)]
    QeT2 = [K.tile(512, BF16) for _ in range(2)]
    KeT2 = [K.tile(512, BF16) for _ in range(2)]
    ATm2 = [K.tile(512, BF16) for _ in range(2)]
    Ketm2 = [K.tile(512, BF16) for _ in range(2)]
    t12 = [K.tile(1024, F32) for _ in range(2)]
    sog2 = [K.tile(1024, F32) for _ in range(2)]
    gtm2 = [K.tile(1024, BF16) for _ in range(2)]
    junk = K.tile(256, BF16)
    rs2 = [K.tile(16, F32) for _ in range(2)]
    s1 = K.tile(256, F32)
    identb = io["identb"]

    s_gqk3 = io["s_fm"][2048:3072, :].rearrange("(h p) t -> p h t", p=128)
    K.dma("q_sp", gqk[0], s_gqk3[:, :, 0:128], [], [("gqk", 0)])
    s_tm3 = io["s_tm"].rearrange("(kt p) c -> p kt c", p=128)
    K.dma("q_sp", gv, s_tm3[:, :, 512:1536], [], ["gv"])
    K.dma("q_sp", og, s_tm3[:, :, 1536:2560], [], ["og"])
    K.dma("q_sp", alphaT[0:16, :], io["s_alphaT"][:, :], [], ["alphaT"])
    K.dma("q_sp", wa2[0:16, :], io["w_alpha2"][:, :], [], ["wa2"])
    K.dma("q_sp", ba[0:1, :], io["b_alpha"].rearrange("(a n) -> a n", a=1), [], ["ba"])
    K.dma("q_sp", tris, io["c_tris"][:, :], [], ["tris"])
    K.dma("q_sp", mask01, io["c_mask01"][:, :], [], ["mask01"])
    K.dma("q_sp", gnb, io["g_gla"].partition_broadcast(128), [], ["gnb"])
    K.op("dve", "memset", [], ["ones"], ap=ones, constant=1.0)
    K.op("dve", "memset", [], ["st"], ap=st, constant=0.0)
    K.op("pool", "memset", [], ["stb"], ap=stb, constant=0.0)

    def front(ti):
        ts_ = slice(ti * 128, (ti + 1) * 128)
        gqT = gqk[ti % 2][:, 0:4, :]
        gkT = gqk[ti % 2][:, 4:8, :]
        gkey = ("gqk", ti % 2)
        pq = ti % 2
        ex, lg, eb, enb, QeT, KeT, ATm, Ketm = ex2[pq], lg2[pq], eb2[pq], enb2[pq], QeT2[pq], KeT2[pq], ATm2[pq], Ketm2[pq]
        t1, sog, gtm, rs = t12[pq], sog2[pq], gtm2[pq], rs2[pq]
        if ti + 1 < NT:
            K.dma("q_sp", gqk[(ti + 1) % 2], s_gqk3[:, :, (ti + 1) * 128:(ti + 2) * 128], [], [("gqk", (ti + 1) % 2)])
        io["bg"].emit(3)
        K.op("pe", "matmul", ["alphaT", "wa2"], [("ps", 0)], out=K.bank(0), lhsT=alphaT[0:16, ts_], rhs=wa2[0:16, :],
             start=True, stop=False)
        K.op("pe", "matmul", ["ones", "ba"], [("ps", 0)], out=K.bank(0), lhsT=ones[0:1, :], rhs=ba[0:1, :],
             start=False, stop=True)
        K.op("act", "activation", [("ps", 0)], [("ex", pq)], out=ex, in_=K.bank(0), func=AF.Exp, scale=-1.0)
        K.op("act", "activation", [("ex", pq)], [("lg", pq)], out=lg, in_=ex, func=AF.Ln, bias=1.0)
        for h in range(4):
            K.op("pe", "matmul", [("lg", pq), "tris"], [("ps", 1)], out=K.bank(1)[:, h * 128:(h + 1) * 128],
                 lhsT=lg[:, h * 128:(h + 1) * 128], rhs=tris, start=True, stop=True)
        K.op("act", "activation", [("ps", 1)], [("eb", pq)], out=eb, in_=K.bank(1), func=AF.Exp)
        K.op("act", "activation", [("ps", 1)], [("enb", pq)], out=enb, in_=K.bank(1), func=AF.Exp, scale=-1.0)
        K.op("dve", "scalar_tensor_tensor", [("eb", pq), gkey], [("QeT", pq)], out=v3(QeT, 4, 128), in0=v3(eb, 4, 128), scalar=SCALE,
             in1=gqT, op0=ALU.mult, op1=ALU.mult)
        K.op("dve", "tensor_tensor", [("enb", pq), gkey], [("KeT", pq)], out=v3(KeT, 4, 128), in0=v3(enb, 4, 128), in1=gkT,
             op=ALU.mult)
        for h in range(4):
            K.op("pe", "matmul", [("KeT", pq), ("QeT", pq)], [("ps", 2)], out=K.bank(2)[:, h * 128:(h + 1) * 128],
                 lhsT=KeT[:, h * 128:(h + 1) * 128], rhs=QeT[:, h * 128:(h + 1) * 128], start=True, stop=True)
        K.op("dve", "tensor_tensor", [("ps", 2), "mask01"], [("ATm", pq)], out=v3(ATm, 4, 128), in0=v3(K.bank(2), 4, 128),
             in1=mask01.unsqueeze(1).broadcast_to([128, 4, 128]), op=ALU.mult)
        for h in range(4):
            K.op("pe", "transpose", [("KeT", pq), "identb"], [("ps", 5)], out=K.bank(5, 128, BF16, h * 64),
                 in_=KeT[:, h * 128:(h + 1) * 128], identity=identb)
        K.op("act", "activation", [("ps", 5)], [("Ketm", pq)], out=Ketm, in_=K.bank(5, 512, BF16, 0), func=AF.Copy)


    def mid(ti):
        ts_ = slice(ti * 128, (ti + 1) * 128)
        pq = ti % 2
        ex, lg, eb, enb, QeT, KeT, ATm, Ketm = ex2[pq], lg2[pq], eb2[pq], enb2[pq], QeT2[pq], KeT2[pq], ATm2[pq], Ketm2[pq]
        t1, sog, gtm, rs = t12[pq], sog2[pq], gtm2[pq], rs2[pq]
        for h in range(4):
            bk = 3 + h // 2
            o = K.bank(bk)[:, (h % 2) * 256:(h % 2) * 256 + 256]
            K.op("pe", "matmul", [("ATm", pq), "gv"], [("ps", bk)], out=o, lhsT=ATm[:, h * 128:(h + 1) * 128],
                 rhs=gv[:, ti, h * 256:(h + 1) * 256], start=True, stop=False)
            K.op("pe", "matmul", [("QeT", pq), "stb"], [("ps", bk)], out=o, lhsT=QeT[:, h * 128:(h + 1) * 128],
                 rhs=stb[:, h, :], start=False, stop=True)
        for h in range(4):
            bk = 6 + h // 2
            o = K.bank(bk)[:, (h % 2) * 256:(h % 2) * 256 + 256]
            K.op("pe", "matmul", [("Ketm", pq), "gv"], [("ps", bk)], out=o, lhsT=Ketm[:, h * 128:(h + 1) * 128],
                 rhs=gv[:, ti, h * 256:(h + 1) * 256], start=True, stop=True)
            ebl = eb[:, h * 128 + 127:h * 128 + 128]
            K.op("dve", "tensor_scalar", ["st", ("eb", pq)], ["s1"], out=s1, in0=st[:, h, :], scalar1=ebl, scalar2=None,
                 op0=ALU.mult)
            K.op("dve", "scalar_tensor_tensor", [("ps", bk), ("eb", pq), "s1"], ["st"], out=st[:, h, :], in0=o, scalar=ebl,
                 in1=s1, op0=ALU.mult, op1=ALU.add)
        K.op("act", "activation", ["st"], ["stb"], out=stb, in_=st, func=AF.Copy)


    def outp(ti):
        ts_ = slice(ti * 128, (ti + 1) * 128)
        pq = ti % 2
        ex, lg, eb, enb, QeT, KeT, ATm, Ketm = ex2[pq], lg2[pq], eb2[pq], enb2[pq], QeT2[pq], KeT2[pq], ATm2[pq], Ketm2[pq]
        t1, sog, gtm, rs = t12[pq], sog2[pq], gtm2[pq], rs2[pq]
        for h in range(4):
            bk = 3 + h // 2
            o = K.bank(bk)[:, (h % 2) * 256:(h % 2) * 256 + 256]
            K.op("act", "activation", [("ps", bk)], ["junk", ("rs", pq)], out=junk, in_=o, func=AF.Square,
                 accum_out=rs[:, h:h + 1])
        K.op("dve", "tensor_scalar", [("rs", pq)], [("rs", pq)], out=rs[:, 4:8], in0=rs[:, 0:4], scalar1=1.0 / 256, scalar2=EPS,
             op0=ALU.mult, op1=ALU.add)
        K.op("act", "activation", [("rs", pq)], [("rs", pq)], out=rs[:, 8:12], in_=rs[:, 4:8], func=AF.Sqrt)
        K.op("dve", "reciprocal", [("rs", pq)], [("rs", pq)], out=rs[:, 12:16], in_=rs[:, 8:12])
        K.op("act", "activation", ["og"], [("sog", pq)], out=sog, in_=og[:, ti, :], func=AF.Silu)
        for h in range(4):
            bk = 3 + h // 2
            o = K.bank(bk)[:, (h % 2) * 256:(h % 2) * 256 + 256]
            K.op("dve", "scalar_tensor_tensor", [("ps", bk), ("rs", pq), "gnb"], [("t1", pq)], out=t1[:, h * 256:(h + 1) * 256], in0=o,
                 scalar=rs[:, 12 + h:13 + h], in1=gnb, op0=ALU.mult, op1=ALU.mult)
        K.op("dve", "tensor_tensor", [("t1", pq), ("sog", pq)], [("gtm", pq)], out=gtm, in0=t1, in1=sog, op=ALU.mult)
        for fc in range(8):
            K.op("pe", "transpose", [("gtm", pq), "identb"], [("ps", 5)], out=K.bank(5, 128, BF16, fc * 64),
                 in_=gtm[:, fc * 128:(fc + 1) * 128], identity=identb)
        K.evac(glaT[:, :, ts_], v3(K.bank(5, 1024, BF16, 0), 8, 128), [("ps", 5)], [("glaT", ti)])

    front(0)
    for ti in range(NT):
        mid(ti)
        if ti + 1 < NT:
            front(ti + 1)
        outp(ti)
    gk = [("glaT", ti) for ti in range(NT)]
    for fc in range(8):
        K.dma("q_sp", io["s_mixT"][1024 + fc * 128:1024 + (fc + 1) * 128, :], glaT[:, fc, :], gk, [("s_mixT", 8 + fc)])
    S.barrier()


def phase_e(K, io):
    S = K.S
    K.reset()
    mixs = [v3(K.tile(KC * 128, BF16), KC, 128) for _ in range(2)]
    wout = v3(K.tile(KC * D, BF16), KC, D)
    xt = [K.tile(D, F32) for _ in range(2)]
    xn = [K.tile(D, F32) for _ in range(2)]
    junk = K.tile(D, BF16)
    hTf = [K.tile(KC * 128, F32) for _ in range(2)]
    wr = v3(K.tile(KC * 36, F32), KC, 36)
    brt = K.tile(36, F32)
    lgt = [K.tile(36, F32) for _ in range(2)]
    sm = [K.tile(8, F32) for _ in range(2)]
    sr = [K.tile(24, F32) for _ in range(2)]
    oh = [K.tile(4, F32) for _ in range(2)]
    esel = [K.tile(8, F32) for _ in range(2)]
    exs = [K.tile(8, F32) for _ in range(2)]
    msk = [K.tile(8, F32) for _ in range(2)]
    mx8 = [K.tile(8, F32) for _ in range(2)]
    C3 = v3(io["C"], NT, 32)
    gffn, identf = io["gffn"], io["identf"]
    gfb = K.tile(D, F32)
    h2t = [K.tile(D, BF16) for _ in range(2)]
    K.dma("q_sp", gfb, io["g_ffn"].partition_broadcast(128), [], ["gfb"])
    s_mix3 = io["s_mixT"].rearrange("(kc p) t -> p kc t", p=128)
    K.dma("q_sp", mixs[0], s_mix3[:, :, 0:128], [], [("mix", 0)])
    w3 = io["w_out"].rearrange("(kc p) c -> p kc c", p=128)
    for j in range(4):
        K.dma("q_pool", wout[:, j * 4:(j + 1) * 4, :], w3[:, j * 4:(j + 1) * 4, :], [], [("wout", j)])
    wk = [("wout", j) for j in range(4)]
    K.dma("q_sp", wr[:, :, 0:4], io["w_rg"].rearrange("(kc p) c -> p kc c", p=128), [], ["wr0"])
    K.dma("q_sp", wr[:, :, 4:36], io["w_re"].rearrange("(kc p) c -> p kc c", p=128), [], ["wr1"])
    K.dma("q_sp", brt[:, 0:4], io["b_rg"].partition_broadcast(128), [], ["br0"])
    K.dma("q_sp", brt[:, 4:36], io["b_re"].partition_broadcast(128), [], ["br1"])

    def stage1(ti):
        b = ti % 2
        ts_ = slice(ti * 128, (ti + 1) * 128)
        xk, smk = ("xt", b), ("sm", b)
        if ti == 0:
            K.dma("q_sp", xt[0], io["x"][0:128, :], [], [("xt", 0)])
        if ti + 1 < NT:
            K.dma("q_sp", xt[(ti + 1) % 2], io["x"][(ti + 1) * 128:(ti + 2) * 128, :], [], [("xt", (ti + 1) % 2)])
            K.dma("q_sp", mixs[(ti + 1) % 2], s_mix3[:, :, (ti + 1) * 128:(ti + 2) * 128], [], [("mix", (ti + 1) % 2)])
        io["bg"].emit(2)
        for c in range(4):
            for kc in range(KC):
                K.op("pe", "matmul", [("mix", b)] + wk, [("ps", c)], out=K.bank(c), lhsT=mixs[b][:, kc, :],
                     rhs=wout[:, kc, c * 512:(c + 1) * 512], start=(kc == 0), stop=(kc == KC - 1))
        for c in range(4):
            K.op("dve", "tensor_tensor", [("ps", c), xk], [xk], out=xt[b][:, c * 512:(c + 1) * 512], in0=K.bank(c),
                 in1=xt[b][:, c * 512:(c + 1) * 512], op=ALU.add)
        K.dma("q_sp", io["s_x1"][ts_, :], xt[b], [xk], [("s_x1", ti)])
        K.op("act", "activation", [xk], ["junk", smk], out=junk, in_=xt[b], func=AF.Square, accum_out=sm[b][:, 0:1])
        K.op("dve", "tensor_scalar", [smk], [smk], out=sm[b][:, 1:2], in0=sm[b][:, 0:1], scalar1=1.0 / D, scalar2=EPS,
             op0=ALU.mult, op1=ALU.add)
        K.op("act", "activation", [smk], [smk], out=sm[b][:, 2:3], in_=sm[b][:, 1:2], func=AF.Sqrt)
        K.op("dve", "reciprocal", [smk], [smk], out=sm[b][:, 3:4], in_=sm[b][:, 2:3])
        K.op("dve", "tensor_scalar", [xk, smk], [("xn", b)], out=xn[b], in0=xt[b], scalar1=sm[b][:, 3:4], scalar2=None,
             op0=ALU.mult)
        K.op("pool", "tensor_tensor", [("xn", b), "gfb"], [("h2t", b)], out=h2t[b], in0=xn[b], in1=gfb, op=ALU.mult)
        K.dma("q_sp", io["s_h2"][ts_, :], h2t[b], [("h2t", b)], [("s_h2", ti)])

    def stage2(ti):
        b = ti % 2
        hk, lk, rk = ("hTf", b), ("lgt", b), ("sr", b)
        r = sr[b]
        for q4 in range(4):
            bk = 4 + q4
            for j in range(4):
                kc = q4 * 4 + j
                K.op("pe", "transpose", [("xn", b), "identf"], [("ps", bk)], out=K.bank(bk, 128, F32, j * 128),
                     in_=xn[b][:, kc * 128:(kc + 1) * 128], identity=identf)
            for j in range(4):
                kc = q4 * 4 + j
                K.op("act", "activation", [("ps", bk), "gffn"], [hk], out=hTf[b][:, kc * 128:(kc + 1) * 128],
                     in_=K.bank(bk, 128, F32, j * 128), func=AF.Copy, scale=gffn[:, kc:kc + 1])
        for kc in range(KC):
            K.op("pe", "matmul", [hk, "wr0", "wr1"], [("ps", 4 + b)], out=K.bank(4 + b)[:, 0:36],
                 lhsT=hTf[b][:, kc * 128:(kc + 1) * 128], rhs=wr[:, kc, :], start=(kc == 0), stop=(kc == KC - 1))
        K.op("dve", "tensor_tensor", [("ps", 4 + b), "br0", "br1"], [lk], out=lgt[b], in0=K.bank(4 + b)[:, 0:36], in1=brt,
             op=ALU.add)
        L = lgt[b]
        K.op("dve", "tensor_reduce", [lk], [rk], out=r[:, 8:9], in_=L[:, 0:4], axis=AX.X, op=ALU.max)
        K.op("dve", "tensor_scalar", [rk], [rk], out=r[:, 9:10], in0=r[:, 8:9], scalar1=-1.0, scalar2=None, op0=ALU.mult)
        K.op("act", "activation", [lk, rk], [("oh", b), rk], out=oh[b], in_=L[:, 0:4], func=AF.Exp, bias=r[:, 9:10],
             accum_out=r[:, 10:11])
        K.op("dve", "tensor_scalar", [lk, rk], [("oh", b)], out=oh[b], in0=L[:, 0:4], scalar1=r[:, 8:9], scalar2=None,
             op0=ALU.is_ge)
        K.op("dve", "tensor_scalar", [lk, ("oh", b)], [("esel", b)], out=esel[b], in0=L[:, 4:12], scalar1=oh[b][:, 0:1],
             scalar2=None, op0=ALU.mult)
        for g in range(1, 4):
            K.op("dve", "scalar_tensor_tensor", [lk, ("oh", b), ("esel", b)], [("esel", b)], out=esel[b],
                 in0=L[:, 4 + 8 * g:12 + 8 * g], scalar=oh[b][:, g:g + 1], in1=esel[b], op0=ALU.mult, op1=ALU.add)
        K.op("dve", "max", [("esel", b)], [("mx8", b)], out=mx8[b], in_=esel[b])
        K.op("dve", "tensor_scalar", [("mx8", b)], [rk], out=r[:, 12:13], in0=mx8[b][:, 0:1], scalar1=-1.0, scalar2=None,
             op0=ALU.mult)
        K.op("act", "activation", [("esel", b), rk], [("exs", b)], out=exs[b], in_=esel[b], func=AF.Exp, bias=r[:, 12:13])
        K.op("dve", "tensor_scalar", [("esel", b), ("mx8", b)], [("msk", b)], out=msk[b], in0=esel[b],
             scalar1=mx8[b][:, 1:2], scalar2=None, op0=ALU.is_ge)
        K.op("act", "activation", [("mx8", b), rk], [rk], out=r[:, 13:14], in_=mx8[b][:, 1:2], func=AF.Exp,
             bias=r[:, 12:13])
        K.op("dve", "tensor_scalar", [rk], [rk], out=r[:, 14:15], in0=r[:, 13:14], scalar1=1.0, scalar2=r[:, 10:11],
             op0=ALU.add, op1=ALU.mult)
        K.op("dve", "reciprocal", [rk], [rk], out=r[:, 15:16], in_=r[:, 14:15])
        K.op("dve", "tensor_scalar", [("oh", b), rk], [rk], out=r[:, 16:20], in0=oh[b], scalar1=r[:, 15:16], scalar2=None,
             op0=ALU.mult)
        for g in range(4):
            K.op("dve", "scalar_tensor_tensor", [("exs", b), rk, ("msk", b)], ["C"], out=C3[:, ti, g * 8:(g + 1) * 8],
                 in0=exs[b], scalar=r[:, 16 + g:17 + g], in1=msk[b], op0=ALU.mult, op1=ALU.mult)

    stage1(0)
    for ti in range(NT):
        if ti + 1 < NT:
            stage1(ti + 1)
        stage2(ti)
    S.barrier()


def phase_f(K, io):
    S = K.S
    C3 = v3(io["C"], NT, 32)
    for half in range(2):
        K.reset()
        h2T = v3(K.tile(KC * 1024, BF16), KC, 1024)
        acc = v3(K.tile(8 * D, F32), 8, D)
        hid = [v3(K.tile(2 * 1024, BF16), 2, 1024) for _ in range(2)]
        sg = [K.tile(512, BF16) for _ in range(2)]
        mark = K.off
        wg = [v3(K.tile(KC * 256, BF16), KC, 256) for _ in range(2)]
        wu = [v3(K.tile(KC * 256, BF16), KC, 256) for _ in range(2)]
        wd = [v3(K.tile(2 * D, BF16), 2, D) for _ in range(2)]
        K.dma("q_sp", h2T, io["s_h2T"].rearrange("(kc p) t -> p kc t", p=128)[:, :, half * 1024:(half + 1) * 1024],
              [], ["h2T"])
        K.op("dve", "memset", [], ["acc%d" % i for i in range(8)], ap=acc, constant=0.0)
        units = [(e, fh) for e in range(32) for fh in range(2)]
        gbc = [0]

        def load_gu(u):
            e, fh = units[u]
            g, ee = e // 8, e % 8
            wb = u % 2
            fs = slice(fh * 256, (fh + 1) * 256)
            K.dma("q_pool", wg[wb], io["w_eg"][g, ee].rearrange("(kc p) f -> p kc f", p=128)[:, :, fs], [], [("wg", wb)])
            K.dma("q_pool", wu[wb], io["w_eu"][g, ee].rearrange("(kc p) f -> p kc f", p=128)[:, :, fs], [], [("wu", wb)])

        def load_d(u):
            e, fh = units[u]
            g, ee = e // 8, e % 8
            wb = u % 2
            fs = slice(fh * 256, (fh + 1) * 256)
            K.dma("q_pool", wd[wb], io["w_ed"][g, ee][fs, :].rearrange("(fc p) d -> p fc d", p=128), [], [("wd", wb)])

        def gate_up_piece(u, piece):
            wb = u % 2
            fc, tb = piece // 2, piece % 2
            b0 = (gbc[0] % 2) * 2
            gbc[0] += 1
            for (bk, w, wkey) in ((b0, wg[wb], ("wg", wb)), (b0 + 1, wu[wb], ("wu", wb))):
                for kc in range(KC):
                    K.op("pe", "matmul", [wkey, "h2T"], [("ps", bk)], out=K.bank(bk),
                         lhsT=w[:, kc, fc * 128:(fc + 1) * 128], rhs=h2T[:, kc, tb * 512:(tb + 1) * 512],
                         start=(kc == 0), stop=(kc == KC - 1))
            sb_ = gbc[0] % 2
            K.op("act", "activation", [("ps", b0)], [("sg", sb_)], out=sg[sb_], in_=K.bank(b0), func=AF.Silu)
            K.op("dve", "tensor_tensor", [("ps", b0 + 1), ("sg", sb_)], [("hid", wb, fc, tb)],
                 out=hid[wb][:, fc, tb * 512:(tb + 1) * 512], in0=K.bank(b0 + 1), in1=sg[sb_], op=ALU.mult)

        def down_tile(u, ti):
            e, fh = units[u]
            wb = u % 2
            hk = [("hid", wb, fc, ti // 4) for fc in range(2)]
            for c in range(4):
                bk = 4 + c
                for fc in range(2):
                    K.op("pe", "matmul", hk + [("wd", wb)], [("ps", bk)], out=K.bank(bk),
                         lhsT=hid[wb][:, fc, ti * 128:(ti + 1) * 128], rhs=wd[wb][:, fc, c * 512:(c + 1) * 512],
                         start=(fc == 0), stop=(fc == 1))
                K.op("dve", "scalar_tensor_tensor", [("ps", bk), "C", "acc%d" % ti], ["acc%d" % ti],
                     out=acc[:, ti, c * 512:(c + 1) * 512], in0=K.bank(bk),
                     scalar=C3[:, half * 8 + ti, e:e + 1], in1=acc[:, ti, c * 512:(c + 1) * 512],
                     op0=ALU.mult, op1=ALU.add)

        nu = len(units)
        load_gu(0)
        load_d(0)
        load_gu(1)
        load_d(1)
        for p in range(4):
            gate_up_piece(0, p)
        for u in range(nu):
            if u + 2 < nu:
                load_gu(u + 2)
            for ti in range(8):
                down_tile(u, ti)
                if ti % 2 == 1 and u + 1 < nu:
                    gate_up_piece(u + 1, ti // 2)
            if u + 2 < nu:
                load_d(u + 2)
        S.barrier()
        K.off = mark
        gfin = K.tile(D, F32)
        x1t = [K.tile(D, F32) for _ in range(2)]
        junk = K.tile(D, BF16)
        sm = [K.tile(8, F32) for _ in range(2)]
        K.dma("q_sp", gfin, io["g_final"].partition_broadcast(128), [], ["gfin"])
        for ti in range(8):
            b = ti % 2
            tg = half * 8 + ti
            xk, sk = ("x1t", b), ("smf", b)
            K.dma("q_sp", x1t[b], io["s_x1"][tg * 128:(tg + 1) * 128, :], [], [xk])
            K.op("dve", "tensor_tensor", [xk, "acc%d" % ti], [xk], out=x1t[b], in0=x1t[b], in1=acc[:, ti, :], op=ALU.add)
            K.op("act", "activation", [xk], ["junkf", sk], out=junk, in_=x1t[b], func=AF.Square, accum_out=sm[b][:, 0:1])
            K.op("dve", "tensor_scalar", [sk], [sk], out=sm[b][:, 1:2], in0=sm[b][:, 0:1], scalar1=1.0 / D, scalar2=EPS,
                 op0=ALU.mult, op1=ALU.add)
            K.op("act", "activation", [sk], [sk], out=sm[b][:, 2:3], in_=sm[b][:, 1:2], func=AF.Sqrt)
            K.op("dve", "reciprocal", [sk], [sk], out=sm[b][:, 3:4], in_=sm[b][:, 2:3])
            K.op("dve", "scalar_tensor_tensor", [xk, sk, "gfin"], [xk], out=x1t[b], in0=x1t[b], scalar=sm[b][:, 3:4],
                 in1=gfin, op0=ALU.mult, op1=ALU.mult)
            K.dma("q_sp", io["y"][tg * 128:(tg + 1) * 128, :], x1t[b], [xk], [("y", tg)])
        S.barrier()


I32 = mybir.dt.int32
NTILE = 48
TS = 256
NSLOT = NTILE * TS


class BgCast:
    def __init__(self, K, io, stg):
        self.K, self.io, self.stg = K, io, stg
        self.jobs = []
        for e in range(32):
            g, ee = e // 8, e % 8
            for (src, dst, kind) in ((io["w_eg"], io["s_wgb"], 0), (io["w_eu"], io["s_wub"], 0), (io["w_ed"], io["s_wdb"], 1)):
                for q in range(4):
                    self.jobs.append((src, dst, kind, e, g, ee, q))
        self.i = 0

    def emit(self, n):
        K = self.K
        for _ in range(n):
            if self.i >= len(self.jobs):
                return
            src, dst, kind, e, g, ee, q = self.jobs[self.i]
            b = self.i % len(self.stg)
            self.i += 1
            st = self.stg[b]
            if kind == 0:
                K.dma("q_pool", v3(st, 4, 512), src[g, ee].rearrange("(kc p) f -> p kc f", p=128)[:, q * 4:(q + 1) * 4, :],
                      [], [("stg", b)])
            else:
                K.dma("q_pool", st, src[g, ee][q * 128:(q + 1) * 128, :], [], [("stg", b)])
            K.dma("q_sp", dst[e * 128:(e + 1) * 128, q * 2048:(q + 1) * 2048], st, [("stg", b)], [("wb", self.i)])


def phase_w(K, io):
    io["bg"].emit(100000)
    K.S.barrier()


def phase_e2(K, io):
    S = K.S
    K.reset()
    C3 = v3(io["C"], NT, 32)
    Cf = io["C"]
    Mnz = K.tile(512, F32)
    M1 = K.tile(512, F32)
    M2 = K.tile(512, F32)
    slotmat = K.tile(512, F32)
    tmp = K.tile(512, F32)
    ones = K.tile(128, F32)
    stri = K.tile(128, F32)
    thr = K.tile(8, F32)
    jidx = K.tile(NTILE, F32)
    pcol = K.tile(1, F32)
    n_ = K.tile(32, F32)
    cmp3 = K.tile(256, F32)
    tiles = K.tile(32, F32)
    one32 = K.tile(32, F32)
    tend = K.tile(32, F32)
    tbase = K.tile(32, F32)
    rowmax = K.tile(NT, F32)
    sl = K.tile(2 * NT, F32)
    cmpj = K.tile(NTILE * 32, F32)
    eid = K.tile(NTILE, F32)
    widf = K.tile(NTILE, F32)
    K.dma("q_sp", stri, io["c_stri"][:, :], [], ["stri"])
    K.dma("q_sp", thr, io["c_thr"][:, :], [], ["thr"])
    K.dma("q_sp", jidx, io["c_jidx"][:, :], [], ["jidx"])
    K.dma("q_sp", pcol, io["c_pcol"][:, :], [], ["pcol"])
    K.op("dve", "memset", [], ["ones"], ap=ones, constant=1.0)
    K.op("dve", "memset", [], ["one32"], ap=one32, constant=1.0)
    K.op("dve", "tensor_scalar", ["C"], ["Mnz"], out=Mnz, in0=Cf, scalar1=0.0, scalar2=None, op0=ALU.is_gt)
    Mnz3 = v3(Mnz, NT, 32)
    for ti in range(NT):
        o = K.bank(0)[:, ti * 32:(ti + 1) * 32]
        K.op("pe", "matmul", ["stri", "Mnz"], [("ps", 0)], out=o, lhsT=stri, rhs=Mnz3[:, ti, :], start=True, stop=(ti == 0))
        for tj in range(ti):
            K.op("pe", "matmul", ["ones", "Mnz"], [("ps", 0)], out=o, lhsT=ones, rhs=Mnz3[:, tj, :], start=False,
                 stop=(tj == ti - 1))
    for ti in range(NT):
        K.op("pe", "matmul", ["ones", "Mnz"], [("ps", 1)], out=K.bank(1)[:, 0:32], lhsT=ones, rhs=Mnz3[:, ti, :],
             start=(ti == 0), stop=(ti == NT - 1))
    K.op("dve", "tensor_copy", [("ps", 1)], ["n"], out=n_, in_=K.bank(1)[:, 0:32])
    K.op("dve", "tensor_tensor", ["n", "thr"], ["cmp3"], out=v3(cmp3, 32, 8), in0=n_.unsqueeze(2).broadcast_to([128, 32, 8]),
         in1=thr.unsqueeze(1).broadcast_to([128, 32, 8]), op=ALU.is_gt)
    K.op("dve", "tensor_reduce", ["cmp3"], ["tiles"], out=tiles, in_=v3(cmp3, 32, 8), axis=AX.X, op=ALU.add)
    K.op("dve", "tensor_tensor_scan", ["tiles", "one32"], ["tend"], out=tend, data0=one32, data1=tiles, initial=0.0,
         op0=ALU.mult, op1=ALU.add)
    K.op("dve", "tensor_tensor", ["tend", "tiles"], ["tbase"], out=tbase, in0=tend, in1=tiles, op=ALU.subtract)
    K.op("dve", "scalar_tensor_tensor", ["tbase", ("ps", 0)], ["slotmat"], out=v3(slotmat, NT, 32),
         in0=tbase.unsqueeze(1).broadcast_to([128, NT, 32]), scalar=float(TS), in1=v3(K.bank(0), NT, 32),
         op0=ALU.mult, op1=ALU.add)
    K.op("dve", "tensor_reduce", ["C"], ["rowmax"], out=rowmax, in_=C3, axis=AX.X, op=ALU.max)
    K.op("dve", "tensor_tensor", ["C", "rowmax"], ["M1"], out=v3(M1, NT, 32), in0=C3,
         in1=rowmax.unsqueeze(2).broadcast_to([128, NT, 32]), op=ALU.is_ge)
    K.op("dve", "tensor_tensor", ["Mnz", "M1"], ["M2"], out=M2, in0=Mnz, in1=M1, op=ALU.subtract)
    W2 = io["w12"]
    K.op("dve", "tensor_copy", ["rowmax"], ["w12"], out=W2[:, 0:NT], in_=rowmax)
    for (m, mk, k) in ((M1, "M1", 0), (M2, "M2", 1)):
        K.op("dve", "tensor_tensor", [mk, "slotmat"], ["tmp"], out=tmp, in0=m, in1=slotmat, op=ALU.mult)
        K.op("dve", "tensor_reduce", ["tmp"], ["sl"], out=sl[:, k * NT:(k + 1) * NT], in_=v3(tmp, NT, 32), axis=AX.X,
             op=ALU.add)
    K.op("dve", "tensor_tensor", ["M2", "C"], ["tmp"], out=tmp, in0=M2, in1=Cf, op=ALU.mult)
    K.op("dve", "tensor_reduce", ["tmp"], ["w12"], out=W2[:, NT:2 * NT], in_=v3(tmp, NT, 32), axis=AX.X, op=ALU.add)
    K.op("dve", "tensor_copy", ["sl"], ["slot"], out=io["slot"], in_=sl)
    K.op("dve", "tensor_tensor", ["tend", "jidx"], ["cmpj"], out=v3(cmpj, NTILE, 32),
         in0=tend.unsqueeze(1).broadcast_to([128, NTILE, 32]), in1=jidx.unsqueeze(2).broadcast_to([128, NTILE, 32]),
         op=ALU.is_le)
    K.op("dve", "tensor_reduce", ["cmpj"], ["eid"], out=eid, in_=v3(cmpj, NTILE, 32), axis=AX.X, op=ALU.add)
    rowb = K.tile(2 * NTILE, F32)
    unus = K.tile(NTILE, F32)
    K.dma("q_sp", rowb, io["c_rowbase"][:, :], [], ["rowb"])
    K.op("dve", "tensor_scalar", ["eid"], ["unus"], out=unus, in0=eid, scalar1=31.5, scalar2=1.0e6, op0=ALU.is_gt,
         op1=ALU.mult)
    K.op("dve", "tensor_tensor", ["rowb", "unus"], ["rowb"], out=v3(rowb, NTILE, 2), in0=v3(rowb, NTILE, 2),
         in1=unus.unsqueeze(2).broadcast_to([128, NTILE, 2]), op=ALU.add)
    K.op("dve", "tensor_copy", ["rowb"], ["ridx"], out=io["ridx"], in_=rowb)
    K.op("dve", "tensor_scalar", ["eid", "pcol"], ["widf"], out=widf, in0=eid, scalar1=128.0, scalar2=pcol[:, 0:1],
         op0=ALU.mult, op1=ALU.add)
    K.op("dve", "tensor_copy", ["widf"], ["widx"], out=io["widx"], in_=widf)
    if "dbg_slot" in io:
        K.dma("q_sp", io["dbg_slot"][:, :], sl, ["sl"], ["dbg_slot"])
        K.dma("q_sp", io["dbg_w12"][:, :], W2, ["w12"], ["dbg_w12"])
        K.dma("q_sp", io["dbg_wid"][:, :], widf, ["widf"], ["dbg_wid"])
    S.barrier()


def phase_fs(K, io):
    S = K.S
    K.reset()
    slot, widx, W2, ridx = io["slot"], io["widx"], io["w12"], io["ridx"]
    regs = {}

    def breg(e, v):
        if v not in regs:
            regs[v] = e.to_reg(v)
        return regs[v]

    IOA = bass.IndirectOffsetOnAxis
    ht = [K.tile(D, BF16) for _ in range(2)]
    for ti in range(NT):
        b = ti % 2
        K.dma("q_sp", ht[b], io["s_h2"][ti * 128:(ti + 1) * 128, :], [], [("ht", b)])
        for k in range(2):
            S.op("pool", lambda e, b=b, k=k, ti=ti: e.indirect_dma_start(
                out=io["s_xs"], out_offset=IOA(ap=slot[:, k * NT + ti:k * NT + ti + 1], axis=0), in_=ht[b], in_offset=None),
                [("ht", b), "slot"], [("xs", ti, k)], dma="q_pool")
    S.barrier()
    K.reset()
    wg = [K.tile(8192, BF16) for _ in range(2)]
    wu = [K.tile(8192, BF16) for _ in range(2)]
    wd = [K.tile(8192, BF16) for _ in range(2)]
    xs = [K.tile(2 * D, BF16) for _ in range(2)]
    xT = [K.tile(KC * TS, BF16) for _ in range(2)]
    hid = [K.tile(4 * TS, BF16) for _ in range(2)]
    sg = [K.tile(TS, BF16) for _ in range(2)]
    yt = [K.tile(D, BF16) for _ in range(2)]
    identb = io["identb"]
    yb = 0
    for j in range(NTILE):
        b = j % 2
        for (wt, src, nm) in ((wg[b], io["s_wgb"], "wg"), (wu[b], io["s_wub"], "wu"), (wd[b], io["s_wdb"], "wd")):
            S.op("pool", lambda e, wt=wt, src=src, j=j: e.indirect_dma_start(
                out=wt, out_offset=None, in_=src, in_offset=IOA(ap=widx[:, j:j + 1], axis=0),
                bounds_check=breg(e, 4095), oob_is_err=False),
                ["widx"], [(nm, b)], dma="q_pool")
        xs3 = v3(xs[b], 2, D)
        for sh in range(2):
            S.op("pool", lambda e, o=xs3[:, sh, :], c=2 * j + sh: e.indirect_dma_start(
                out=o, out_offset=None, in_=io["s_xs"], in_offset=IOA(ap=ridx[:, c:c + 1], axis=0),
                bounds_check=breg(e, NSLOT - 1), oob_is_err=False),
                ["ridx"], [("xsb", b)] if sh == 0 else [("xsb2", b)], dma="q_pool")
        xT3 = v3(xT[b], KC, TS)
        for sh in range(2):
            for q2 in range(2):
                bk = sh * 2 + q2
                for i in range(8):
                    kc = q2 * 8 + i
                    K.op("pe", "transpose", [("xsb", b), ("xsb2", b), "identb"], [("ps", bk)],
                         out=K.bank(bk, 128, BF16, i * 64), in_=xs3[:, sh, kc * 128:(kc + 1) * 128], identity=identb)
                K.evac(xT3[:, q2 * 8:(q2 + 1) * 8, sh * 128:(sh + 1) * 128], v3(K.bank(bk, 1024, BF16, 0), 8, 128),
                       [("ps", bk)], [("xT", b)])
        wg3, wu3, wd3 = v3(wg[b], KC, 512), v3(wu[b], KC, 512), v3(wd[b], 4, D)
        hid3 = v3(hid[b], 4, TS)
        for fc in range(4):
            bk = 4 + fc % 2
            for (w3_, nm, o0) in ((wg3, "wg", 0), (wu3, "wu", 256)):
                for kc in range(KC):
                    K.op("pe", "matmul", [(nm, b), ("xT", b)], [("ps", bk)], out=K.bank(bk)[:, o0:o0 + TS],
                         lhsT=w3_[:, kc, fc * 128:(fc + 1) * 128], rhs=xT3[:, kc, :], start=(kc == 0 and o0 == 0),
                         stop=(kc == KC - 1 and o0 == 256), skip_group_check=True)
            sb_ = fc % 2
            K.op("act", "activation", [("ps", bk)], [("sg", sb_)], out=sg[sb_], in_=K.bank(bk)[:, 0:TS], func=AF.Silu)
            K.op("dve", "tensor_tensor", [("ps", bk), ("sg", sb_)], [("hid", b)], out=hid3[:, fc, :],
                 in0=K.bank(bk)[:, TS:2 * TS], in1=sg[sb_], op=ALU.mult)
        for sh in range(2):
            y_ = yt[yb % 2]
            yk = ("yt", yb % 2)
            yb += 1
            for c in range(4):
                bk = 6 + c % 2
                for fc in range(4):
                    K.op("pe", "matmul", [("hid", b), ("wd", b)], [("ps", bk)], out=K.bank(bk),
                         lhsT=hid3[:, fc, sh * 128:(sh + 1) * 128], rhs=wd3[:, fc, c * 512:(c + 1) * 512],
                         start=(fc == 0), stop=(fc == 3))
                K.evac(y_[:, c * 512:(c + 1) * 512], K.bank(bk), [("ps", bk)], [yk])
            K.dma("q_sp", io["s_ys"][j * TS + sh * 128:j * TS + (sh + 1) * 128, :], y_, [yk], [("ys", j, sh)])
    S.barrier()
    K.reset()
    gfin = K.tile(D, F32)
    x1t = [K.tile(D, F32) for _ in range(2)]
    y1 = [K.tile(D, BF16) for _ in range(2)]
    y2 = [K.tile(D, BF16) for _ in range(2)]
    junk = K.tile(D, BF16)
    sm = [K.tile(8, F32) for _ in range(2)]
    K.dma("q_sp", gfin, io["g_final"].partition_broadcast(128), [], ["gfin"])
    def f2_loads(ti):
        b = ti % 2
        K.dma("q_sp", x1t[b], io["s_x1"][ti * 128:(ti + 1) * 128, :], [], [("x1t", b)])
        for (yy, nm, k) in ((y1[b], "y1", 0), (y2[b], "y2", 1)):
            S.op("pool", lambda e, yy=yy, k=k, ti=ti: e.indirect_dma_start(
                out=yy, out_offset=None, in_=io["s_ys"], in_offset=IOA(ap=slot[:, k * NT + ti:k * NT + ti + 1], axis=0)),
                ["slot"], [(nm, b)], dma="q_pool")

    f2_loads(0)
    for ti in range(NT):
        b = ti % 2
        xk, sk = ("x1t", b), ("smf", b)
        if ti + 1 < NT:
            f2_loads(ti + 1)
        K.op("dve", "scalar_tensor_tensor", [("y1", b), "w12", xk], [xk], out=x1t[b], in0=y1[b], scalar=W2[:, ti:ti + 1],
             in1=x1t[b], op0=ALU.mult, op1=ALU.add)
        K.op("dve", "scalar_tensor_tensor", [("y2", b), "w12", xk], [xk], out=x1t[b], in0=y2[b],
             scalar=W2[:, NT + ti:NT + ti + 1], in1=x1t[b], op0=ALU.mult, op1=ALU.add)
        K.op("act", "activation", [xk], ["junkf", sk], out=junk, in_=x1t[b], func=AF.Square, accum_out=sm[b][:, 0:1])
        K.op("dve", "tensor_scalar", [sk], [sk], out=sm[b][:, 1:2], in0=sm[b][:, 0:1], scalar1=1.0 / D, scalar2=EPS,
             op0=ALU.mult, op1=ALU.add)
        K.op("act", "activation", [sk], [sk], out=sm[b][:, 2:3], in_=sm[b][:, 1:2], func=AF.Sqrt)
        K.op("dve", "reciprocal", [sk], [sk], out=sm[b][:, 3:4], in_=sm[b][:, 2:3])
        K.op("dve", "scalar_tensor_tensor", [xk, sk, "gfin"], [xk], out=x1t[b], in0=x1t[b], scalar=sm[b][:, 3:4],
             in1=gfin, op0=ALU.mult, op1=ALU.mult)
        K.dma("q_sp", io["y"][ti * 128:(ti + 1) * 128, :], x1t[b], [xk], [("y", ti)])
    S.barrier()


def build_nc(upto="a", debug=False):
    nc = bass.Bass("TRN2", target_bir_lowering=False)
    io = {}

    def inp(name, shape, dt=F32):
        io[name] = nc.dram_tensor(name, list(shape), dt, kind="ExternalInput").ap()

    def scratch(name, shape, dt):
        kind = "ExternalOutput" if debug else "Internal"
        io[name] = nc.dram_tensor(name, list(shape), dt, kind=kind).ap()

    inp("x", [T, D])
    inp("w_in", [D, W_IN])
    inp("c_identf", [128, 128])
    inp("c_gmix", [128, KC])
    inp("b_gate", [24])
    for kv in "kv":
        inp("w_cmp_%s1" % kv, [4096, 256])
        inp("w_cmp_%s2" % kv, [256, 128])
        inp("c_pos%s" % kv, [128, 32])
    inp("c_dsel", [128, 2048])
    inp("c_dwin", [128, 640])
    inp("c_dcmp", [128, NT * 127])
    inp("c_rowvalid", [128, NT])
    inp("c_mulmask", [128, NT * 32])
    inp("c_addmask", [128, NT * 32])
    inp("c_overlap", [127, 32])
    inp("w_alpha2", [16, 512])
    inp("b_alpha", [512])
    inp("g_gla", [256])
    inp("c_tris", [128, 128])
    inp("w_out", [D, D])
    inp("c_gffn", [128, KC])
    inp("w_rg", [D, 4])
    inp("w_re", [D, 32])
    inp("b_rg", [4])
    inp("b_re", [32])
    inp("w_eg", [4, 8, D, 512])
    inp("w_eu", [4, 8, D, 512])
    inp("w_ed", [4, 8, 512, D])
    inp("g_final", [D])
    inp("c_mask01", [128, 128])
    scratch("s_fm", [FM_ROWS, T], BF16)
    scratch("s_tm", [T, TM_COLS], BF16)
    scratch("s_gate", [T, 24], F32)
    scratch("s_alphaT", [16, T], F32)
    scratch("s_mixT", [MIX_ROWS, T], BF16)
    scratch("s_x1", [T, D], F32)
    scratch("s_h2T", [D, T], BF16)
    scratch("s_h2", [T, D], BF16)
    for nm in ("s_wgb", "s_wub", "s_wdb"):
        io[nm] = nc.dram_tensor(nm, [4096, 8192], BF16, kind="Internal").ap()
    io["s_xs"] = nc.dram_tensor("s_xs", [NSLOT, D], BF16, kind="Internal").ap()
    io["s_ys"] = nc.dram_tensor("s_ys", [NSLOT, D], BF16, kind="Internal").ap()
    inp("g_ffn", [D])
    inp("c_stri", [128, 128])
    inp("c_thr", [128, 8])
    inp("c_jidx", [128, NTILE])
    inp("c_pcol", [128, 1])
    inp("c_rowbase", [128, 2 * NTILE])
    if debug:
        io["dbg_slot"] = nc.dram_tensor("dbg_slot", [128, 2 * NT], F32, kind="ExternalOutput").ap()
        io["dbg_w12"] = nc.dram_tensor("dbg_w12", [128, 2 * NT], F32, kind="ExternalOutput").ap()
        io["dbg_wid"] = nc.dram_tensor("dbg_wid", [128, NTILE], F32, kind="ExternalOutput").ap()
    io["y"] = nc.dram_tensor("y", [T, D], F32, kind="ExternalOutput").ap()

    with ExitStack() as st:
        S = Sched(nc)
        S.setup(st)
        big = st.enter_context(nc.sbuf_tensor("big", [128, SBUF_WORDS], F32))
        ps = st.enter_context(nc.psum_tensor("ps", [128, 4096], F32))
        K = Ctx(nc, S, big, ps)
        identf = K.tile(128, F32)
        gmix = K.tile(KC, F32)
        identb = K.tile(128, BF16)
        io["kcT"] = K.tile(256, BF16)
        io["vc"] = K.tile(256, BF16)
        io["C"] = K.tile(NT * 32, F32)
        io["w12"] = K.tile(2 * NT, F32)
        io["slot"] = K.tile(2 * NT, F32).bitcast(I32)
        io["widx"] = K.tile(NTILE, F32).bitcast(I32)
        io["ridx"] = K.tile(2 * NTILE, F32).bitcast(I32)
        io["bg"] = BgCast(K, io, [K.tile(2048, BF16) for _ in range(4)])
        gffn = K.tile(KC, F32)
        io["gffn"] = gffn
        K.base = K.off
        K.dma("q_sp", gffn, io["c_gffn"][:, :], [], ["gffn"])
        K.dma("q_sp", identf, io["c_identf"][:, :], [], ["identf"])
        K.dma("q_sp", gmix, io["c_gmix"][:, :], [], ["gmix"])
        io["identf"] = identf
        io["gmix"] = gmix
        io["identb"] = identb
        K.op("dve", "tensor_copy", ["identf"], ["identb"], out=identb, in_=identf)
        S.barrier()
        if "a" in upto:
            phase_a(K, io)
        if "b" in upto:
            phase_b(K, io)
        if "c" in upto:
            phase_c(K, io)
        if "d" in upto:
            phase_d(K, io)
        if "e" in upto:
            phase_e(K, io)
        if "f" in upto:
            phase_f(K, io)
        if "w" in upto:
            phase_w(K, io)
        if "g" in upto:
            phase_e2(K, io)
        if "s" in upto:
            phase_fs(K, io)
        S.barrier()
        with nc.Block() as block:
            S.emit(block)
    return nc, S


def _make_consts():
    f = np.float32
    c = {}
    q = np.arange(128)[:, None]
    u = np.arange(2048)[None, :]
    d = (q + 1920 - u).astype(f)
    c["c_dsel"] = np.where(d < 0, BIGD, d).astype(f)
    cc = np.arange(640)[None, :]
    d = (q + 512 - cc).astype(f)
    c["c_dwin"] = np.where((d < 0) | (d >= 512), BIGD, d).astype(f)
    n = np.arange(127)[None, None, :]
    qt = np.arange(NT)[None, :, None]
    d = (qt * 128 + q[:, :, None] - (16 * n + 31)).astype(f)
    c["c_dcmp"] = np.where(d < 0, BIGD, d).astype(f).reshape(128, NT * 127)
    t = (np.arange(NT)[None, :] * 128 + q)
    c["c_rowvalid"] = (t >= 31).astype(f)
    j = np.arange(32)[None, None, :]
    tt = t[:, :, None]
    cur = tt // 64
    forced = (j == 0) | ((j <= cur) & (j > cur - 2))
    causal = (j * 64 <= tt)
    c["c_mulmask"] = ((~forced) & causal).astype(f).reshape(128, NT * 32)
    c["c_addmask"] = np.where(forced, BIG, np.where(causal, 0.0, -BIG)).astype(f).reshape(128, NT * 32)
    cs = np.arange(127)[:, None] * 16
    ss = np.arange(32)[None, :] * 64
    c["c_overlap"] = ((cs < ss + 64) & (cs + 32 > ss)).astype(f)
    c["c_identf"] = np.eye(128, dtype=f)
    tri = (np.arange(128)[:, None] <= np.arange(128)[None, :])
    c["c_tris"] = (tri * (-1.0 / 16.0)).astype(f)
    c["c_mask01"] = tri.astype(f)
    c["c_stri"] = (np.arange(128)[:, None] < np.arange(128)[None, :]).astype(f)
    c["c_thr"] = np.tile((np.arange(8) * 256.0)[None, :], (128, 1)).astype(f)
    c["c_jidx"] = np.tile(np.arange(48, dtype=f)[None, :], (128, 1)).astype(f)
    c["c_pcol"] = np.arange(128, dtype=f)[:, None]
    c["c_rowbase"] = (np.arange(96, dtype=f)[None, :] * 128 + np.arange(128, dtype=f)[:, None]).astype(f)
    return {k: np.ascontiguousarray(v) for k, v in c.items()}


_CONSTS = _make_consts()


def host_inputs(inputs, b):
    f = np.float32
    m = {}
    m["x"] = np.ascontiguousarray(inputs["x"][b], dtype=f)
    m["w_in"] = np.ascontiguousarray(inputs["w_in"][0], dtype=f)
    m["c_identf"] = np.eye(128, dtype=f)
    m["c_gmix"] = np.ascontiguousarray(inputs["g_mix_norm"][0].reshape(KC, 128).T, dtype=f)
    m["b_gate"] = np.ascontiguousarray(inputs["b_nsa_gate"][0], dtype=f)
    m["w_cmp_k1"] = np.ascontiguousarray(inputs["w_cmp_k1"][0], dtype=f)
    m["w_cmp_k2"] = np.ascontiguousarray(inputs["w_cmp_k2"][0], dtype=f)
    m["w_cmp_v1"] = np.ascontiguousarray(inputs["w_cmp_v1"][0], dtype=f)
    m["w_cmp_v2"] = np.ascontiguousarray(inputs["w_cmp_v2"][0], dtype=f)
    m["c_posk"] = np.ascontiguousarray(inputs["cmp_pos_k"][0].T, dtype=f)
    m["c_posv"] = np.ascontiguousarray(inputs["cmp_pos_v"][0].T, dtype=f)
    m["w_alpha2"] = np.ascontiguousarray(inputs["w_alpha2"][0], dtype=f)
    m["b_alpha"] = np.ascontiguousarray(inputs["b_alpha"][0], dtype=f)
    m["g_gla"] = np.ascontiguousarray(inputs["g_gla_norm"][0], dtype=f)
    m["w_out"] = np.ascontiguousarray(inputs["w_out"][0], dtype=f)
    m["c_gffn"] = np.ascontiguousarray(inputs["g_ffn_norm"][0].reshape(KC, 128).T, dtype=f)
    m["w_rg"] = np.ascontiguousarray(inputs["w_router_group"][0], dtype=f)
    m["w_re"] = np.ascontiguousarray(inputs["w_router_expert"][0].reshape(D, 32), dtype=f)
    m["b_rg"] = np.ascontiguousarray(inputs["b_router_group"][0], dtype=f)
    m["b_re"] = np.ascontiguousarray(inputs["b_router_expert"][0].reshape(32), dtype=f)
    m["w_eg"] = np.ascontiguousarray(inputs["w_expert_gate"][0], dtype=f)
    m["w_eu"] = np.ascontiguousarray(inputs["w_expert_up"][0], dtype=f)
    m["w_ed"] = np.ascontiguousarray(inputs["w_expert_down"][0], dtype=f)
    m["g_final"] = np.ascontiguousarray(inputs["g_final_norm"], dtype=f)
    m["g_ffn"] = np.ascontiguousarray(inputs["g_ffn_norm"][0], dtype=f)
    m.update(_CONSTS)
    return m


def kernel(**inputs):
    nc, _ = build_nc("abcdewgs", debug=False)
    in_maps = [host_inputs(inputs, b) for b in range(8)]
    res = run_bass_kernel_spmd(nc, in_maps, core_ids=list(range(8)))
    return np.stack([np.asarray(r["y"], dtype=np.float32) for r in res.results], axis=0)
```
